# Optimizing a Trainium2 kernel written in Bass

```python
import jax, jax.numpy as jnp
from jax import lax
import numpy as np

D_MODEL = 2048
BATCH = 4
SEQ = 4096
DEPTH = 1

CHUNK = 64
Q_BLOCK = 128
ROPE_THETA = 500000.0
EPS = 1e-5

A_HEADS = 8
A_HEAD_DIM = 128
A_ROT_DIM = A_HEAD_DIM // 4
IDX_HEADS = 16
IDX_DIM = 64
IDX_ROT_DIM = IDX_DIM // 4
TOPK_MAX = 256
INDEX_SCALE = (IDX_DIM ** -0.5) * (IDX_HEADS ** -0.5)

B_HEADS = 8
Q_LORA = 512
KV_LORA = 256
QK_NOPE = 128
QK_ROPE = 64
V_HEAD = 128

A_WIDTH = A_HEADS * A_HEAD_DIM
B_WIDTH = B_HEADS * V_HEAD
MIX_WIDTH = A_WIDTH + B_WIDTH

IN_SPLITS = (A_WIDTH, A_WIDTH, A_WIDTH,
             IDX_HEADS * IDX_DIM, IDX_DIM, IDX_HEADS,
             Q_LORA, KV_LORA, QK_ROPE)
IN_WIDTH = sum(IN_SPLITS)

N_EXPERTS = 32
TOP_K_EXPERTS = 4
D_FF = D_MODEL
SWIGLU_LIMIT = 7.0
SWIGLU_ALPHA = 1.702
MOE_BLOCK = 128

ALPHA = (2 * DEPTH) ** 0.25
BETA = (8 * DEPTH) ** -0.25

kernel_name = "hybrid_dsa_mla_moe_deepnorm_adaln"


def layer_norm_plain(x):
    xf = x.astype(jnp.float32)
    mu = jnp.mean(xf, -1, keepdims=True)
    var = jnp.mean(jnp.square(xf - mu), -1, keepdims=True)
    return ((xf - mu) * lax.rsqrt(var + EPS)).astype(x.dtype)


def layer_norm(x, g, b):
    return (layer_norm_plain(x) * g + b).astype(x.dtype)


def rms_norm(x, g):
    xf = x.astype(jnp.float32)
    y = xf * lax.rsqrt(jnp.mean(jnp.square(xf), -1, keepdims=True) + EPS)
    return (y * g).astype(x.dtype)


def apply_rope(x, positions, rot_dim):
    half = rot_dim // 2
    inv_freq = ROPE_THETA ** (-jnp.arange(half, dtype=jnp.float32) / half)
    ang = positions.astype(jnp.float32)[..., None] * inv_freq
    cos = jnp.cos(ang)[:, :, None, :]
    sin = jnp.sin(ang)[:, :, None, :]
    xr = x[..., :rot_dim].astype(jnp.float32)
    x1, x2 = xr[..., :half], xr[..., half:]
    rot = jnp.concatenate([x1 * cos - x2 * sin, x2 * cos + x1 * sin], -1).astype(x.dtype)
    return jnp.concatenate([rot, x[..., rot_dim:]], -1)


def dsa_attention(q, k, v, iq, ik, iw):
    B, S, H, dh = q.shape
    n_chunks = S // CHUNK
    topk = min(TOPK_MAX, S // 4)
    key_chunk = jnp.arange(S) // CHUNK
    gather = jax.vmap(lambda t, i: t[i])

    def to_chunks(t):
        return jnp.moveaxis(t.reshape((B, n_chunks, CHUNK) + t.shape[2:]), 1, 0)

    def one_chunk(args):
        qc, iqc, iwc, ci = args
        rel = jax.nn.relu(jnp.einsum("bqhd,bsd->bqhs", iqc, ik,
                                     preferred_element_type=jnp.float32))
        index_score = jnp.einsum("bqhs,bqh->bqs", rel, iwc.astype(jnp.float32)) * INDEX_SCALE
        visible = key_chunk <= ci
        index_score = jnp.where(visible[None, None, :], index_score, -jnp.inf)
        _, sel = lax.top_k(index_score, topk)
        k_sel = gather(k, sel)
        v_sel = gather(v, sel)
        s = jnp.einsum("bqhd,bqkhd->bhqk", qc, k_sel,
                       preferred_element_type=jnp.float32) * (dh ** -0.5)
        ok = (sel // CHUNK) <= ci
        s = jnp.where(ok[:, None], s, -jnp.inf)
        p = jax.nn.softmax(s, axis=-1).astype(v.dtype)
        return jnp.einsum("bhqk,bqkhd->bqhd", p, v_sel)

    out = lax.map(one_chunk, (to_chunks(q), to_chunks(iq), to_chunks(iw),
                              jnp.arange(n_chunks)))
    return jnp.moveaxis(out, 0, 1).reshape(B, S, H, dh)


def block_causal_attention(q, k, v):
    B, S, H, dq = q.shape
    n_blocks = S // Q_BLOCK
    key_chunk = jnp.arange(S) // CHUNK
    qs = jnp.moveaxis(q.reshape(B, n_blocks, Q_BLOCK, H, dq), 1, 0)

    def one_block(args):
        qb, bi = args
        q_chunk = (bi * Q_BLOCK + jnp.arange(Q_BLOCK)) // CHUNK
        s = jnp.einsum("bqhd,bshd->bhqs", qb, k,
                       preferred_element_type=jnp.float32) * (dq ** -0.5)
        mask = key_chunk[None, :] <= q_chunk[:, None]
        s = jnp.where(mask, s, -jnp.inf)
        p = jax.nn.softmax(s, axis=-1).astype(v.dtype)
        return jnp.einsum("bhqs,bshd->bqhd", p, v)

    out = lax.map(one_block, (qs, jnp.arange(n_blocks)))
    return jnp.moveaxis(out, 0, 1).reshape(B, S, H, v.shape[-1])


def token_mixers(h, positions, w_in, idx_k_norm_g, idx_k_norm_b, q_norm_g, w_q_up,
                 kv_norm_g, w_kv_up, out_norm_a_g, out_norm_b_g, w_out):
    B, S, _ = h.shape
    proj = h @ w_in
    offsets = np.cumsum(IN_SPLITS)[:-1].tolist()
    aq, ak, av, iq, ik, iw, qd, kvd, kr = jnp.split(proj, offsets, axis=-1)

    aq = apply_rope(aq.reshape(B, S, A_HEADS, A_HEAD_DIM), positions, A_ROT_DIM)
    ak = apply_rope(ak.reshape(B, S, A_HEADS, A_HEAD_DIM), positions, A_ROT_DIM)
    av = av.reshape(B, S, A_HEADS, A_HEAD_DIM)
    iq = apply_rope(iq.reshape(B, S, IDX_HEADS, IDX_DIM), positions, IDX_ROT_DIM)
    ik = layer_norm(ik, idx_k_norm_g, idx_k_norm_b)
    ik = apply_rope(ik[:, :, None, :], positions, IDX_ROT_DIM)[:, :, 0, :]
    out_a = dsa_attention(aq, ak, av, iq, ik, iw).reshape(B, S, A_WIDTH)

    cq = rms_norm(qd, q_norm_g)
    qb = (cq @ w_q_up).reshape(B, S, B_HEADS, QK_NOPE + QK_ROPE)
    q_rope = apply_rope(qb[..., QK_NOPE:], positions, QK_ROPE)
    qb = jnp.concatenate([qb[..., :QK_NOPE], q_rope], -1)
    ckv = rms_norm(kvd, kv_norm_g)
    kv = (ckv @ w_kv_up).reshape(B, S, B_HEADS, QK_NOPE + V_HEAD)
    k_rope = apply_rope(kr[:, :, None, :], positions, QK_ROPE)
    kb = jnp.concatenate([kv[..., :QK_NOPE],
                          jnp.broadcast_to(k_rope, (B, S, B_HEADS, QK_ROPE))], -1)
    vb = kv[..., QK_NOPE:]
    out_b = block_causal_attention(qb, kb, vb).reshape(B, S, B_WIDTH)

    merged = jnp.concatenate([rms_norm(out_a, out_norm_a_g),
                              rms_norm(out_b, out_norm_b_g)], -1)
    return merged @ w_out


def moe_ffn(h, w_router, b_router, w_gate_up, b_gate_up, w_down, b_down):
    B, S, D = h.shape
    N = B * S
    xt = h.reshape(N, D)
    logits = (xt @ w_router + b_router).astype(jnp.float32)
    top_logit, top_e = lax.top_k(logits, TOP_K_EXPERTS)
    gates = jax.nn.softmax(top_logit, axis=-1)

    A = N * TOP_K_EXPERTS
    e_flat = top_e.reshape(A)
    tok_flat = jnp.repeat(jnp.arange(N, dtype=jnp.int32), TOP_K_EXPERTS)
    g_flat = gates.reshape(A)
    order = jnp.argsort(e_flat)
    e_sorted, tok_sorted, g_sorted = e_flat[order], tok_flat[order], g_flat[order]

    counts = jnp.zeros((N_EXPERTS,), jnp.int32).at[e_flat].add(1)
    starts = jnp.cumsum(counts) - counts
    padded = (counts + MOE_BLOCK - 1) // MOE_BLOCK * MOE_BLOCK
    pad_ends = jnp.cumsum(padded)
    pad_starts = pad_ends - padded
    dest = pad_starts[e_sorted] + (jnp.arange(A, dtype=jnp.int32) - starts[e_sorted])

    n_blocks = (A + MOE_BLOCK - 1) // MOE_BLOCK + N_EXPERTS
    R = n_blocks * MOE_BLOCK
    row_tok = jnp.full((R,), N, jnp.int32).at[dest].set(tok_sorted)
    row_gate = jnp.zeros((R,), jnp.float32).at[dest].set(g_sorted)
    block_e = jnp.minimum(
        jnp.searchsorted(pad_ends, jnp.arange(n_blocks, dtype=jnp.int32) * MOE_BLOCK,
                         side="right"), N_EXPERTS - 1)
    x_pad = jnp.concatenate([xt, jnp.zeros((1, D), xt.dtype)], 0)

    def one_block(args):
        toks, gts, e = args
        xb = x_pad[toks]
        gu = xb @ w_gate_up[e] + b_gate_up[e]
        g, u = gu[:, ::2], gu[:, 1::2]
        g = jnp.minimum(g, SWIGLU_LIMIT)
        u = jnp.clip(u, -SWIGLU_LIMIT, SWIGLU_LIMIT)
        act = (u + 1.0) * (g * jax.nn.sigmoid(SWIGLU_ALPHA * g))
        y = act @ w_down[e] + b_down[e]
        return y * gts[:, None].astype(y.dtype)

    ys = lax.map(one_block, (row_tok.reshape(n_blocks, MOE_BLOCK),
                             row_gate.reshape(n_blocks, MOE_BLOCK), block_e))
    out = jnp.zeros((N + 1, D), h.dtype).at[row_tok].add(ys.reshape(R, D))
    return out[:N].reshape(B, S, D)


def setup_inputs(seed: int = 0) -> dict:
    key = jax.random.key(seed)
    ks = jax.random.split(key, 25)
    f32 = jnp.float32

    def nrm(k, shape, fan_in, scale=1.0):
        return jax.random.normal(k, shape, f32) * (scale * fan_in ** -0.5)

    def gain(k, shape):
        return 1.0 + 0.01 * jax.random.normal(k, shape, f32)

    def bias(k, shape):
        return 0.01 * jax.random.normal(k, shape, f32)

    L = DEPTH
    return {
        "x": jax.random.normal(ks[0], (BATCH, SEQ, D_MODEL), f32),
        "c": jax.random.normal(ks[1], (BATCH, D_MODEL), f32),
        "positions": jnp.arange(SEQ, dtype=jnp.int32)[None, :]
                     + jax.random.randint(ks[2], (BATCH, 1), 0, 8192, jnp.int32),
        "w_ada": nrm(ks[3], (L, D_MODEL, 6 * D_MODEL), D_MODEL, 0.5),
        "b_ada": bias(ks[4], (L, 6 * D_MODEL)),
        "w_in": nrm(ks[5], (L, D_MODEL, IN_WIDTH), D_MODEL),
        "idx_k_norm_g": gain(ks[6], (L, IDX_DIM)),
        "idx_k_norm_b": bias(ks[7], (L, IDX_DIM)),
        "q_norm_g": gain(ks[8], (L, Q_LORA)),
        "w_q_up": nrm(ks[9], (L, Q_LORA, B_HEADS * (QK_NOPE + QK_ROPE)), Q_LORA),
        "kv_norm_g": gain(ks[10], (L, KV_LORA)),
        "w_kv_up": nrm(ks[11], (L, KV_LORA, B_HEADS * (QK_NOPE + V_HEAD)), KV_LORA),
        "out_norm_a_g": gain(ks[12], (L, A_WIDTH)),
        "out_norm_b_g": gain(ks[13], (L, B_WIDTH)),
        "w_out": nrm(ks[14], (L, MIX_WIDTH, D_MODEL), MIX_WIDTH, BETA),
        "ln_mix_g": gain(ks[15], (L, D_MODEL)),
        "ln_mix_b": bias(ks[16], (L, D_MODEL)),
        "w_router": nrm(ks[17], (L, D_MODEL, N_EXPERTS), D_MODEL),
        "b_router": bias(ks[18], (L, N_EXPERTS)),
        "w_gate_up": nrm(ks[19], (L, N_EXPERTS, D_MODEL, 2 * D_FF), D_MODEL),
        "b_gate_up": bias(ks[20], (L, N_EXPERTS, 2 * D_FF)),
        "w_down": nrm(ks[21], (L, N_EXPERTS, D_FF, D_MODEL), D_FF, BETA),
        "b_down": bias(ks[22], (L, N_EXPERTS, D_MODEL)),
        "ln_ffn_g": gain(ks[23], (L, D_MODEL)),
        "ln_ffn_b": bias(ks[24], (L, D_MODEL)),
    }


def reference(x, c, positions, w_ada, b_ada, w_in, idx_k_norm_g, idx_k_norm_b,
              q_norm_g, w_q_up, kv_norm_g, w_kv_up, out_norm_a_g, out_norm_b_g, w_out,
              ln_mix_g, ln_mix_b, w_router, b_router, w_gate_up, b_gate_up, w_down,
              b_down, ln_ffn_g, ln_ffn_b):
    for l in range(DEPTH):
        ada = jax.nn.silu(c) @ w_ada[l] + b_ada[l]
        sh1, sc1, g1, sh2, sc2, g2 = jnp.split(ada[:, None, :], 6, axis=-1)

        h = layer_norm_plain(x) * (1.0 + sc1) + sh1
        mix = token_mixers(h, positions, w_in[l], idx_k_norm_g[l], idx_k_norm_b[l],
                           q_norm_g[l], w_q_up[l], kv_norm_g[l], w_kv_up[l],
                           out_norm_a_g[l], out_norm_b_g[l], w_out[l])
        x = layer_norm(ALPHA * x + g1 * mix, ln_mix_g[l], ln_mix_b[l])

        h = layer_norm_plain(x) * (1.0 + sc2) + sh2
        ffn = moe_ffn(h, w_router[l], b_router[l], w_gate_up[l], b_gate_up[l],
                      w_down[l], b_down[l])
        x = layer_norm(ALPHA * x + g2 * ffn, ln_ffn_g[l], ln_ffn_b[l])
    return x
```

```python
import os
from contextlib import ExitStack
import numpy as np
import concourse.bass as bass
import concourse.mybir as mybir
from concourse.bass_utils import run_bass_kernel_spmd

F32 = mybir.dt.float32
F32R = mybir.dt.float32r
BF16 = mybir.dt.bfloat16
I32 = mybir.dt.int32
AF = mybir.ActivationFunctionType
ALU = mybir.AluOpType
AX = mybir.AxisListType

D = 2048
SEQ = 4096
NB = 32
NOWN = 16
EPS = 1e-5
ALPHA = 2.0 ** 0.25
THETA = 500000.0
NEXP = 32
NEG = -1.0e30
IN_W = 5008
TWO_PI = 2.0 * np.pi
C1 = 6.28125
C2 = TWO_PI - C1


def R(ap):
    return ap.bitcast(F32R)


class Sched:
    def __init__(self, nc, es, ndma=28):
        self.nc = nc
        self.E = {}
        for name, eng in [("pe", nc.tensor), ("act", nc.scalar), ("dve", nc.vector),
                          ("pool", nc.gpsimd), ("sp", nc.sync)]:
            sem = es.enter_context(nc.semaphore("sem_" + name))
            self.E[name] = dict(eng=eng, sem=sem, cnt=0, waited={})
        self.dsem = [es.enter_context(nc.semaphore("dsem%d" % i)) for i in range(ndma)]
        self.dcnt = [0] * ndma
        self.dpool = {"sp": list(range(0, 16)), "act": list(range(16, ndma))}
        self.drr = {"sp": 0, "act": 0}
        self.lw = {}
        self.rd = {}
        self.ninst = 0

    def semh(self, sid):
        if isinstance(sid, tuple):
            return self.dsem[sid[1]]
        return self.E[sid]["sem"]

    def _wait(self, en, toks):
        E = self.E[en]
        need = {}
        for t in toks:
            if t is None:
                continue
            sid, v = t
            if E["waited"].get(sid, 0) < v:
                need[sid] = max(need.get(sid, 0), v)
        for sid, v in need.items():
            E["eng"].wait_ge(self.semh(sid), v)
            E["waited"][sid] = v
            self.ninst += 1

    def _deps(self, reads, writes):
        toks = []
        for k in reads:
            toks.append(self.lw.get(k))
            if isinstance(k, str) and k.startswith("ps"):
                toks += list(self.rd.get(k, {}).items())
        for k in writes:
            toks.append(self.lw.get(k))
            toks += list(self.rd.get(k, {}).items())
        return toks

    def _record(self, tok, reads, writes):
        for k in reads:
            d = self.rd.setdefault(k, {})
            d[tok[0]] = max(d.get(tok[0], 0), tok[1])
        for k in writes:
            self.lw[k] = tok
            self.rd[k] = {}

    def op(self, en, fn, reads=(), writes=(), nosync_self=False):
        toks = self._deps(reads, writes)
        if nosync_self:
            toks = [t for t in toks if t is not None and t[0] != en]
        self._wait(en, toks)
        E = self.E[en]
        E["cnt"] += 1
        ins = fn(E["eng"])
        ins.then_inc(E["sem"], 1)
        self.ninst += 1
        tok = (en, E["cnt"])
        E["waited"][en] = max(E["waited"].get(en, 0), 0)
        self._record(tok, reads, writes)
        return tok

    def dma(self, qn, out, in_, reads=(), writes=()):
        if qn == "pool":
            qn = "act"
        pl = self.dpool[qn]
        i = pl[self.drr[qn] % len(pl)]
        self.drr[qn] += 1
        toks = self._deps(reads, writes)
        if self.dcnt[i] > 0:
            toks.append((("d", i), 16 * self.dcnt[i]))
        self._wait(qn, toks)
        self.dcnt[i] += 1
        self.E[qn]["eng"].dma_start(out=out, in_=in_).then_inc(self.dsem[i], 16)
        self.ninst += 1
        tok = (("d", i), 16 * self.dcnt[i])
        self._record(tok, reads, writes)
        return tok

    def barrier(self):
        toks = [(n, e["cnt"]) for n, e in self.E.items() if e["cnt"] > 0]
        toks += [(("d", i), 16 * c) for i, c in enumerate(self.dcnt) if c > 0]
        for en in self.E:
            self._wait(en, toks)
        self.lw = {}
        self.rd = {}


def build(stage=99, debug=False):
    nc = bass.Bass("TRN2", target_bir_lowering=False)
    nc.dge_precook = False
    es = ExitStack()
    S = Sched(nc, es)
    dbg_kind = "ExternalOutput" if debug else "Internal"

    def dram_in(name, shape, dt=F32):
        return nc.dram_tensor(name, list(shape), dt, kind="ExternalInput").ap()

    def dram_scr(name, shape, dt=F32):
        return nc.dram_tensor(name, list(shape), dt, kind=dbg_kind).ap()

    xl = dram_in("xl", [SEQ, D])
    c_fm = dram_in("c_fm", [128, 16])
    pos_i = dram_in("pos_i", [128, NB], I32)
    invf = dram_in("invf", [1, 56])
    w_ada = dram_in("w_ada", [D, 6 * D], F32R)
    b_ada = dram_in("b_ada", [1, 6 * D])
    w_in = dram_in("w_in", [D, IN_W], F32R)
    ikg = dram_in("ikg", [1, 64])
    ikb = dram_in("ikb", [1, 64])
    qng = dram_in("qng", [1, 512])
    w_q_up = dram_in("w_q_up", [512, 1536], F32R)
    kvng = dram_in("kvng", [1, 256])
    w_kv_up = dram_in("w_kv_up", [256, 2048], F32R)
    ong_fm = dram_in("ong_fm", [128, 16])
    w_out = dram_in("w_out", [D, D], F32R)
    lnmg = dram_in("lnmg", [1, D])
    lnmb = dram_in("lnmb", [1, D])
    w_router = dram_in("w_router", [D, NEXP], F32R)
    b_router = dram_in("b_router", [1, NEXP])
    big = stage >= 6
    w_gu = dram_in("w_gu", [NEXP, D, 2 * D], F32R) if big else None
    bgu_fm = dram_in("bgu_fm", [128, NEXP * 32])
    w_dn = dram_in("w_dn", [NEXP, D, D], F32R) if big else None
    b_dn = dram_in("b_dn", [NEXP, D], F32R)
    lnfg = dram_in("lnfg", [1, D])
    lnfb = dram_in("lnfb", [1, D])
    ident_d = dram_in("ident", [128, 128])
    ones_d = dram_in("ones", [128, 128], F32R)
    esel_d = dram_in("esel", [NEXP, NEXP * 128], F32R)
    diagb_d = dram_in("diagb", [128, 128])
    rbias_d = dram_in("rbias", [128, 128])
    mlam_d = dram_in("mlam", [128, 4 * 256])
    out_d = nc.dram_tensor("out", [NOWN * 128, D], F32, kind="ExternalOutput").ap()

    ada_d = dram_scr("ada_d", [1, 6 * D])
    AKT = dram_scr("AKT", [8, 128, SEQ])
    AV = dram_scr("AV", [SEQ, 1024])
    KBT = dram_scr("KBT", [8, 128, SEQ])
    VB = dram_scr("VB", [SEQ, 1024])
    AQT = dram_scr("AQT", [8, 128, 2048])
    IQT = dram_scr("IQT", [8, 128, 2048])
    QBN = dram_scr("QBN", [8, 128, 2048])
    QBR = dram_scr("QBR", [8, 64, 2048])
    IKT_d = dram_scr("IKT_d", [128, SEQ])
    KRT_d = dram_scr("KRT_d", [64, SEQ])
    SGN_d = dram_scr("SGN_d", [2048, 16])
    X1 = dram_scr("X1", [2048, D])
    H2T = dram_scr("H2T", [128, 16, 2048])
    GT_d = dram_scr("GT_d", [NEXP, 2048])
    MRG = dram_scr("MRG", [128, 16, 2048])

    scopes = [es]

    used_names = {}

    def T(name, shape, dt=F32):
        n = used_names.get(name, 0)
        used_names[name] = n + 1
        nm = "sb_" + name + ("" if n == 0 else "_v%d" % n)
        return scopes[-1].enter_context(nc.sbuf_tensor(nm, list(shape), dt))

    def push():
        scopes.append(ExitStack())

    def pop():
        S.barrier()
        scopes.pop().close()

    def P(name, shape, dt=F32):
        return es.enter_context(nc.psum_tensor("pp_" + name, list(shape), dt))

    ident = T("ident", [128, 128])
    ones = T("ones", [128, 128])
    S.dma("sp", ident[:], ident_d, writes=["ident"])
    S.dma("sp", R(ones[:]), ones_d, writes=["ones"])
    ps = [P("ps%d" % i, [128, 512]) for i in range(8)]
    psk = ["ps%d" % i for i in range(8)]
    eps_t = T("eps_t", [128, 1])
    S.op("dve", lambda e: e.memset(eps_t[:], EPS), writes=["eps"])

    push()
    wbuf = [T("wbuf%d" % i, [128, 16, 256]) for i in range(3)]
    wk = ["wbuf%d" % i for i in range(3)]
    push()
    cfm = T("cfm", [128, 16])
    scr = T("scr", [128, 16, 128])
    S.dma("sp", cfm[:], c_fm, writes=["cfm"])
    S.op("act", lambda e: e.activation(out=cfm[:], in_=cfm[:], func=AF.Silu), reads=["cfm"], writes=["cfm"])
    S.op("dve", lambda e: e.tensor_copy(out=R(scr[:]), in_=cfm[:].unsqueeze(2).to_broadcast([128, 16, 128])),
         reads=["cfm"], writes=["scr"])
    bada = [T("bada%d" % i, [128, 256]) for i in range(2)]
    w_ada_v = w_ada.rearrange("(kc p) f -> p kc f", p=128)
    for g in range(48):
        wb, wkk = wbuf[g % 3], wk[g % 3]
        bt, bk = bada[g % 2], "bada%d" % (g % 2)
        S.dma("sp", R(wb[:]), w_ada_v[:, :, g * 256:(g + 1) * 256], writes=[wkk])
        S.dma("sp", bt[:], b_ada[:, g * 256:(g + 1) * 256].partition_broadcast(128), writes=[bk])
        pst, pk = ps[g % 2], psk[g % 2]
        for kc in range(16):
            S.op("pe", lambda e, kc=kc: e.matmul(pst[:, 0:256], lhsT=R(scr[:, kc, :]), rhs=R(wb[:, kc, :]),
                                                 start=(kc == 0), stop=(kc == 15)),
                 reads=["scr", wkk], writes=[pk], nosync_self=True)
        S.op("dve", lambda e: e.tensor_tensor(out=bt[:], in0=pst[:, 0:256], in1=bt[:], op=ALU.add),
             reads=[pk, bk], writes=[bk])
        S.dma("pool", ada_d[:, g * 256:(g + 1) * 256], bt[0:1, :], reads=[bk], writes=["ada_d"])
    pop()
    if stage <= 1:
        return nc, S, es

    SIN = T("SIN", [128, NB, 56])
    COS = T("COS", [128, NB, 56])
    push()
    posi = T("posi", [128, NB], I32)
    posf = T("posf", [128, NB])
    invb = T("invb", [128, 56])
    ang = T("ang", [128, NB, 56])
    tmpa = T("tmpa", [128, NB, 56])
    tmpi = T("tmpi", [128, NB, 56], I32)
    tmpb = T("tmpb", [128, NB, 56])
    S.dma("sp", posi[:], pos_i, writes=["posi"])
    S.dma("sp", invb[:], invf.partition_broadcast(128), writes=["invb"])
    S.op("dve", lambda e: e.tensor_copy(out=posf[:], in_=posi[:]), reads=["posi"], writes=["posf"])
    for blk in range(NB):
        S.op("dve", lambda e, blk=blk: e.tensor_scalar(out=ang[:, blk, :], in0=invb[:], scalar1=posf[:, blk:blk + 1],
                                                       scalar2=None, op0=ALU.mult),
             reads=["posf", "invb"], writes=["ang"])

    def sin_table(dst, dkey, shift):
        S.op("dve", lambda e: e.tensor_scalar(out=tmpa[:], in0=ang[:], scalar1=1.0 / TWO_PI,
                                              scalar2=shift / TWO_PI + 0.5, op0=ALU.mult, op1=ALU.add),
             reads=["ang"], writes=["tmpa"])
        S.op("dve", lambda e: e.tensor_copy(out=tmpi[:], in_=tmpa[:]), reads=["tmpa"], writes=["tmpi"])
        S.op("dve", lambda e: e.tensor_copy(out=tmpa[:], in_=tmpi[:]), reads=["tmpi"], writes=["tmpa"])
        S.op("dve", lambda e: e.scalar_tensor_tensor(out=tmpb[:], in0=tmpa[:], scalar=-C1, in1=ang[:],
                                                     op0=ALU.mult, op1=ALU.add),
             reads=["tmpa", "ang"], writes=["tmpb"])
        S.op("dve", lambda e: e.scalar_tensor_tensor(out=tmpb[:], in0=tmpa[:], scalar=-C2, in1=tmpb[:],
                                                     op0=ALU.mult, op1=ALU.add),
             reads=["tmpa", "tmpb"], writes=["tmpb"])
        if shift != 0.0:
            S.op("dve", lambda e: e.tensor_scalar(out=tmpb[:], in0=tmpb[:], scalar1=shift, scalar2=None, op0=ALU.add),
                 reads=["tmpb"], writes=["tmpb"])
        S.op("dve", lambda e: e.tensor_scalar(out=tmpa[:], in0=tmpb[:], scalar1=np.pi, scalar2=-TWO_PI,
                                              op0=ALU.is_gt, op1=ALU.mult), reads=["tmpb"], writes=["tmpa"])
        S.op("dve", lambda e: e.tensor_tensor(out=tmpb[:], in0=tmpb[:], in1=tmpa[:], op=ALU.add),
             reads=["tmpa", "tmpb"], writes=["tmpb"])
        S.op("dve", lambda e: e.tensor_scalar(out=tmpa[:], in0=tmpb[:], scalar1=-np.pi, scalar2=TWO_PI,
                                              op0=ALU.is_lt, op1=ALU.mult), reads=["tmpb"], writes=["tmpa"])
        S.op("dve", lambda e: e.tensor_tensor(out=tmpb[:], in0=tmpb[:], in1=tmpa[:], op=ALU.add),
             reads=["tmpa", "tmpb"], writes=["tmpb"])
        S.op("dve", lambda e: e.tensor_scalar(out=tmpb[:], in0=tmpb[:], scalar1=3.1415925, scalar2=-3.1415925,
                                              op0=ALU.min, op1=ALU.max), reads=["tmpb"], writes=["tmpb"])
        S.op("act", lambda e: e.activation(out=dst[:], in_=tmpb[:], func=AF.Sin), reads=["tmpb"], writes=[dkey])

    sin_table(SIN, "SIN", 0.0)
    sin_table(COS, "COS", np.pi / 2)
    pop()
    FA, FI, FM = (0, 16), (16, 8), (24, 32)

    G = 4
    sh1 = T("sh1", [128, D])
    sc1 = T("sc1", [128, D])
    S.dma("sp", sh1[:], ada_d[:, 0:D].partition_broadcast(128), writes=["sh1"])
    S.dma("sp", sc1[:], ada_d[:, D:2 * D].partition_broadcast(128), writes=["sc1"])
    S.op("dve", lambda e: e.tensor_scalar(out=sc1[:], in0=sc1[:], scalar1=1.0, scalar2=None, op0=ALU.add),
         reads=["sc1"], writes=["sc1"])
    ikg_b = T("ikg_b", [128, 64]); ikb_b = T("ikb_b", [128, 64])
    qng_b = T("qng_b", [128, 512]); kvng_b = T("kvng_b", [128, 256])
    S.dma("sp", ikg_b[:], ikg.partition_broadcast(128), writes=["ikg_b"])
    S.dma("sp", ikb_b[:], ikb.partition_broadcast(128), writes=["ikb_b"])
    S.dma("sp", qng_b[:], qng.partition_broadcast(128), writes=["qng_b"])
    S.dma("sp", kvng_b[:], kvng.partition_broadcast(128), writes=["kvng_b"])

    xt = [T("xt%d" % i, [128, D]) for i in range(2)]
    hT = T("hT", [128, 16, G * 128])
    stg = [T("stg%d" % i, [128, 512]) for i in range(2)]
    stT = [T("stT%d" % i, [128, 4, G * 128]) for i in range(2)]
    qdst = T("qdst", [128, G, 512])
    cqT = T("cqT", [128, 4, G * 128])
    ckvT = T("ckvT", [128, 2, G * 128])
    absw = T("absw", [128, G, 16])
    sgn = T("sgn", [128, G, 16])
    st6 = T("st6", [128, 4, 6]); mv = T("mv", [128, 2]); rstd = T("rstd", [128, 1])
    sm = [T("sm%d" % i, [128, 4, 32]) for i in range(4)]
    IKT = T("IKT", [128, SEQ])
    KRT = T("KRT", [64, SEQ])
    ikst = T("ikst", [128, 128])
    w_in_v = w_in.rearrange("(kc p) f -> p kc f", p=128)
    wq_v = w_q_up.rearrange("(kc p) f -> p kc f", p=128)
    wkv_v = w_kv_up.rearrange("(kc p) f -> p kc f", p=128)
    cnt = dict(w=0, ps=0, stg=0, stT=0, x=0)

    def ln_stats(src_ap, n, skey):
        return _ln_stats(src_ap, n, skey)

    def _ln_stats(src_ap, n, skey):
        nch = max(1, n // 512)
        w_ = n // nch
        for c in range(nch):
            S.op("dve", lambda e, c=c: e.bn_stats(out=st6[:, c, :], in_=src_ap[:, c * w_:(c + 1) * w_]),
                 reads=[skey], writes=["st6"])
        S.op("dve", lambda e: e.bn_aggr(out=mv[:], in_=st6[:, 0:nch, :]), reads=["st6"], writes=["mv"])
        S.op("act", lambda e: e.activation(out=rstd[:], in_=mv[:, 1:2], func=AF.Ln, bias=eps_t[:], scale=1.0),
             reads=["mv", "eps"], writes=["rstd"])
        S.op("act", lambda e: e.activation(out=rstd[:], in_=rstd[:], func=AF.Exp, scale=-0.5),
             reads=["rstd"], writes=["rstd"])

    def rms_rstd(src_ap, n, skey, junk_ap, jkey):
        S.op("act", lambda e: e.activation(out=junk_ap, in_=src_ap, func=AF.Square, accum_out=mv[:, 0:1]),
             reads=[skey], writes=[jkey, "mv"])
        S.op("act", lambda e: e.activation(out=rstd[:], in_=mv[:, 0:1], func=AF.Ln, bias=eps_t[:], scale=1.0 / n),
             reads=["mv", "eps"], writes=["rstd"])
        S.op("act", lambda e: e.activation(out=rstd[:], in_=rstd[:], func=AF.Exp, scale=-0.5),
             reads=["rstd"], writes=["rstd"])

    def rope(dst, dkey, src, skey, H, off, half, blk, fslot):
        f0 = fslot[0]
        cosb = COS[:, blk, f0:f0 + half].unsqueeze(1).to_broadcast([128, H, half])
        sinb = SIN[:, blk, f0:f0 + half].unsqueeze(1).to_broadcast([128, H, half])
        x1 = src[:, :, off:off + half]
        x2 = src[:, :, off + half:off + 2 * half]
        t = [sm[i][:, 0:H, 0:half] for i in range(4)]
        S.op("dve", lambda e: e.tensor_tensor(out=t[0], in0=x1, in1=cosb, op=ALU.mult), reads=[skey, "COS"], writes=["sm0"])
        S.op("dve", lambda e: e.tensor_tensor(out=t[1], in0=x2, in1=sinb, op=ALU.mult), reads=[skey, "SIN"], writes=["sm1"])
        S.op("dve", lambda e: e.tensor_tensor(out=t[2], in0=x2, in1=cosb, op=ALU.mult), reads=[skey, "COS"], writes=["sm2"])
        S.op("dve", lambda e: e.tensor_tensor(out=t[3], in0=x1, in1=sinb, op=ALU.mult), reads=[skey, "SIN"], writes=["sm3"])
        S.op("dve", lambda e: e.tensor_tensor(out=dst[:, :, off:off + half], in0=t[0], in1=t[1], op=ALU.subtract),
             reads=["sm0", "sm1"], writes=[dkey])
        S.op("dve", lambda e: e.tensor_tensor(out=dst[:, :, off + half:off + 2 * half], in0=t[2], in1=t[3], op=ALU.add),
             reads=["sm2", "sm3"], writes=[dkey])

    def transpose_to(dst_ap, dkey, src_ap, skey, ncol):
        i = cnt["ps"] % 4 + 4
        cnt["ps"] += 1
        S.op("pe", lambda e: e.transpose(out=ps[i][0:ncol, 0:128], in_=src_ap, identity=ident[:]),
             reads=[skey, "ident"], writes=[psk[i]])
        S.op("act", lambda e: e.copy(out=R(dst_ap), in_=ps[i][0:ncol, 0:128]), reads=[psk[i]], writes=[dkey])

    def proj_block(lhs_tile, lkey, nkc, wtile, wkey, ncols, bi):
        i = cnt["ps"] % 4
        cnt["ps"] += 1
        for kc in range(nkc):
            S.op("pe", lambda e, kc=kc: e.matmul(ps[i][:, 0:ncols], lhsT=R(lhs_tile[:, kc, bi * 128:(bi + 1) * 128]),
                                                 rhs=R(wtile[:, kc, 0:ncols]), start=(kc == 0), stop=(kc == nkc - 1)),
                 reads=[lkey, wkey], writes=[psk[i]], nosync_self=True)
        return ps[i], psk[i]

    def load_w(view, c0, ncols, nkc):
        i = cnt["w"] % len(wbuf)
        cnt["w"] += 1
        wt = wbuf[i]
        flat = wt[:].rearrange("p a b -> p (a b)")
        dst = flat[:, 0:nkc * ncols].rearrange("p (a b) -> p a b", a=nkc)
        S.dma("sp", R(dst), view[:, 0:nkc, c0:c0 + ncols], writes=[wk[i]])
        return dst, wk[i]

    ngroups = NB // G
    tglist = list(range(int(os.environ.get("KDBG_TG", ngroups))))
    if os.environ.get("KDBG_TGLIST"):
        tglist = [int(v) for v in os.environ["KDBG_TGLIST"].split(",")]
    for tg in tglist:
        own = tg < (NOWN // G)
        for bi in range(G):
            blk = tg * G + bi
            xi = cnt["x"] % 2
            cnt["x"] += 1
            xb, xk = xt[xi], "xt%d" % xi
            S.dma("sp", xb[:], xl[blk * 128:(blk + 1) * 128, :], writes=[xk])
            ln_stats(xb, D, xk)
            S.op("dve", lambda e: e.tensor_scalar(out=xb[:], in0=xb[:], scalar1=mv[:, 0:1], scalar2=rstd[:],
                                                  op0=ALU.subtract, op1=ALU.mult), reads=[xk, "mv", "rstd"], writes=[xk])
            S.op("pool", lambda e: e.tensor_tensor(out=xb[:], in0=xb[:], in1=sc1[:], op=ALU.mult),
                 reads=[xk, "sc1"], writes=[xk])
            S.op("pool", lambda e: e.tensor_tensor(out=xb[:], in0=xb[:], in1=sh1[:], op=ALU.add),
                 reads=[xk, "sh1"], writes=[xk])
            for kc in range(16):
                transpose_to(hT[:, kc, bi * 128:(bi + 1) * 128], "hT", xb[:, kc * 128:(kc + 1) * 128], xk, 128)

        def stage_out(kind, sub):
            pass

        def run_group(kind, c0, ncols, sub):
            if os.environ.get("KDBG_KINDS") and kind not in os.environ["KDBG_KINDS"].split(","):
                return
            if kind == "qup":
                wt, wkey = load_w(wq_v, c0, ncols, 4)
                lhs, lkey, nkc = cqT, "cqT", 4
            elif kind == "kvup":
                wt, wkey = load_w(wkv_v, c0, ncols, 2)
                lhs, lkey, nkc = ckvT, "ckvT", 2
            else:
                wt, wkey = load_w(w_in_v, c0, ncols, 16)
                lhs, lkey, nkc = hT, "hT", 16
            si = cnt["stT"] % 2
            cnt["stT"] += 1
            sT, sTk = stT[si], "stT%d" % si
            for bi in range(G):
                blk = tg * G + bi
                pt, pk = proj_block(lhs, lkey, nkc, wt, wkey, ncols, bi)
                gi = cnt["stg"] % 2
                cnt["stg"] += 1
                sg, sgk = stg[gi], "stg%d" % gi
                if kind in ("aq", "ak"):
                    S.op("act", lambda e: e.copy(out=sg[:, 0:256], in_=pt[:, 0:256]), reads=[pk], writes=[sgk])
                    v = sg[:, 0:256].rearrange("p (h d) -> p h d", h=2)
                    pv = pt[:, 0:256].rearrange("p (h d) -> p h d", h=2)
                    rope(v, sgk, v, sgk, 2, 0, 16, blk, FA)
                    for h in range(2):
                        transpose_to(sT[:, h, bi * 128:(bi + 1) * 128], sTk, sg[:, h * 128:(h + 1) * 128], sgk, 128)
                elif kind == "av":
                    S.op("act", lambda e: e.copy(out=sg[:, 0:256], in_=pt[:, 0:256]), reads=[pk], writes=[sgk])
                    S.dma("pool", AV[blk * 128:(blk + 1) * 128, sub * 256:(sub + 1) * 256], sg[:, 0:256], reads=[sgk], writes=["AV"])
                elif kind == "iq":
                    S.op("act", lambda e: e.copy(out=sg[:, 0:256], in_=pt[:, 0:256]), reads=[pk], writes=[sgk])
                    v = sg[:, 0:256].rearrange("p (h d) -> p h d", h=4)
                    pv = pt[:, 0:256].rearrange("p (h d) -> p h d", h=4)
                    rope(v, sgk, v, sgk, 4, 0, 8, blk, FI)
                    S.op("dve", lambda e: e.tensor_tensor(out=v, in0=v, in1=absw[:, bi, sub * 4:(sub + 1) * 4].unsqueeze(2).to_broadcast([128, 4, 64]),
                                                          op=ALU.mult), reads=[sgk, "absw"], writes=[sgk])
                    for h in range(2):
                        transpose_to(sT[:, h, bi * 128:(bi + 1) * 128], sTk, sg[:, h * 128:(h + 1) * 128], sgk, 128)
                elif kind == "misc":
                    S.op("act", lambda e: e.copy(out=sg[:, 0:80], in_=pt[:, 0:80]), reads=[pk], writes=[sgk])
                    ln_stats(sg[:, 0:64], 64, sgk)
                    S.op("dve", lambda e: e.tensor_scalar(out=sg[:, 0:64], in0=sg[:, 0:64], scalar1=mv[:, 0:1], scalar2=rstd[:],
                                                          op0=ALU.subtract, op1=ALU.mult), reads=[sgk, "mv", "rstd"], writes=[sgk])
                    S.op("dve", lambda e: e.tensor_tensor(out=sg[:, 0:64], in0=sg[:, 0:64], in1=ikg_b[:], op=ALU.mult),
                         reads=[sgk, "ikg_b"], writes=[sgk])
                    S.op("dve", lambda e: e.tensor_tensor(out=sg[:, 128:192], in0=sg[:, 0:64], in1=ikb_b[:], op=ALU.add),
                         reads=[sgk, "ikb_b"], writes=[sgk])
                    v = sg[:, 128:192].rearrange("p (h d) -> p h d", h=1)
                    rope(v, sgk, v, sgk, 1, 0, 8, blk, FI)
                    S.op("dve", lambda e: e.tensor_copy(out=sg[:, 192:256], in_=sg[:, 128:192]), reads=[sgk], writes=[sgk])
                    transpose_to(IKT[:, blk * 128:(blk + 1) * 128], "IKT", sg[:, 128:256], sgk, 128)
                    if own:
                        S.op("act", lambda e: e.activation(out=absw[:, bi, :], in_=sg[:, 64:80], func=AF.Abs),
                             reads=[sgk], writes=["absw"])
                        S.op("dve", lambda e: e.tensor_scalar(out=sgn[:, bi, :], in0=sg[:, 64:80], scalar1=0.0, scalar2=-0.5,
                                                              op0=ALU.is_ge, op1=ALU.add), reads=[sgk], writes=["sgn"])
                        S.dma("pool", SGN_d[blk * 128:(blk + 1) * 128, :], sgn[:, bi, :], reads=["sgn"], writes=["SGN_d"])
                elif kind == "qd":
                    S.op("act", lambda e: e.copy(out=qdst[:, bi, sub * 256:(sub + 1) * 256], in_=pt[:, 0:256]),
                         reads=[pk], writes=["qdst"])
                    if sub == 1:
                        rms_rstd(qdst[:, bi, :], 512, "qdst", sg[:, 0:512], sgk)
                        S.op("dve", lambda e: e.scalar_tensor_tensor(out=qdst[:, bi, :], in0=qdst[:, bi, :], scalar=rstd[:],
                                                                     in1=qng_b[:], op0=ALU.mult, op1=ALU.mult),
                             reads=["qdst", "rstd", "qng_b"], writes=["qdst"])
                        for c in range(4):
                            transpose_to(cqT[:, c, bi * 128:(bi + 1) * 128], "cqT", qdst[:, bi, c * 128:(c + 1) * 128], "qdst", 128)
                elif kind == "kvd":
                    S.op("act", lambda e: e.copy(out=sg[:, 0:256], in_=pt[:, 0:256]), reads=[pk], writes=[sgk])
                    rms_rstd(sg[:, 0:256], 256, sgk, sg[:, 256:512], sgk)
                    S.op("dve", lambda e: e.scalar_tensor_tensor(out=sg[:, 0:256], in0=sg[:, 0:256], scalar=rstd[:],
                                                                 in1=kvng_b[:], op0=ALU.mult, op1=ALU.mult),
                         reads=[sgk, "rstd", "kvng_b"], writes=[sgk])
                    for c in range(2):
                        transpose_to(ckvT[:, c, bi * 128:(bi + 1) * 128], "ckvT", sg[:, c * 128:(c + 1) * 128], sgk, 128)
                elif kind == "kr":
                    S.op("act", lambda e: e.copy(out=sg[:, 0:64], in_=pt[:, 0:64]), reads=[pk], writes=[sgk])
                    v = sg[:, 0:64].rearrange("p (h d) -> p h d", h=1)
                    rope(v, sgk, v, sgk, 1, 0, 32, blk, FM)
                    transpose_to(KRT[:, blk * 128:(blk + 1) * 128], "KRT", sg[:, 0:64], sgk, 64)
                elif kind == "qup":
                    S.op("act", lambda e: e.copy(out=sg[:, 0:384], in_=pt[:, 0:384]), reads=[pk], writes=[sgk])
                    v = sg[:, 0:384].rearrange("p (h d) -> p h d", h=2)
                    pv = pt[:, 0:384].rearrange("p (h d) -> p h d", h=2)
                    rope(v, sgk, v, sgk, 2, 128, 32, blk, FM)
                    for h in range(2):
                        transpose_to(sT[:, h, bi * 128:(bi + 1) * 128], sTk, sg[:, h * 192:h * 192 + 128], sgk, 128)
                        transpose_to(sT[0:64, 2 + h, bi * 128:(bi + 1) * 128], sTk, sg[:, h * 192 + 128:(h + 1) * 192], sgk, 64)
                elif kind == "kvup":
                    S.op("act", lambda e: e.copy(out=sg[:, 0:256], in_=pt[:, 0:256]), reads=[pk], writes=[sgk])
                    transpose_to(sT[:, 0, bi * 128:(bi + 1) * 128], sTk, sg[:, 0:128], sgk, 128)
                    S.dma("pool", VB[blk * 128:(blk + 1) * 128, sub * 128:(sub + 1) * 128], sg[:, 128:256], reads=[sgk], writes=["VB"])
            t0 = tg * G * 128
            tw = G * 128
            if kind == "ak":
                for h in range(2):
                    S.dma("pool", AKT[sub * 2 + h, :, t0:t0 + tw], sT[:, h, :], reads=[sTk], writes=["AKT"])
            elif kind == "aq":
                for h in range(2):
                    S.dma("pool", AQT[sub * 2 + h, :, t0:t0 + tw], sT[:, h, :], reads=[sTk], writes=["AQT"])
            elif kind == "iq":
                for h in range(2):
                    S.dma("pool", IQT[sub * 2 + h, :, t0:t0 + tw], sT[:, h, :], reads=[sTk], writes=["IQT"])
            elif kind == "qup":
                for h in range(2):
                    S.dma("pool", QBN[sub * 2 + h, :, t0:t0 + tw], sT[:, h, :], reads=[sTk], writes=["QBN"])
                    S.dma("pool", QBR[sub * 2 + h, :, t0:t0 + tw], sT[0:64, 2 + h, :], reads=[sTk], writes=["QBR"])
            elif kind == "kvup":
                S.dma("pool", KBT[sub, :, t0:t0 + tw], sT[:, 0, :], reads=[sTk], writes=["KBT"])

        run_group("misc", 4096, 80, 0)
        for s_ in range(4):
            run_group("ak", 1024 + s_ * 256, 256, s_)
        for s_ in range(4):
            run_group("av", 2048 + s_ * 256, 256, s_)
        run_group("kvd", 4688, 256, 0)
        run_group("kr", 4944, 64, 0)
        for s_ in range(8):
            run_group("kvup", s_ * 256, 256, s_)
        if own:
            for s_ in range(4):
                run_group("aq", s_ * 256, 256, s_)
            for s_ in range(4):
                run_group("iq", 3072 + s_ * 256, 256, s_)
            for s_ in range(2):
                run_group("qd", 4176 + s_ * 256, 256, s_)
            for s_ in range(4):
                run_group("qup", s_ * 384, 384, s_)
    for tg in tglist:
        S.dma("pool", IKT_d[:, tg * 512:(tg + 1) * 512], IKT[:, tg * 512:(tg + 1) * 512], reads=["IKT"], writes=["IKT_d"])
        S.dma("pool", KRT_d[:, tg * 512:(tg + 1) * 512], KRT[:, tg * 512:(tg + 1) * 512], reads=["KRT"], writes=["KRT_d"])
    pop()
    if stage <= 2:
        return nc, S, es

    SCALE_A = 128.0 ** -0.5
    SCALE_B = 192.0 ** -0.5
    BF = BF16

    def attn_core(j, h, kT, kTk, vt, vk, qT_ap, qkey, scale, OT, okey, mask_fn, extra_qk=None):
        NKB = 2 * j + 2
        blocks = [(part, kb) for part in range(2) for kb in range(0, NKB, 2)]
        for idx, (part, kb) in enumerate(blocks):
            pi = cnt["ps"] % 4
            cnt["ps"] += 1
            for t in range(2):
                kcol = (kb + t) * 128
                S.op("pe", lambda e, t=t, kcol=kcol: e.matmul(ps[pi][:, t * 256:(t + 1) * 256], lhsT=R(kT[part][:, kcol:kcol + 128]),
                                                              rhs=R(qT_ap), start=True, stop=(extra_qk is None)),
                     reads=[kTk[part], qkey], writes=[psk[pi]], nosync_self=True)
                if extra_qk is not None:
                    kr_ap, krkey, qr_ap, qrkey = extra_qk
                    S.op("pe", lambda e, t=t, kcol=kcol: e.matmul(ps[pi][:, t * 256:(t + 1) * 256],
                                                                  lhsT=R(kr_ap[0:64, part * 2048 + kcol:part * 2048 + kcol + 128]),
                                                                  rhs=R(qr_ap), start=False, stop=True),
                         reads=[krkey, qrkey], writes=[psk[pi]], nosync_self=True)
            pti = cnt["pt"] % 2
            cnt["pt"] += 1
            pt, ptk = ptl[pti], "pt%d" % pti
            m_ap = mask_fn(part, kb)
            if m_ap is None:
                S.op("act", lambda e: e.activation(out=R(pt[:]), in_=ps[pi][:, 0:512], func=AF.Exp, scale=scale),
                     reads=[psk[pi]], writes=[ptk])
                src, srck = pt, ptk
            else:
                S.op("act", lambda e: e.activation(out=R(pt[:]), in_=ps[pi][:, 0:512], func=AF.Exp, scale=scale),
                     reads=[psk[pi]], writes=[ptk])
                pm, pmk = ptm[pti], "ptm%d" % pti
                eng = "dve" if (idx % 2 == 0) else "pool"
                S.op(eng, lambda e: e.tensor_tensor(out=R(pm[:]), in0=pt[:], in1=m_ap, op=ALU.mult),
                     reads=[ptk, "mskT"], writes=[pmk])
                src, srck = pm, pmk
            last = idx == len(blocks) - 1
            for t in range(2):
                S.op("pe", lambda e, t=t: e.matmul(ps[6][:, 0:256], lhsT=R(vt[part][:, kb + t, :]), rhs=R(src[:, t * 256:(t + 1) * 256]),
                                                   start=(idx == 0 and t == 0), stop=(last and t == 1)),
                     reads=[vk[part], srck], writes=[psk[6]], nosync_self=True)
                S.op("pe", lambda e, t=t: e.matmul(ps[7][:, 0:256], lhsT=R(ones[:]), rhs=R(src[:, t * 256:(t + 1) * 256]),
                                                   start=(idx == 0 and t == 0), stop=(last and t == 1)),
                     reads=["ones", srck], writes=[psk[7]], nosync_self=True)
        S.op("dve", lambda e: e.reciprocal(out=rden[:], in_=ps[7][:, 0:256]), reads=[psk[7]], writes=["rden"])
        S.op("dve", lambda e: e.tensor_tensor(out=OT[:, h, :], in0=ps[6][:, 0:256], in1=rden[:], op=ALU.mult),
             reads=[psk[6], "rden"], writes=[okey])

    def out_norm(j, OT, okey, goff):
        q0 = j * 256
        for h in range(8):
            S.op("pool", lambda e, h=h: e.tensor_tensor(out=R(sq[:, h, :]), in0=OT[:, h, :], in1=OT[:, h, :], op=ALU.mult),
                 reads=[okey], writes=["sq"])
        for h in range(8):
            S.op("pe", lambda e, h=h: e.matmul(ps[5][:, 0:256], lhsT=R(ones[:]), rhs=R(sq[:, h, :]), start=(h == 0), stop=(h == 7)),
                 reads=["ones", "sq"], writes=[psk[5]], nosync_self=True)
        S.op("act", lambda e: e.activation(out=rden[:], in_=ps[5][:, 0:256], func=AF.Ln, bias=eps_t[:], scale=1.0 / 1024.0),
             reads=[psk[5], "eps"], writes=["rden"])
        S.op("act", lambda e: e.activation(out=rden[:], in_=rden[:], func=AF.Exp, scale=-0.5), reads=["rden"], writes=["rden"])
        for h in range(8):
            S.op("dve", lambda e, h=h: e.scalar_tensor_tensor(out=sq2[:, h, :], in0=OT[:, h, :], scalar=ong[:, goff + h:goff + h + 1],
                                                              in1=rden[:], op0=ALU.mult, op1=ALU.mult),
                 reads=[okey, "ong", "rden"], writes=["sq2"])
        S.dma("pool", MRG[:, goff:goff + 8, q0:q0 + 256], sq2[:], reads=["sq2"], writes=["MRG"])

    cnt["pt"] = 0
    push()
    IKT2 = T("IKT2", [128, SEQ])
    for tg in tglist:
        S.dma("sp", IKT2[:, tg * 512:(tg + 1) * 512], IKT_d[:, tg * 512:(tg + 1) * 512], writes=["IKT2"])
    ong = T("ong", [128, 16])
    S.dma("sp", ong[:], ong_fm, writes=["ong"])
    identb = T("identb", [128, 128], BF)
    S.op("dve", lambda e: e.tensor_copy(out=identb[:], in_=ident[:]), reads=["ident"], writes=["identb"])
    diagb = T("diagb", [128, 128]); rbias = T("rbias", [128, 128])
    S.dma("sp", diagb[:], diagb_d, writes=["diagb"])
    S.dma("sp", rbias[:], rbias_d, writes=["rbias"])
    Isc = T("Isc", [128, 4096])
    work = T("work", [128, 4096])
    msk = T("msk", [128, 4096], BF)
    mskT = T("mskT", [128, 2, 16, 256], BF)
    iqT = T("iqT", [128, 8, 256])
    aqT = T("aqT", [128, 8, 256])
    sgnq = T("sgnq", [128, 2, 16])
    tmpr = [T("tmpr%d" % i, [128, 512]) for i in range(2)]
    m8 = T("m8", [128, 8]); thr = T("thr", [128, 1])
    kT = [T("kT%d" % i, [128, 2048]) for i in range(2)]
    vt = [T("vt%d" % i, [128, 16, 128]) for i in range(2)]
    kTk = ["kT0", "kT1"]; vk = ["vt0", "vt1"]
    ptl = [T("pt%d" % i, [128, 512]) for i in range(2)]
    ptm = [T("ptm%d" % i, [128, 512]) for i in range(2)]
    OT = T("OT", [128, 8, 256])
    sq = T("sq", [128, 8, 256])
    sq2 = T("sq2", [128, 8, 256])
    rden = T("rden", [128, 256])
    npairs = int(os.environ.get("KDBG_NPAIR", 8))
    for j in range(npairs):
        q0 = j * 256
        NKB = 2 * j + 2
        S.dma("sp", iqT[:], IQT[:, :, q0:q0 + 256].rearrange("h p q -> p h q"), writes=["iqT"])
        S.dma("sp", R(aqT[:]), R(AQT[:, :, q0:q0 + 256]).rearrange("h p q -> p h q"), writes=["aqT"])
        S.dma("sp", sgnq[:], SGN_d[q0:q0 + 256, :].rearrange("(b p) h -> p b h", p=128), writes=["sgnq"])
        for part in range(2):
            S.op("pool", lambda e, part=part: e.memset(mskT[:, part, NKB - 1, 0:128], 0.0), writes=["mskT"])
        for qb in range(2):
            i = 2 * j + qb
            nk = i + 1
            for part in range(2):
                for c0 in range(0, nk * 128, 512):
                    cw = min(512, nk * 128 - c0)
                    for h in range(16):
                        pi = cnt["ps"] % 4
                        cnt["ps"] += 1
                        p0 = (h % 2) * 64
                        S.op("pe", lambda e, h=h, p0=p0: e.matmul(ps[pi][:, 0:cw], lhsT=iqT[p0:p0 + 64, h // 2, qb * 128:(qb + 1) * 128],
                                                                  rhs=IKT2[p0:p0 + 64, part * 2048 + c0:part * 2048 + c0 + cw],
                                                                  start=True, stop=True),
                             reads=["iqT", "IKT2"], writes=[psk[pi]], nosync_self=True)
                        ti = cnt["pt"] % 2
                        cnt["pt"] += 1
                        S.op("act", lambda e: e.activation(out=tmpr[ti][:, 0:cw], in_=ps[pi][:, 0:cw], func=AF.Relu),
                             reads=[psk[pi]], writes=["tmpr%d" % ti])
                        if h == 0:
                            S.op("dve", lambda e: e.tensor_scalar(out=Isc[:, part * nk * 128 + c0:part * nk * 128 + c0 + cw], in0=tmpr[ti][:, 0:cw],
                                                                  scalar1=sgnq[:, qb, 0:1], scalar2=None, op0=ALU.mult),
                                 reads=["tmpr%d" % ti, "sgnq"], writes=["Isc"])
                        else:
                            S.op("dve", lambda e, h=h: e.scalar_tensor_tensor(out=Isc[:, part * nk * 128 + c0:part * nk * 128 + c0 + cw], in0=tmpr[ti][:, 0:cw],
                                                                              scalar=sgnq[:, qb, h:h + 1], in1=Isc[:, part * nk * 128 + c0:part * nk * 128 + c0 + cw],
                                                                              op0=ALU.mult, op1=ALU.add),
                                 reads=["tmpr%d" % ti, "sgnq", "Isc"], writes=["Isc"])
            S.op("dve", lambda e: e.tensor_tensor(out=Isc[:, i * 128:(i + 1) * 128], in0=Isc[:, i * 128:(i + 1) * 128],
                                                  in1=diagb[:], op=ALU.add), reads=["Isc", "diagb"], writes=["Isc"])
            S.op("dve", lambda e: e.tensor_tensor(out=Isc[:, nk * 128 + i * 128:nk * 128 + (i + 1) * 128],
                                                  in0=Isc[:, nk * 128 + i * 128:nk * 128 + (i + 1) * 128],
                                                  in1=rbias[:], op=ALU.add), reads=["Isc", "rbias"], writes=["Isc"])
            Iv = Isc[:, 0:2 * nk * 128]
            Wv = work[:, 0:2 * nk * 128]
            if i == 0:
                S.op("dve", lambda e: e.memset(thr[:], -1.0e29), writes=["thr"])
            else:
                for it in range(32):
                    src = Iv if it == 0 else Wv
                    S.op("dve", lambda e: e.max(out=m8[:], in_=src), reads=["Isc", "work"], writes=["m8"])
                    if it < 31:
                        S.op("dve", lambda e: e.match_replace(out=Wv, in_to_replace=m8[:], in_values=src, imm_value=NEG),
                             reads=["Isc", "work", "m8"], writes=["work"])
                S.op("dve", lambda e: e.tensor_scalar(out=thr[:], in0=m8[:, 7:8], scalar1=-1.0e29, scalar2=None, op0=ALU.max),
                     reads=["m8"], writes=["thr"])
            S.op("dve", lambda e: e.tensor_scalar(out=msk[:, 0:2 * nk * 128], in0=Iv, scalar1=thr[:], scalar2=None, op0=ALU.is_ge),
                 reads=["Isc", "thr"], writes=["msk"])
            for part in range(2):
                for kb in range(nk):
                    pi = cnt["ps"] % 4
                    cnt["ps"] += 1
                    pb = ps[pi][:].bitcast(BF)
                    S.op("pe", lambda e: e.transpose(out=pb[:, 0:128], in_=msk[:, (part * nk + kb) * 128:(part * nk + kb + 1) * 128], identity=identb[:]),
                         reads=["msk", "identb"], writes=[psk[pi]])
                    S.op("act", lambda e: e.copy(out=mskT[:, part, kb, qb * 128:(qb + 1) * 128], in_=pb[:, 0:128]),
                         reads=[psk[pi]], writes=["mskT"])
        for h in range(8):
            for part in range(2):
                S.dma("sp", R(kT[part][:, 0:NKB * 128]), R(AKT[h, :, part * 2048:part * 2048 + NKB * 128]), writes=[kTk[part]])
                S.dma("sp", R(vt[part][:, 0:NKB, :]),
                      R(AV[part * 2048:part * 2048 + NKB * 128, h * 128:(h + 1) * 128]).rearrange("(kb p) d -> p kb d", p=128),
                      writes=[vk[part]])
            attn_core(j, h, kT, kTk, vt, vk, aqT[:, h, :], "aqT", SCALE_A, OT, "OT",
                      lambda part, kb: mskT[:, part, kb:kb + 2, :].rearrange("p a b -> p (a b)"))
        out_norm(j, OT, "OT", 0)
    pop()
    if stage <= 3:
        return nc, S, es

    push()
    KRT2 = T("KRT2", [64, SEQ])
    for tg in tglist:
        S.dma("sp", R(KRT2[:, tg * 512:(tg + 1) * 512]), R(KRT_d[:, tg * 512:(tg + 1) * 512]), writes=["KRT2"])
    ong = T("ong", [128, 16])
    S.dma("sp", ong[:], ong_fm, writes=["ong"])
    mlam = T("mlam", [128, 4, 256])
    S.dma("sp", mlam[:], mlam_d.rearrange("p (a b) -> p a b", a=4), writes=["mskT"])
    qbn = T("qbn", [128, 8, 256])
    qbr = T("qbr", [64, 8, 256])
    kT = [T("kT%d" % i, [128, 2048]) for i in range(2)]
    vt = [T("vt%d" % i, [128, 16, 128]) for i in range(2)]
    ptl = [T("pt%d" % i, [128, 512]) for i in range(2)]
    ptm = [T("ptm%d" % i, [128, 512]) for i in range(2)]
    OT = T("OT", [128, 8, 256])
    sq = T("sq", [128, 8, 256])
    sq2 = T("sq2", [128, 8, 256])
    rden = T("rden", [128, 256])
    for j in range(npairs):
        q0 = j * 256
        NKB = 2 * j + 2
        S.dma("sp", R(qbn[:]), R(QBN[:, :, q0:q0 + 256]).rearrange("h p q -> p h q"), writes=["qbn"])
        S.dma("sp", R(qbr[:]), R(QBR[:, :, q0:q0 + 256]).rearrange("h p q -> p h q"), writes=["qbr"])
        for h in range(8):
            for part in range(2):
                S.dma("sp", R(kT[part][:, 0:NKB * 128]), R(KBT[h, :, part * 2048:part * 2048 + NKB * 128]), writes=[kTk[part]])
                S.dma("sp", R(vt[part][:, 0:NKB, :]),
                      R(VB[part * 2048:part * 2048 + NKB * 128, h * 128:(h + 1) * 128]).rearrange("(kb p) d -> p kb d", p=128),
                      writes=[vk[part]])
            attn_core(j, h, kT, kTk, vt, vk, qbn[:, h, :], "qbn", SCALE_B, OT, "OT",
                      lambda part, kb, NKB=NKB: (mlam[:, part * 2:part * 2 + 2, :].rearrange("p a b -> p (a b)")
                                                 if kb == NKB - 2 else None),
                      extra_qk=(KRT2, "KRT2", qbr[:, h, :], "qbr"))
        out_norm(j, OT, "OT", 8)
    pop()
    if stage <= 4:
        return nc, S, es

    push()
    wbuf = [T("wbuf%d" % i, [128, 16, 256]) for i in range(3)]
    bc = {}
    for nm, src in [("g1", ada_d[:, 2 * D:3 * D]), ("sh2", ada_d[:, 3 * D:4 * D]), ("sc2", ada_d[:, 4 * D:5 * D]),
                    ("lnmg", lnmg), ("lnmb", lnmb)]:
        bc[nm] = T("bc_" + nm, [128, D])
        S.dma("sp", bc[nm][:], src.partition_broadcast(128), writes=["bc_" + nm])
    S.op("dve", lambda e: e.tensor_scalar(out=bc["sc2"][:], in0=bc["sc2"][:], scalar1=1.0, scalar2=None, op0=ALU.add),
         reads=["bc_sc2"], writes=["bc_sc2"])
    mT = T("mT", [128, 16, 256])
    ymix = [T("ymix%d" % i, [128, D]) for i in range(2)]
    xt = [T("xt%d" % i, [128, D]) for i in range(2)]
    h2b = T("h2b", [128, 16, 128])
    wr = T("wr", [128, 16, NEXP])
    S.dma("sp", R(wr[:]), w_router.rearrange("(kc p) e -> p kc e", p=128), writes=["wr"])
    brt = T("brt", [128, NEXP])
    S.dma("sp", brt[:], b_router.partition_broadcast(128), writes=["brt"])
    lg = T("lg", [128, NEXP]); ex = T("ex", [128, NEXP]); mk = T("mk", [128, NEXP])
    m8 = T("m8", [128, 8]); nmx = T("nmx", [128, 1]); rs = T("rs", [128, 1])
    gts = T("gts", [128, 128])
    st6 = T("st6", [128, 4, 6]); mv = T("mv", [128, 2]); rstd = T("rstd", [128, 1])
    w_out_v = w_out.rearrange("(kc p) f -> p kc f", p=128)
    for j in range(npairs):
        q0 = j * 256
        S.dma("sp", R(mT[:]), R(MRG[:, :, q0:q0 + 256]), writes=["mT"])
        for tb in range(2):
            S.dma("sp", xt[tb][:], xl[q0 + tb * 128:q0 + (tb + 1) * 128, :], writes=["xt%d" % tb])
        for cg in range(8):
            wt, wkey = load_w(w_out_v, cg * 256, 256, 16)
            for tb in range(2):
                pt_, pk = proj_block(mT, "mT", 16, wt, wkey, 256, tb)
                S.op("dve", lambda e: e.tensor_tensor(out=ymix[tb][:, cg * 256:(cg + 1) * 256], in0=pt_[:, 0:256],
                                                      in1=bc["g1"][:, cg * 256:(cg + 1) * 256], op=ALU.mult),
                     reads=[pk, "bc_g1"], writes=["ymix%d" % tb])
        for tb in range(2):
            blk = 2 * j + tb
            xb, xk = xt[tb], "xt%d" % tb
            ym, yk = ymix[tb], "ymix%d" % tb
            S.op("dve", lambda e: e.scalar_tensor_tensor(out=ym[:], in0=xb[:], scalar=ALPHA, in1=ym[:], op0=ALU.mult, op1=ALU.add),
                 reads=[xk, yk], writes=[yk])
            ln_stats2 = lambda src, key: _ln_stats(src, D, key)
            _ln_stats(ym, D, yk)
            S.op("dve", lambda e: e.tensor_scalar(out=ym[:], in0=ym[:], scalar1=mv[:, 0:1], scalar2=rstd[:],
                                                  op0=ALU.subtract, op1=ALU.mult), reads=[yk, "mv", "rstd"], writes=[yk])
            S.op("pool", lambda e: e.tensor_tensor(out=ym[:], in0=ym[:], in1=bc["lnmg"][:], op=ALU.mult),
                 reads=[yk, "bc_lnmg"], writes=[yk])
            S.op("pool", lambda e: e.tensor_tensor(out=ym[:], in0=ym[:], in1=bc["lnmb"][:], op=ALU.add),
                 reads=[yk, "bc_lnmb"], writes=[yk])
            S.dma("pool", X1[blk * 128:(blk + 1) * 128, :], ym[:], reads=[yk], writes=["X1"])
            _ln_stats(ym, D, yk)
            S.op("dve", lambda e: e.tensor_scalar(out=xb[:], in0=ym[:], scalar1=mv[:, 0:1], scalar2=rstd[:],
                                                  op0=ALU.subtract, op1=ALU.mult), reads=[yk, "mv", "rstd"], writes=[xk])
            S.op("pool", lambda e: e.tensor_tensor(out=xb[:], in0=xb[:], in1=bc["sc2"][:], op=ALU.mult),
                 reads=[xk, "bc_sc2"], writes=[xk])
            S.op("pool", lambda e: e.tensor_tensor(out=xb[:], in0=xb[:], in1=bc["sh2"][:], op=ALU.add),
                 reads=[xk, "bc_sh2"], writes=[xk])
            for kc in range(16):
                transpose_to(h2b[:, kc, :], "h2b", xb[:, kc * 128:(kc + 1) * 128], xk, 128)
            S.dma("pool", H2T[:, :, blk * 128:(blk + 1) * 128], h2b[:], reads=["h2b"], writes=["H2T"])
            pi = cnt["ps"] % 4
            cnt["ps"] += 1
            for kc in range(16):
                S.op("pe", lambda e, kc=kc: e.matmul(ps[pi][:, 0:NEXP], lhsT=R(h2b[:, kc, :]), rhs=R(wr[:, kc, :]),
                                                     start=(kc == 0), stop=(kc == 15)),
                     reads=["h2b", "wr"], writes=[psk[pi]], nosync_self=True)
            S.op("dve", lambda e: e.tensor_tensor(out=lg[:], in0=ps[pi][:, 0:NEXP], in1=brt[:], op=ALU.add),
                 reads=[psk[pi], "brt"], writes=["lg"])
            S.op("dve", lambda e: e.max(out=m8[:], in_=lg[:]), reads=["lg"], writes=["m8"])
            S.op("dve", lambda e: e.tensor_scalar(out=nmx[:], in0=m8[:, 0:1], scalar1=-1.0, scalar2=None, op0=ALU.mult),
                 reads=["m8"], writes=["nmx"])
            S.op("act", lambda e: e.activation(out=ex[:], in_=lg[:], func=AF.Exp, bias=nmx[:], scale=1.0),
                 reads=["lg", "nmx"], writes=["ex"])
            S.op("dve", lambda e: e.tensor_scalar(out=mk[:], in0=lg[:], scalar1=m8[:, 3:4], scalar2=None, op0=ALU.is_ge),
                 reads=["lg", "m8"], writes=["mk"])
            S.op("dve", lambda e: e.tensor_tensor(out=ex[:], in0=ex[:], in1=mk[:], op=ALU.mult), reads=["ex", "mk"], writes=["ex"])
            S.op("dve", lambda e: e.reduce_sum(out=rs[:], in_=ex[:], axis=AX.X), reads=["ex"], writes=["rs"])
            S.op("dve", lambda e: e.reciprocal(out=rs[:], in_=rs[:]), reads=["rs"], writes=["rs"])
            S.op("dve", lambda e: e.tensor_scalar(out=ex[:], in0=ex[:], scalar1=rs[:], scalar2=None, op0=ALU.mult),
                 reads=["ex", "rs"], writes=["ex"])
            transpose_to(gts[0:NEXP, :], "gts", ex[:], "ex", NEXP)
            S.dma("pool", GT_d[:, blk * 128:(blk + 1) * 128], gts[0:NEXP, :], reads=["gts"], writes=["GT_d"])
    pop()
    if stage <= 5:
        return nc, S, es

    push()
    wbuf = [T("wbuf%d" % i, [128, 16, 256]) for i in range(2)]
    TS = 512
    h2T = T("h2T", [128, 16, TS])
    accT = T("accT", [128, 16, TS])
    actT = T("actT", [128, 16, TS])
    gbc = [T("gbc%d" % i, [128, TS]) for i in range(2)]
    gt = T("gt", [NEXP, TS])
    bdn = T("bdn", [NEXP, D])
    S.dma("sp", R(bdn[:]), b_dn, writes=["bdn"])
    bgu = T("bgu", [128, NEXP, 16, 2])
    S.dma("sp", bgu[:], bgu_fm.rearrange("p (e j t) -> p e j t", e=NEXP, j=16), writes=["bgu"])
    g2b = T("g2b", [128, D])
    S.dma("sp", g2b[:], ada_d[:, 5 * D:6 * D].partition_broadcast(128), writes=["g2b"])
    tg_ = [T("tg%d" % i, [128, TS]) for i in range(2)]
    tsg = [T("tsg%d" % i, [128, TS]) for i in range(2)]
    tu = [T("tu%d" % i, [128, TS]) for i in range(2)]
    st6 = T("st6", [128, 4, 6]); mv = T("mv", [128, 2]); rstd = T("rstd", [128, 1])
    yf_t = T("yf", [128, D]); x1t_t = T("x1t", [128, D]); lgb_t = T("lgb", [128, D]); lbb_t = T("lbb", [128, D])
    yf = yf_t[:]; x1t = x1t_t[:]; lgb = lgb_t[:]; lbb = lbb_t[:]
    nsp = int(os.environ.get("KDBG_NSP", 2048 // TS))
    nexp = int(os.environ.get("KDBG_NEXP", NEXP))
    for sp in range(nsp):
        t0 = sp * TS
        S.dma("sp", R(h2T[:]), R(H2T[:, :, t0:t0 + TS]), writes=["h2T"])
        S.dma("sp", R(gt[:]), R(GT_d[:, t0:t0 + TS]), writes=["gt"])
        for dc in range(16):
            pi = cnt["ps"] % 4
            cnt["ps"] += 1
            S.op("pe", lambda e: e.matmul(ps[pi][:, 0:TS], lhsT=R(bdn[:, dc * 128:(dc + 1) * 128]), rhs=R(gt[:]), start=True, stop=True),
                 reads=["bdn", "gt"], writes=[psk[pi]], nosync_self=True)
            S.op("act", lambda e: e.copy(out=accT[:, dc, :], in_=ps[pi][:, 0:TS]), reads=[psk[pi]], writes=["accT"])
        for ex_ in range(nexp):
            gb, gbk = gbc[ex_ % 2], "gbc%d" % (ex_ % 2)
            S.dma("sp", gb[:], GT_d[ex_:ex_ + 1, t0:t0 + TS].partition_broadcast(128), writes=[gbk])
            wgu_v = w_gu[ex_].rearrange("(kc p) f -> p kc f", p=128)
            wdn_v = w_dn[ex_].rearrange("(kc p) f -> p kc f", p=128)
            for jf in range(16):
                wt, wkey = load_w(wgu_v, jf * 256, 256, 16)
                pg = cnt["ps"] % 4; cnt["ps"] += 1
                pu = cnt["ps"] % 4; cnt["ps"] += 1
                for kc in range(16):
                    S.op("pe", lambda e, kc=kc: e.matmul(ps[pg][:, 0:TS], lhsT=R(wt[:, kc, 0:256:2]), rhs=R(h2T[:, kc, :]),
                                                         start=(kc == 0), stop=(kc == 15)),
                         reads=[wkey, "h2T"], writes=[psk[pg]], nosync_self=True)
                for kc in range(16):
                    S.op("pe", lambda e, kc=kc: e.matmul(ps[pu][:, 0:TS], lhsT=R(wt[:, kc, 1:256:2]), rhs=R(h2T[:, kc, :]),
                                                         start=(kc == 0), stop=(kc == 15)),
                         reads=[wkey, "h2T"], writes=[psk[pu]], nosync_self=True)
                a = jf % 2
                S.op("dve", lambda e: e.tensor_scalar(out=tg_[a][:], in0=ps[pg][:, 0:TS], scalar1=bgu[:, ex_, jf, 0:1], scalar2=7.0,
                                                      op0=ALU.add, op1=ALU.min), reads=[psk[pg], "bgu"], writes=["tg%d" % a])
                S.op("act", lambda e: e.activation(out=tsg[a][:], in_=tg_[a][:], func=AF.Sigmoid, scale=1.702),
                     reads=["tg%d" % a], writes=["tsg%d" % a])
                S.op("dve", lambda e: e.tensor_scalar(out=tu[a][:], in0=ps[pu][:, 0:TS], scalar1=bgu[:, ex_, jf, 1:2], scalar2=7.0,
                                                      op0=ALU.add, op1=ALU.min), reads=[psk[pu], "bgu"], writes=["tu%d" % a])
                S.op("pool", lambda e: e.tensor_scalar(out=tu[a][:], in0=tu[a][:], scalar1=-7.0, scalar2=1.0,
                                                       op0=ALU.max, op1=ALU.add), reads=["tu%d" % a], writes=["tu%d" % a])
                S.op("pool", lambda e: e.tensor_tensor(out=tg_[a][:], in0=tg_[a][:], in1=tsg[a][:], op=ALU.mult),
                     reads=["tg%d" % a, "tsg%d" % a], writes=["tg%d" % a])
                S.op("dve", lambda e: e.tensor_tensor(out=tg_[a][:], in0=tg_[a][:], in1=tu[a][:], op=ALU.mult),
                     reads=["tg%d" % a, "tu%d" % a], writes=["tg%d" % a])
                S.op("pool", lambda e: e.tensor_tensor(out=R(actT[:, jf, :]), in0=tg_[a][:], in1=gb[:], op=ALU.mult),
                     reads=["tg%d" % a, gbk], writes=["actT"])
            for dg in range(8):
                wt, wkey = load_w(wdn_v, dg * 256, 256, 16)
                for dd in range(2):
                    dc = dg * 2 + dd
                    pi = cnt["ps"] % 4
                    cnt["ps"] += 1
                    for fc in range(16):
                        S.op("pe", lambda e, fc=fc: e.matmul(ps[pi][:, 0:TS], lhsT=R(wt[:, fc, dd * 128:(dd + 1) * 128]),
                                                             rhs=R(actT[:, fc, :]), start=(fc == 0), stop=(fc == 15)),
                             reads=[wkey, "actT"], writes=[psk[pi]], nosync_self=True)
                    S.op("dve", lambda e: e.tensor_tensor(out=accT[:, dc, :], in0=ps[pi][:, 0:TS], in1=accT[:, dc, :], op=ALU.add),
                         reads=[psk[pi], "accT"], writes=["accT"])
        S.dma("sp", lgb, lnfg.partition_broadcast(128), reads=["actT"], writes=["actT"])
        S.dma("sp", lbb, lnfb.partition_broadcast(128), reads=["actT"], writes=["actT"])
        for tb in range(TS // 128):
            blk = sp * (TS // 128) + tb
            S.dma("sp", x1t, X1[blk * 128:(blk + 1) * 128, :], reads=["actT"], writes=["actT"])
            for dc in range(16):
                i = cnt["ps"] % 4 + 4
                cnt["ps"] += 1
                S.op("pe", lambda e: e.transpose(out=ps[i][:, 0:128], in_=accT[:, dc, tb * 128:(tb + 1) * 128], identity=ident[:]),
                     reads=["accT", "ident"], writes=[psk[i]])
                S.op("act", lambda e: e.copy(out=yf[:, dc * 128:(dc + 1) * 128], in_=ps[i][:, 0:128]), reads=[psk[i]], writes=["actT"])
            S.op("dve", lambda e: e.tensor_tensor(out=yf, in0=yf, in1=g2b[:], op=ALU.mult), reads=["actT", "g2b"], writes=["actT"])
            S.op("dve", lambda e: e.scalar_tensor_tensor(out=yf, in0=x1t, scalar=ALPHA, in1=yf, op0=ALU.mult, op1=ALU.add),
                 reads=["actT"], writes=["actT"])
            _ln_stats(yf, D, "actT")
            S.op("dve", lambda e: e.tensor_scalar(out=yf, in0=yf, scalar1=mv[:, 0:1], scalar2=rstd[:],
                                                  op0=ALU.subtract, op1=ALU.mult), reads=["actT", "mv", "rstd"], writes=["actT"])
            S.op("dve", lambda e: e.tensor_tensor(out=yf, in0=yf, in1=lgb, op=ALU.mult), reads=["actT"], writes=["actT"])
            S.op("dve", lambda e: e.tensor_tensor(out=yf, in0=yf, in1=lbb, op=ALU.add), reads=["actT"], writes=["actT"])
            S.dma("pool", out_d[blk * 128:(blk + 1) * 128, :], yf, reads=["actT"], writes=["out"])
    pop()
    return nc, S, es


def finish(nc, S, out_written=True):
    S.barrier()


_INVF = np.concatenate([
    (THETA ** (-np.arange(16, dtype=np.float32) / np.float32(16))).astype(np.float32),
    (THETA ** (-np.arange(8, dtype=np.float32) / np.float32(8))).astype(np.float32),
    (THETA ** (-np.arange(32, dtype=np.float32) / np.float32(32))).astype(np.float32),
]).astype(np.float32)[None, :]


def make_in_maps(inp):
    f = lambda a: np.ascontiguousarray(np.asarray(a, dtype=np.float32))
    x = f(inp["x"]); c = f(inp["c"]); pos = np.asarray(inp["positions"]).astype(np.int32)
    shared = dict(
        w_ada=f(inp["w_ada"][0]), b_ada=f(inp["b_ada"][0])[None, :], w_in=f(inp["w_in"][0]),
        ikg=f(inp["idx_k_norm_g"][0])[None, :], ikb=f(inp["idx_k_norm_b"][0])[None, :],
        qng=f(inp["q_norm_g"][0])[None, :], w_q_up=f(inp["w_q_up"][0]),
        kvng=f(inp["kv_norm_g"][0])[None, :], w_kv_up=f(inp["w_kv_up"][0]),
        ong_fm=f(np.concatenate([np.asarray(inp["out_norm_a_g"][0]).reshape(8, 128),
                                 np.asarray(inp["out_norm_b_g"][0]).reshape(8, 128)], 0).T),
        w_out=f(inp["w_out"][0]), lnmg=f(inp["ln_mix_g"][0])[None, :], lnmb=f(inp["ln_mix_b"][0])[None, :],
        w_router=f(inp["w_router"][0]), b_router=f(inp["b_router"][0])[None, :],
        w_gu=f(inp["w_gate_up"][0]),
        bgu_fm=f(np.asarray(inp["b_gate_up"][0]).reshape(NEXP, 16, 128, 2).transpose(2, 0, 1, 3).reshape(128, NEXP * 32)),
        w_dn=f(inp["w_down"][0]), b_dn=f(inp["b_down"][0]),
        lnfg=f(inp["ln_ffn_g"][0])[None, :], lnfb=f(inp["ln_ffn_b"][0])[None, :],
        ident=np.eye(128, dtype=np.float32), ones=np.ones((128, 128), np.float32),
        invf=_INVF,
    )
    esel = np.zeros((NEXP, NEXP, 128), np.float32)
    for e in range(NEXP):
        esel[e, e, :] = 1.0
    shared["esel"] = esel.reshape(NEXP, NEXP * 128)
    qi = np.arange(128)[:, None] // 64
    ki = np.arange(128)[None, :] // 64
    diag_ok = (ki <= qi)
    shared["diagb"] = np.where(diag_ok, 0.0, NEG).astype(np.float32)
    maps = []
    for core in range(8):
        b, r = core // 2, core % 2
        own_blocks = [2 * i + r for i in range(NOWN)]
        oth_blocks = [2 * i + 1 - r for i in range(NOWN)]
        order = own_blocks + oth_blocks
        xb = x[b].reshape(NB, 128, D)[order].reshape(SEQ, D)
        pb = pos[b].reshape(NB, 128)[order]
        m = dict(shared)
        m["xl"] = np.ascontiguousarray(xb)
        m["c_fm"] = np.ascontiguousarray(c[b].reshape(16, 128).T)
        m["pos_i"] = np.ascontiguousarray(pb.T.astype(np.int32))
        m["rbias"] = np.full((128, 128), 0.0 if r == 1 else NEG, np.float32)
        dT = diag_ok.T.astype(np.float32)
        one = np.ones((128, 128), np.float32); zero = np.zeros((128, 128), np.float32)
        rr = one * float(r)
        mm = np.stack([np.concatenate([dT, one], 1), np.concatenate([zero, dT], 1),
                       np.concatenate([rr, one], 1), np.concatenate([zero, rr], 1)], 1)
        m["mlam"] = np.ascontiguousarray(mm.reshape(128, 1024).astype(np.float32))
        maps.append(m)
    return maps


def kernel(**inputs):
    nc, S, es = build()
    finish(nc, S)
    maps = make_in_maps(inputs)
    res = run_bass_kernel_spmd(nc, maps, core_ids=list(range(8)))
    out = np.zeros((4, SEQ, D), np.float32)
    for core in range(8):
        b, r = core // 2, core % 2
        o = np.asarray(res.results[core]["out"]).reshape(NOWN, 128, D)
        ov = out[b].reshape(NB, 128, D)
        for i in range(NOWN):
            ov[2 * i + r] = o[i]
    return out
```

```python
import os
from contextlib import ExitStack
import numpy as np
import concourse.bass as bass
import concourse.mybir as mybir
from concourse.bass_utils import run_bass_kernel_spmd

F32 = mybir.dt.float32
F32R = mybir.dt.float32r
BF16 = mybir.dt.bfloat16
I32 = mybir.dt.int32
AF = mybir.ActivationFunctionType
ALU = mybir.AluOpType
AX = mybir.AxisListType

D = 2048
SEQ = 4096
NB = 32
NOWN = 16
EPS = 1e-5
ALPHA = 2.0 ** 0.25
THETA = 500000.0
NEXP = 32
NEG = -1.0e30
IN_W = 5008
TWO_PI = 2.0 * np.pi
C1 = 6.28125
C2 = TWO_PI - C1


def R(ap):
    return ap.bitcast(F32R)


class Sched:
    def __init__(self, nc, es, ndma=28):
        self.nc = nc
        self.E = {}
        for name, eng in [("pe", nc.tensor), ("act", nc.scalar), ("dve", nc.vector),
                          ("pool", nc.gpsimd), ("sp", nc.sync)]:
            sem = es.enter_context(nc.semaphore("sem_" + name))
            self.E[name] = dict(eng=eng, sem=sem, cnt=0, waited={})
        self.dsem = [es.enter_context(nc.semaphore("dsem%d" % i)) for i in range(ndma)]
        self.dcnt = [0] * ndma
        self.dpool = {"sp": list(range(0, 16)), "act": list(range(16, ndma))}
        self.drr = {"sp": 0, "act": 0}
        self.lw = {}
        self.rd = {}
        self.ninst = 0

    def semh(self, sid):
        if isinstance(sid, tuple):
            return self.dsem[sid[1]]
        return self.E[sid]["sem"]

    def _wait(self, en, toks):
        E = self.E[en]
        need = {}
        for t in toks:
            if t is None:
                continue
            sid, v = t
            if E["waited"].get(sid, 0) < v:
                need[sid] = max(need.get(sid, 0), v)
        for sid, v in need.items():
            E["eng"].wait_ge(self.semh(sid), v)
            E["waited"][sid] = v
            self.ninst += 1

    def _deps(self, reads, writes):
        toks = []
        for k in reads:
            toks.append(self.lw.get(k))
            if isinstance(k, str) and k.startswith("ps"):
                toks += list(self.rd.get(k, {}).items())
        for k in writes:
            toks.append(self.lw.get(k))
            toks += list(self.rd.get(k, {}).items())
        return toks

    def _record(self, tok, reads, writes):
        for k in reads:
            d = self.rd.setdefault(k, {})
            d[tok[0]] = max(d.get(tok[0], 0), tok[1])
        for k in writes:
            self.lw[k] = tok
            self.rd[k] = {}

    def op(self, en, fn, reads=(), writes=(), nosync_self=False):
        toks = self._deps(reads, writes)
        if nosync_self:
            toks = [t for t in toks if t is not None and t[0] != en]
        self._wait(en, toks)
        E = self.E[en]
        E["cnt"] += 1
        ins = fn(E["eng"])
        ins.then_inc(E["sem"], 1)
        self.ninst += 1
        tok = (en, E["cnt"])
        E["waited"][en] = max(E["waited"].get(en, 0), 0)
        self._record(tok, reads, writes)
        return tok

    def dma(self, qn, out, in_, reads=(), writes=()):
        if qn == "pool":
            qn = "act"
        pl = self.dpool[qn]
        i = pl[self.drr[qn] % len(pl)]
        self.drr[qn] += 1
        toks = self._deps(reads, writes)
        if self.dcnt[i] > 0:
            toks.append((("d", i), 16 * self.dcnt[i]))
        self._wait(qn, toks)
        self.dcnt[i] += 1
        self.E[qn]["eng"].dma_start(out=out, in_=in_).then_inc(self.dsem[i], 16)
        self.ninst += 1
        tok = (("d", i), 16 * self.dcnt[i])
        self._record(tok, reads, writes)
        return tok

    def barrier(self):
        toks = [(n, e["cnt"]) for n, e in self.E.items() if e["cnt"] > 0]
        toks += [(("d", i), 16 * c) for i, c in enumerate(self.dcnt) if c > 0]
        for en in self.E:
            self._wait(en, toks)
        self.lw = {}
        self.rd = {}


def build(stage=99, debug=False):
    nc = bass.Bass("TRN2", target_bir_lowering=False)
    nc.dge_precook = False
    es = ExitStack()
    S = Sched(nc, es)
    dbg_kind = "ExternalOutput" if debug else "Internal"

    def dram_in(name, shape, dt=F32):
        return nc.dram_tensor(name, list(shape), dt, kind="ExternalInput").ap()

    def dram_scr(name, shape, dt=F32):
        return nc.dram_tensor(name, list(shape), dt, kind=dbg_kind).ap()

    xl = dram_in("xl", [SEQ, D])
    c_fm = dram_in("c_fm", [128, 16])
    pos_i = dram_in("pos_i", [128, NB], I32)
    invf = dram_in("invf", [1, 56])
    w_ada = dram_in("w_ada", [D, 6 * D], F32R)
    b_ada = dram_in("b_ada", [1, 6 * D])
    w_in = dram_in("w_in", [D, IN_W], F32R)
    ikg = dram_in("ikg", [1, 64])
    ikb = dram_in("ikb", [1, 64])
    qng = dram_in("qng", [1, 512])
    w_q_up = dram_in("w_q_up", [512, 1536], F32R)
    kvng = dram_in("kvng", [1, 256])
    w_kv_up = dram_in("w_kv_up", [256, 2048], F32R)
    ong_fm = dram_in("ong_fm", [128, 16])
    w_out = dram_in("w_out", [D, D], F32R)
    lnmg = dram_in("lnmg", [1, D])
    lnmb = dram_in("lnmb", [1, D])
    w_router = dram_in("w_router", [D, NEXP], F32R)
    b_router = dram_in("b_router", [1, NEXP])
    big = stage >= 6
    w_gu = dram_in("w_gu", [NEXP, D, 2 * D], F32R) if big else None
    bgu_fm = dram_in("bgu_fm", [128, NEXP * 32])
    w_dn = dram_in("w_dn", [NEXP, D, D], F32R) if big else None
    b_dn = dram_in("b_dn", [NEXP, D], F32R)
    lnfg = dram_in("lnfg", [1, D])
    lnfb = dram_in("lnfb", [1, D])
    ident_d = dram_in("ident", [128, 128])
    ones_d = dram_in("ones", [128, 128], F32R)
    esel_d = dram_in("esel", [NEXP, NEXP * 128], F32R)
    diagb_d = dram_in("diagb", [128, 128])
    rbias_d = dram_in("rbias", [128, 128])
    mlam_d = dram_in("mlam", [128, 4 * 256])
    out_d = nc.dram_tensor("out", [NOWN * 128, D], F32, kind="ExternalOutput").ap()

    ada_d = dram_scr("ada_d", [1, 6 * D])
    AKT = dram_scr("AKT", [8, 128, SEQ])
    AV = dram_scr("AV", [SEQ, 1024])
    KBT = dram_scr("KBT", [8, 128, SEQ])
    VB = dram_scr("VB", [SEQ, 1024])
    AQT = dram_scr("AQT", [8, 128, 2048])
    IQT = dram_scr("IQT", [8, 128, 2048])
    QBN = dram_scr("QBN", [8, 128, 2048])
    QBR = dram_scr("QBR", [8, 64, 2048])
    IKT_d = dram_scr("IKT_d", [128, SEQ])
    KRT_d = dram_scr("KRT_d", [64, SEQ])
    SGN_d = dram_scr("SGN_d", [2048, 16])
    X1 = dram_scr("X1", [2048, D])
    H2T = dram_scr("H2T", [128, 16, 2048])
    GT_d = dram_scr("GT_d", [NEXP, 2048])
    MRG = dram_scr("MRG", [128, 16, 2048])

    scopes = [es]

    used_names = {}

    def T(name, shape, dt=F32):
        n = used_names.get(name, 0)
        used_names[name] = n + 1
        nm = "sb_" + name + ("" if n == 0 else "_v%d" % n)
        return scopes[-1].enter_context(nc.sbuf_tensor(nm, list(shape), dt))

    def push():
        scopes.append(ExitStack())

    def pop():
        S.barrier()
        scopes.pop().close()

    def P(name, shape, dt=F32):
        return es.enter_context(nc.psum_tensor("pp_" + name, list(shape), dt))

    ident = T("ident", [128, 128])
    ones = T("ones", [128, 128])
    S.dma("sp", ident[:], ident_d, writes=["ident"])
    S.dma("sp", R(ones[:]), ones_d, writes=["ones"])
    ps = [P("ps%d" % i, [128, 512]) for i in range(8)]
    psk = ["ps%d" % i for i in range(8)]
    eps_t = T("eps_t", [128, 1])
    S.op("dve", lambda e: e.memset(eps_t[:], EPS), writes=["eps"])

    push()
    wbuf = [T("wbuf%d" % i, [128, 16, 256]) for i in range(3)]
    wk = ["wbuf%d" % i for i in range(3)]
    push()
    cfm = T("cfm", [128, 16])
    scr = T("scr", [128, 16, 128])
    S.dma("sp", cfm[:], c_fm, writes=["cfm"])
    S.op("act", lambda e: e.activation(out=cfm[:], in_=cfm[:], func=AF.Silu), reads=["cfm"], writes=["cfm"])
    S.op("dve", lambda e: e.tensor_copy(out=R(scr[:]), in_=cfm[:].unsqueeze(2).to_broadcast([128, 16, 128])),
         reads=["cfm"], writes=["scr"])
    bada = [T("bada%d" % i, [128, 256]) for i in range(2)]
    w_ada_v = w_ada.rearrange("(kc p) f -> p kc f", p=128)
    for g in range(48):
        wb, wkk = wbuf[g % 3], wk[g % 3]
        bt, bk = bada[g % 2], "bada%d" % (g % 2)
        S.dma("sp", R(wb[:]), w_ada_v[:, :, g * 256:(g + 1) * 256], writes=[wkk])
        S.dma("sp", bt[:], b_ada[:, g * 256:(g + 1) * 256].partition_broadcast(128), writes=[bk])
        pst, pk = ps[g % 2], psk[g % 2]
        for kc in range(16):
            S.op("pe", lambda e, kc=kc: e.matmul(pst[:, 0:256], lhsT=R(scr[:, kc, :]), rhs=R(wb[:, kc, :]),
                                                 start=(kc == 0), stop=(kc == 15)),
                 reads=["scr", wkk], writes=[pk], nosync_self=True)
        S.op("dve", lambda e: e.tensor_tensor(out=bt[:], in0=pst[:, 0:256], in1=bt[:], op=ALU.add),
             reads=[pk, bk], writes=[bk])
        S.dma("pool", ada_d[:, g * 256:(g + 1) * 256], bt[0:1, :], reads=[bk], writes=["ada_d"])
    pop()
    if stage <= 1:
        return nc, S, es

    SIN = T("SIN", [128, NB, 56])
    COS = T("COS", [128, NB, 56])
    push()
    posi = T("posi", [128, NB], I32)
    posf = T("posf", [128, NB])
    invb = T("invb", [128, 56])
    ang = T("ang", [128, NB, 56])
    tmpa = T("tmpa", [128, NB, 56])
    tmpi = T("tmpi", [128, NB, 56], I32)
    tmpb = T("tmpb", [128, NB, 56])
    S.dma("sp", posi[:], pos_i, writes=["posi"])
    S.dma("sp", invb[:], invf.partition_broadcast(128), writes=["invb"])
    S.op("dve", lambda e: e.tensor_copy(out=posf[:], in_=posi[:]), reads=["posi"], writes=["posf"])
    for blk in range(NB):
        S.op("dve", lambda e, blk=blk: e.tensor_scalar(out=ang[:, blk, :], in0=invb[:], scalar1=posf[:, blk:blk + 1],
                                                       scalar2=None, op0=ALU.mult),
             reads=["posf", "invb"], writes=["ang"])

    def sin_table(dst, dkey, shift):
        S.op("dve", lambda e: e.tensor_scalar(out=tmpa[:], in0=ang[:], scalar1=1.0 / TWO_PI,
                                              scalar2=shift / TWO_PI + 0.5, op0=ALU.mult, op1=ALU.add),
             reads=["ang"], writes=["tmpa"])
        S.op("dve", lambda e: e.tensor_copy(out=tmpi[:], in_=tmpa[:]), reads=["tmpa"], writes=["tmpi"])
        S.op("dve", lambda e: e.tensor_copy(out=tmpa[:], in_=tmpi[:]), reads=["tmpi"], writes=["tmpa"])
        S.op("dve", lambda e: e.scalar_tensor_tensor(out=tmpb[:], in0=tmpa[:], scalar=-C1, in1=ang[:],
                                                     op0=ALU.mult, op1=ALU.add),
             reads=["tmpa", "ang"], writes=["tmpb"])
        S.op("dve", lambda e: e.scalar_tensor_tensor(out=tmpb[:], in0=tmpa[:], scalar=-C2, in1=tmpb[:],
                                                     op0=ALU.mult, op1=ALU.add),
             reads=["tmpa", "tmpb"], writes=["tmpb"])
        if shift != 0.0:
            S.op("dve", lambda e: e.tensor_scalar(out=tmpb[:], in0=tmpb[:], scalar1=shift, scalar2=None, op0=ALU.add),
                 reads=["tmpb"], writes=["tmpb"])
        S.op("dve", lambda e: e.tensor_scalar(out=tmpa[:], in0=tmpb[:], scalar1=np.pi, scalar2=-TWO_PI,
                                              op0=ALU.is_gt, op1=ALU.mult), reads=["tmpb"], writes=["tmpa"])
        S.op("dve", lambda e: e.tensor_tensor(out=tmpb[:], in0=tmpb[:], in1=tmpa[:], op=ALU.add),
             reads=["tmpa", "tmpb"], writes=["tmpb"])
        S.op("dve", lambda e: e.tensor_scalar(out=tmpa[:], in0=tmpb[:], scalar1=-np.pi, scalar2=TWO_PI,
                                              op0=ALU.is_lt, op1=ALU.mult), reads=["tmpb"], writes=["tmpa"])
        S.op("dve", lambda e: e.tensor_tensor(out=tmpb[:], in0=tmpb[:], in1=tmpa[:], op=ALU.add),
             reads=["tmpa", "tmpb"], writes=["tmpb"])
        S.op("dve", lambda e: e.tensor_scalar(out=tmpb[:], in0=tmpb[:], scalar1=3.1415925, scalar2=-3.1415925,
                                              op0=ALU.min, op1=ALU.max), reads=["tmpb"], writes=["tmpb"])
        S.op("act", lambda e: e.activation(out=dst[:], in_=tmpb[:], func=AF.Sin), reads=["tmpb"], writes=[dkey])

    sin_table(SIN, "SIN", 0.0)
    sin_table(COS, "COS", np.pi / 2)
    pop()
    FA, FI, FM = (0, 16), (16, 8), (24, 32)

    G = 4
    sh1 = T("sh1", [128, D])
    sc1 = T("sc1", [128, D])
    S.dma("sp", sh1[:], ada_d[:, 0:D].partition_broadcast(128), writes=["sh1"])
    S.dma("sp", sc1[:], ada_d[:, D:2 * D].partition_broadcast(128), writes=["sc1"])
    S.op("dve", lambda e: e.tensor_scalar(out=sc1[:], in0=sc1[:], scalar1=1.0, scalar2=None, op0=ALU.add),
         reads=["sc1"], writes=["sc1"])
    ikg_b = T("ikg_b", [128, 64]); ikb_b = T("ikb_b", [128, 64])
    qng_b = T("qng_b", [128, 512]); kvng_b = T("kvng_b", [128, 256])
    S.dma("sp", ikg_b[:], ikg.partition_broadcast(128), writes=["ikg_b"])
    S.dma("sp", ikb_b[:], ikb.partition_broadcast(128), writes=["ikb_b"])
    S.dma("sp", qng_b[:], qng.partition_broadcast(128), writes=["qng_b"])
    S.dma("sp", kvng_b[:], kvng.partition_broadcast(128), writes=["kvng_b"])

    xt = [T("xt%d" % i, [128, D]) for i in range(2)]
    hT = T("hT", [128, 16, G * 128])
    stg = [T("stg%d" % i, [128, 512]) for i in range(2)]
    stT = [T("stT%d" % i, [128, 4, G * 128]) for i in range(2)]
    qdst = T("qdst", [128, G, 512])
    cqT = T("cqT", [128, 4, G * 128])
    ckvT = T("ckvT", [128, 2, G * 128])
    absw = T("absw", [128, G, 16])
    sgn = T("sgn", [128, G, 16])
    st6 = T("st6", [128, 4, 6]); mv = T("mv", [128, 2]); rstd = T("rstd", [128, 1])
    sm = [T("sm%d" % i, [128, 4, 32]) for i in range(4)]
    IKT = T("IKT", [128, SEQ])
    KRT = T("KRT", [64, SEQ])
    ikst = T("ikst", [128, 128])
    w_in_v = w_in.rearrange("(kc p) f -> p kc f", p=128)
    wq_v = w_q_up.rearrange("(kc p) f -> p kc f", p=128)
    wkv_v = w_kv_up.rearrange("(kc p) f -> p kc f", p=128)
    cnt = dict(w=0, ps=0, stg=0, stT=0, x=0)

    def ln_stats(src_ap, n, skey):
        return _ln_stats(src_ap, n, skey)

    def _ln_stats(src_ap, n, skey):
        nch = max(1, n // 512)
        w_ = n // nch
        for c in range(nch):
            S.op("dve", lambda e, c=c: e.bn_stats(out=st6[:, c, :], in_=src_ap[:, c * w_:(c + 1) * w_]),
                 reads=[skey], writes=["st6"])
        S.op("dve", lambda e: e.bn_aggr(out=mv[:], in_=st6[:, 0:nch, :]), reads=["st6"], writes=["mv"])
        S.op("act", lambda e: e.activation(out=rstd[:], in_=mv[:, 1:2], func=AF.Ln, bias=eps_t[:], scale=1.0),
             reads=["mv", "eps"], writes=["rstd"])
        S.op("act", lambda e: e.activation(out=rstd[:], in_=rstd[:], func=AF.Exp, scale=-0.5),
             reads=["rstd"], writes=["rstd"])

    def rms_rstd(src_ap, n, skey, junk_ap, jkey):
        S.op("act", lambda e: e.activation(out=junk_ap, in_=src_ap, func=AF.Square, accum_out=mv[:, 0:1]),
             reads=[skey], writes=[jkey, "mv"])
        S.op("act", lambda e: e.activation(out=rstd[:], in_=mv[:, 0:1], func=AF.Ln, bias=eps_t[:], scale=1.0 / n),
             reads=["mv", "eps"], writes=["rstd"])
        S.op("act", lambda e: e.activation(out=rstd[:], in_=rstd[:], func=AF.Exp, scale=-0.5),
             reads=["rstd"], writes=["rstd"])

    def rope(dst, dkey, src, skey, H, off, half, blk, fslot):
        f0 = fslot[0]
        cosb = COS[:, blk, f0:f0 + half].unsqueeze(1).to_broadcast([128, H, half])
        sinb = SIN[:, blk, f0:f0 + half].unsqueeze(1).to_broadcast([128, H, half])
        x1 = src[:, :, off:off + half]
        x2 = src[:, :, off + half:off + 2 * half]
        t = [sm[i][:, 0:H, 0:half] for i in range(4)]
        S.op("dve", lambda e: e.tensor_tensor(out=t[0], in0=x1, in1=cosb, op=ALU.mult), reads=[skey, "COS"], writes=["sm0"])
        S.op("dve", lambda e: e.tensor_tensor(out=t[1], in0=x2, in1=sinb, op=ALU.mult), reads=[skey, "SIN"], writes=["sm1"])
        S.op("dve", lambda e: e.tensor_tensor(out=t[2], in0=x2, in1=cosb, op=ALU.mult), reads=[skey, "COS"], writes=["sm2"])
        S.op("dve", lambda e: e.tensor_tensor(out=t[3], in0=x1, in1=sinb, op=ALU.mult), reads=[skey, "SIN"], writes=["sm3"])
        S.op("dve", lambda e: e.tensor_tensor(out=dst[:, :, off:off + half], in0=t[0], in1=t[1], op=ALU.subtract),
             reads=["sm0", "sm1"], writes=[dkey])
        S.op("dve", lambda e: e.tensor_tensor(out=dst[:, :, off + half:off + 2 * half], in0=t[2], in1=t[3], op=ALU.add),
             reads=["sm2", "sm3"], writes=[dkey])

    def transpose_to(dst_ap, dkey, src_ap, skey, ncol):
        i = cnt["ps"] % 4 + 4
        cnt["ps"] += 1
        S.op("pe", lambda e: e.transpose(out=ps[i][0:ncol, 0:128], in_=src_ap, identity=ident[:]),
             reads=[skey, "ident"], writes=[psk[i]])
        S.op("act", lambda e: e.copy(out=R(dst_ap), in_=ps[i][0:ncol, 0:128]), reads=[psk[i]], writes=[dkey])

    def proj_block(lhs_tile, lkey, nkc, wtile, wkey, ncols, bi):
        i = cnt["ps"] % 4
        cnt["ps"] += 1
        for kc in range(nkc):
            S.op("pe", lambda e, kc=kc: e.matmul(ps[i][:, 0:ncols], lhsT=R(lhs_tile[:, kc, bi * 128:(bi + 1) * 128]),
                                                 rhs=R(wtile[:, kc, 0:ncols]), start=(kc == 0), stop=(kc == nkc - 1)),
                 reads=[lkey, wkey], writes=[psk[i]], nosync_self=True)
        return ps[i], psk[i]

    def load_w(view, c0, ncols, nkc):
        i = cnt["w"] % len(wbuf)
        cnt["w"] += 1
        wt = wbuf[i]
        flat = wt[:].rearrange("p a b -> p (a b)")
        dst = flat[:, 0:nkc * ncols].rearrange("p (a b) -> p a b", a=nkc)
        S.dma("sp", R(dst), view[:, 0:nkc, c0:c0 + ncols], writes=[wk[i]])
        return dst, wk[i]


    ngroups = NB // G
    tglist = list(range(int(os.environ.get("KDBG_TG", ngroups))))
    if os.environ.get("KDBG_TGLIST"):
        tglist = [int(v) for v in os.environ["KDBG_TGLIST"].split(",")]
    for tg in tglist:
        own = tg < (NOWN // G)
        for bi in range(G):
            blk = tg * G + bi
            xi = cnt["x"] % 2
            cnt["x"] += 1
            xb, xk = xt[xi], "xt%d" % xi
            S.dma("sp", xb[:], xl[blk * 128:(blk + 1) * 128, :], writes=[xk])
            ln_stats(xb, D, xk)
            S.op("dve", lambda e: e.tensor_scalar(out=xb[:], in0=xb[:], scalar1=mv[:, 0:1], scalar2=rstd[:],
                                                  op0=ALU.subtract, op1=ALU.mult), reads=[xk, "mv", "rstd"], writes=[xk])
            S.op("pool", lambda e: e.tensor_tensor(out=xb[:], in0=xb[:], in1=sc1[:], op=ALU.mult),
                 reads=[xk, "sc1"], writes=[xk])
            S.op("pool", lambda e: e.tensor_tensor(out=xb[:], in0=xb[:], in1=sh1[:], op=ALU.add),
                 reads=[xk, "sh1"], writes=[xk])
            for kc in range(16):
                transpose_to(hT[:, kc, bi * 128:(bi + 1) * 128], "hT", xb[:, kc * 128:(kc + 1) * 128], xk, 128)

        def stage_out(kind, sub):
            pass

        def run_group(kind, c0, ncols, sub):
            if os.environ.get("KDBG_KINDS") and kind not in os.environ["KDBG_KINDS"].split(","):
                return
            if kind == "qup":
                wt, wkey = load_w(wq_v, c0, ncols, 4)
                lhs, lkey, nkc = cqT, "cqT", 4
            elif kind == "kvup":
                wt, wkey = load_w(wkv_v, c0, ncols, 2)
                lhs, lkey, nkc = ckvT, "ckvT", 2
            else:
                wt, wkey = load_w(w_in_v, c0, ncols, 16)
                lhs, lkey, nkc = hT, "hT", 16
            si = cnt["stT"] % 2
            cnt["stT"] += 1
            sT, sTk = stT[si], "stT%d" % si
            for bi in range(G):
                blk = tg * G + bi
                pt, pk = proj_block(lhs, lkey, nkc, wt, wkey, ncols, bi)
                gi = cnt["stg"] % 2
                cnt["stg"] += 1
                sg, sgk = stg[gi], "stg%d" % gi
                if kind in ("aq", "ak"):
                    S.op("act", lambda e: e.copy(out=sg[:, 0:256], in_=pt[:, 0:256]), reads=[pk], writes=[sgk])
                    v = sg[:, 0:256].rearrange("p (h d) -> p h d", h=2)
                    pv = pt[:, 0:256].rearrange("p (h d) -> p h d", h=2)
                    rope(v, sgk, v, sgk, 2, 0, 16, blk, FA)
                    for h in range(2):
                        transpose_to(sT[:, h, bi * 128:(bi + 1) * 128], sTk, sg[:, h * 128:(h + 1) * 128], sgk, 128)
                elif kind == "av":
                    S.op("act", lambda e: e.copy(out=sg[:, 0:256], in_=pt[:, 0:256]), reads=[pk], writes=[sgk])
                    S.dma("pool", AV[blk * 128:(blk + 1) * 128, sub * 256:(sub + 1) * 256], sg[:, 0:256], reads=[sgk], writes=["AV"])
                elif kind == "iq":
                    S.op("act", lambda e: e.copy(out=sg[:, 0:256], in_=pt[:, 0:256]), reads=[pk], writes=[sgk])
                    v = sg[:, 0:256].rearrange("p (h d) -> p h d", h=4)
                    pv = pt[:, 0:256].rearrange("p (h d) -> p h d", h=4)
                    rope(v, sgk, v, sgk, 4, 0, 8, blk, FI)
                    S.op("dve", lambda e: e.tensor_tensor(out=v, in0=v, in1=absw[:, bi, sub * 4:(sub + 1) * 4].unsqueeze(2).to_broadcast([128, 4, 64]),
                                                          op=ALU.mult), reads=[sgk, "absw"], writes=[sgk])
                    for h in range(2):
                        transpose_to(sT[:, h, bi * 128:(bi + 1) * 128], sTk, sg[:, h * 128:(h + 1) * 128], sgk, 128)
                elif kind == "misc":
                    S.op("act", lambda e: e.copy(out=sg[:, 0:80], in_=pt[:, 0:80]), reads=[pk], writes=[sgk])
                    ln_stats(sg[:, 0:64], 64, sgk)
                    S.op("dve", lambda e: e.tensor_scalar(out=sg[:, 0:64], in0=sg[:, 0:64], scalar1=mv[:, 0:1], scalar2=rstd[:],
                                                          op0=ALU.subtract, op1=ALU.mult), reads=[sgk, "mv", "rstd"], writes=[sgk])
                    S.op("dve", lambda e: e.tensor_tensor(out=sg[:, 0:64], in0=sg[:, 0:64], in1=ikg_b[:], op=ALU.mult),
                         reads=[sgk, "ikg_b"], writes=[sgk])
                    S.op("dve", lambda e: e.tensor_tensor(out=sg[:, 128:192], in0=sg[:, 0:64], in1=ikb_b[:], op=ALU.add),
                         reads=[sgk, "ikb_b"], writes=[sgk])
                    v = sg[:, 128:192].rearrange("p (h d) -> p h d", h=1)
                    rope(v, sgk, v, sgk, 1, 0, 8, blk, FI)
                    S.op("dve", lambda e: e.tensor_copy(out=sg[:, 192:256], in_=sg[:, 128:192]), reads=[sgk], writes=[sgk])
                    transpose_to(IKT[:, blk * 128:(blk + 1) * 128], "IKT", sg[:, 128:256], sgk, 128)
                    if own:
                        S.op("act", lambda e: e.activation(out=absw[:, bi, :], in_=sg[:, 64:80], func=AF.Abs),
                             reads=[sgk], writes=["absw"])
                        S.op("dve", lambda e: e.tensor_scalar(out=sgn[:, bi, :], in0=sg[:, 64:80], scalar1=0.0, scalar2=-0.5,
                                                              op0=ALU.is_ge, op1=ALU.add), reads=[sgk], writes=["sgn"])
                        S.dma("pool", SGN_d[blk * 128:(blk + 1) * 128, :], sgn[:, bi, :], reads=["sgn"], writes=["SGN_d"])
                elif kind == "qd":
                    S.op("act", lambda e: e.copy(out=qdst[:, bi, sub * 256:(sub + 1) * 256], in_=pt[:, 0:256]),
                         reads=[pk], writes=["qdst"])
                    if sub == 1:
                        rms_rstd(qdst[:, bi, :], 512, "qdst", sg[:, 0:512], sgk)
                        S.op("dve", lambda e: e.scalar_tensor_tensor(out=qdst[:, bi, :], in0=qdst[:, bi, :], scalar=rstd[:],
                                                                     in1=qng_b[:], op0=ALU.mult, op1=ALU.mult),
                             reads=["qdst", "rstd", "qng_b"], writes=["qdst"])
                        for c in range(4):
                            transpose_to(cqT[:, c, bi * 128:(bi + 1) * 128], "cqT", qdst[:, bi, c * 128:(c + 1) * 128], "qdst", 128)
                elif kind == "kvd":
                    S.op("act", lambda e: e.copy(out=sg[:, 0:256], in_=pt[:, 0:256]), reads=[pk], writes=[sgk])
                    rms_rstd(sg[:, 0:256], 256, sgk, sg[:, 256:512], sgk)
                    S.op("dve", lambda e: e.scalar_tensor_tensor(out=sg[:, 0:256], in0=sg[:, 0:256], scalar=rstd[:],
                                                                 in1=kvng_b[:], op0=ALU.mult, op1=ALU.mult),
                         reads=[sgk, "rstd", "kvng_b"], writes=[sgk])
                    for c in range(2):
                        transpose_to(ckvT[:, c, bi * 128:(bi + 1) * 128], "ckvT", sg[:, c * 128:(c + 1) * 128], sgk, 128)
                elif kind == "kr":
                    S.op("act", lambda e: e.copy(out=sg[:, 0:64], in_=pt[:, 0:64]), reads=[pk], writes=[sgk])
                    v = sg[:, 0:64].rearrange("p (h d) -> p h d", h=1)
                    rope(v, sgk, v, sgk, 1, 0, 32, blk, FM)
                    transpose_to(KRT[:, blk * 128:(blk + 1) * 128], "KRT", sg[:, 0:64], sgk, 64)
                elif kind == "qup":
                    S.op("act", lambda e: e.copy(out=sg[:, 0:384], in_=pt[:, 0:384]), reads=[pk], writes=[sgk])
                    v = sg[:, 0:384].rearrange("p (h d) -> p h d", h=2)
                    pv = pt[:, 0:384].rearrange("p (h d) -> p h d", h=2)
                    rope(v, sgk, v, sgk, 2, 128, 32, blk, FM)
                    for h in range(2):
                        transpose_to(sT[:, h, bi * 128:(bi + 1) * 128], sTk, sg[:, h * 192:h * 192 + 128], sgk, 128)
                        transpose_to(sT[0:64, 2 + h, bi * 128:(bi + 1) * 128], sTk, sg[:, h * 192 + 128:(h + 1) * 192], sgk, 64)
                elif kind == "kvup":
                    S.op("act", lambda e: e.copy(out=sg[:, 0:256], in_=pt[:, 0:256]), reads=[pk], writes=[sgk])
                    transpose_to(sT[:, 0, bi * 128:(bi + 1) * 128], sTk, sg[:, 0:128], sgk, 128)
                    S.dma("pool", VB[blk * 128:(blk + 1) * 128, sub * 128:(sub + 1) * 128], sg[:, 128:256], reads=[sgk], writes=["VB"])
            t0 = tg * G * 128
            tw = G * 128
            if kind == "ak":
                for h in range(2):
                    S.dma("pool", AKT[sub * 2 + h, :, t0:t0 + tw], sT[:, h, :], reads=[sTk], writes=["AKT"])
            elif kind == "aq":
                for h in range(2):
                    S.dma("pool", AQT[sub * 2 + h, :, t0:t0 + tw], sT[:, h, :], reads=[sTk], writes=["AQT"])
            elif kind == "iq":
                for h in range(2):
                    S.dma("pool", IQT[sub * 2 + h, :, t0:t0 + tw], sT[:, h, :], reads=[sTk], writes=["IQT"])
            elif kind == "qup":
                for h in range(2):
                    S.dma("pool", QBN[sub * 2 + h, :, t0:t0 + tw], sT[:, h, :], reads=[sTk], writes=["QBN"])
                    S.dma("pool", QBR[sub * 2 + h, :, t0:t0 + tw], sT[0:64, 2 + h, :], reads=[sTk], writes=["QBR"])
            elif kind == "kvup":
                S.dma("pool", KBT[sub, :, t0:t0 + tw], sT[:, 0, :], reads=[sTk], writes=["KBT"])

        run_group("misc", 4096, 80, 0)
        for s_ in range(4):
            run_group("ak", 1024 + s_ * 256, 256, s_)
        for s_ in range(4):
            run_group("av", 2048 + s_ * 256, 256, s_)
        run_group("kvd", 4688, 256, 0)
        run_group("kr", 4944, 64, 0)
        for s_ in range(8):
            run_group("kvup", s_ * 256, 256, s_)
        if own:
            for s_ in range(4):
                run_group("aq", s_ * 256, 256, s_)
            for s_ in range(4):
                run_group("iq", 3072 + s_ * 256, 256, s_)
            for s_ in range(2):
                run_group("qd", 4176 + s_ * 256, 256, s_)
            for s_ in range(4):
                run_group("qup", s_ * 384, 384, s_)
    for tg in tglist:
        S.dma("pool", IKT_d[:, tg * 512:(tg + 1) * 512], IKT[:, tg * 512:(tg + 1) * 512], reads=["IKT"], writes=["IKT_d"])
        S.dma("pool", KRT_d[:, tg * 512:(tg + 1) * 512], KRT[:, tg * 512:(tg + 1) * 512], reads=["KRT"], writes=["KRT_d"])
    pop()
    if stage <= 2:
        return nc, S, es

    SCALE_A = 128.0 ** -0.5
    SCALE_B = 192.0 ** -0.5
    BF = BF16

    def attn_core(j, h, kT, kTk, vt, vk, qT_ap, qkey, scale, OT, okey, mask_fn, extra_qk=None):
        NKB = 2 * j + 2
        blocks = [(part, kb) for part in range(2) for kb in range(0, NKB, 2)]
        for idx, (part, kb) in enumerate(blocks):
            pi = cnt["ps"] % 4
            cnt["ps"] += 1
            for t in range(2):
                kcol = (kb + t) * 128
                S.op("pe", lambda e, t=t, kcol=kcol: e.matmul(ps[pi][:, t * 256:(t + 1) * 256], lhsT=R(kT[part][:, kcol:kcol + 128]),
                                                              rhs=R(qT_ap), start=True, stop=(extra_qk is None)),
                     reads=[kTk[part], qkey], writes=[psk[pi]], nosync_self=True)
                if extra_qk is not None:
                    kr_ap, krkey, qr_ap, qrkey = extra_qk
                    S.op("pe", lambda e, t=t, kcol=kcol: e.matmul(ps[pi][:, t * 256:(t + 1) * 256],
                                                                  lhsT=R(kr_ap[0:64, part * 2048 + kcol:part * 2048 + kcol + 128]),
                                                                  rhs=R(qr_ap), start=False, stop=True),
                         reads=[krkey, qrkey], writes=[psk[pi]], nosync_self=True)
            pti = cnt["pt"] % 2
            cnt["pt"] += 1
            pt, ptk = ptl[pti], "pt%d" % pti
            m_ap = mask_fn(part, kb)
            if m_ap is None:
                S.op("act", lambda e: e.activation(out=R(pt[:]), in_=ps[pi][:, 0:512], func=AF.Exp, scale=scale),
                     reads=[psk[pi]], writes=[ptk])
                src, srck = pt, ptk
            else:
                S.op("act", lambda e: e.activation(out=R(pt[:]), in_=ps[pi][:, 0:512], func=AF.Exp, scale=scale),
                     reads=[psk[pi]], writes=[ptk])
                pm, pmk = ptm[pti], "ptm%d" % pti
                eng = "dve" if (idx % 2 == 0) else "pool"
                S.op(eng, lambda e: e.tensor_tensor(out=R(pm[:]), in0=pt[:], in1=m_ap, op=ALU.mult),
                     reads=[ptk, "mskT"], writes=[pmk])
                src, srck = pm, pmk
            last = idx == len(blocks) - 1
            for t in range(2):
                S.op("pe", lambda e, t=t: e.matmul(ps[6][:, 0:256], lhsT=R(vt[part][:, kb + t, :]), rhs=R(src[:, t * 256:(t + 1) * 256]),
                                                   start=(idx == 0 and t == 0), stop=(last and t == 1)),
                     reads=[vk[part], srck], writes=[psk[6]], nosync_self=True)
                S.op("pe", lambda e, t=t: e.matmul(ps[7][:, 0:256], lhsT=R(ones[:]), rhs=R(src[:, t * 256:(t + 1) * 256]),
                                                   start=(idx == 0 and t == 0), stop=(last and t == 1)),
                     reads=["ones", srck], writes=[psk[7]], nosync_self=True)
        S.op("dve", lambda e: e.reciprocal(out=rden[:], in_=ps[7][:, 0:256]), reads=[psk[7]], writes=["rden"])
        S.op("dve", lambda e: e.tensor_tensor(out=OT[:, h, :], in0=ps[6][:, 0:256], in1=rden[:], op=ALU.mult),
             reads=[psk[6], "rden"], writes=[okey])

    def out_norm(j, OT, okey, goff):
        q0 = j * 256
        for h in range(8):
            S.op("pool", lambda e, h=h: e.tensor_tensor(out=R(sq[:, h, :]), in0=OT[:, h, :], in1=OT[:, h, :], op=ALU.mult),
                 reads=[okey], writes=["sq"])
        for h in range(8):
            S.op("pe", lambda e, h=h: e.matmul(ps[5][:, 0:256], lhsT=R(ones[:]), rhs=R(sq[:, h, :]), start=(h == 0), stop=(h == 7)),
                 reads=["ones", "sq"], writes=[psk[5]], nosync_self=True)
        S.op("act", lambda e: e.activation(out=rden[:], in_=ps[5][:, 0:256], func=AF.Ln, bias=eps_t[:], scale=1.0 / 1024.0),
             reads=[psk[5], "eps"], writes=["rden"])
        S.op("act", lambda e: e.activation(out=rden[:], in_=rden[:], func=AF.Exp, scale=-0.5), reads=["rden"], writes=["rden"])
        for h in range(8):
            S.op("dve", lambda e, h=h: e.scalar_tensor_tensor(out=sq2[:, h, :], in0=OT[:, h, :], scalar=ong[:, goff + h:goff + h + 1],
                                                              in1=rden[:], op0=ALU.mult, op1=ALU.mult),
                 reads=[okey, "ong", "rden"], writes=["sq2"])
        S.dma("pool", MRG[:, goff:goff + 8, q0:q0 + 256], sq2[:], reads=["sq2"], writes=["MRG"])

    cnt["pt"] = 0
    push()
    IKT2 = T("IKT2", [128, SEQ])
    for tg in tglist:
        S.dma("sp", IKT2[:, tg * 512:(tg + 1) * 512], IKT_d[:, tg * 512:(tg + 1) * 512], writes=["IKT2"])
    ong = T("ong", [128, 16])
    S.dma("sp", ong[:], ong_fm, writes=["ong"])
    identb = T("identb", [128, 128], BF)
    S.op("dve", lambda e: e.tensor_copy(out=identb[:], in_=ident[:]), reads=["ident"], writes=["identb"])
    diagb = T("diagb", [128, 128]); rbias = T("rbias", [128, 128])
    S.dma("sp", diagb[:], diagb_d, writes=["diagb"])
    S.dma("sp", rbias[:], rbias_d, writes=["rbias"])
    Isc = T("Isc", [128, 4096])
    work = T("work", [128, 4096])
    msk = T("msk", [128, 4096], BF)
    mskT = T("mskT", [128, 2, 16, 256], BF)
    iqT = T("iqT", [128, 8, 256])
    aqT = T("aqT", [128, 8, 256])
    sgnq = T("sgnq", [128, 2, 16])
    tmpr = [T("tmpr%d" % i, [128, 512]) for i in range(2)]
    m8 = T("m8", [128, 8]); thr = T("thr", [128, 1])
    kT = [T("kT%d" % i, [128, 2048]) for i in range(2)]
    vt = [T("vt%d" % i, [128, 16, 128]) for i in range(2)]
    kTk = ["kT0", "kT1"]; vk = ["vt0", "vt1"]
    ptl = [T("pt%d" % i, [128, 512]) for i in range(2)]
    ptm = [T("ptm%d" % i, [128, 512]) for i in range(2)]
    OT = T("OT", [128, 8, 256])
    sq = T("sq", [128, 8, 256])
    sq2 = T("sq2", [128, 8, 256])
    rden = T("rden", [128, 256])
    npairs = int(os.environ.get("KDBG_NPAIR", 8))
    for j in range(npairs):
        q0 = j * 256
        NKB = 2 * j + 2
        S.dma("sp", iqT[:], IQT[:, :, q0:q0 + 256].rearrange("h p q -> p h q"), writes=["iqT"])
        S.dma("sp", R(aqT[:]), R(AQT[:, :, q0:q0 + 256]).rearrange("h p q -> p h q"), writes=["aqT"])
        S.dma("sp", sgnq[:], SGN_d[q0:q0 + 256, :].rearrange("(b p) h -> p b h", p=128), writes=["sgnq"])
        for part in range(2):
            S.op("pool", lambda e, part=part: e.memset(mskT[:, part, NKB - 1, 0:128], 0.0), writes=["mskT"])
        for qb in range(2):
            i = 2 * j + qb
            nk = i + 1
            for part in range(2):
                for c0 in range(0, nk * 128, 512):
                    cw = min(512, nk * 128 - c0)
                    for h in range(16):
                        pi = cnt["ps"] % 4
                        cnt["ps"] += 1
                        p0 = (h % 2) * 64
                        S.op("pe", lambda e, h=h, p0=p0: e.matmul(ps[pi][:, 0:cw], lhsT=iqT[p0:p0 + 64, h // 2, qb * 128:(qb + 1) * 128],
                                                                  rhs=IKT2[p0:p0 + 64, part * 2048 + c0:part * 2048 + c0 + cw],
                                                                  start=True, stop=True),
                             reads=["iqT", "IKT2"], writes=[psk[pi]], nosync_self=True)
                        ti = cnt["pt"] % 2
                        cnt["pt"] += 1
                        S.op("act", lambda e: e.activation(out=tmpr[ti][:, 0:cw], in_=ps[pi][:, 0:cw], func=AF.Relu),
                             reads=[psk[pi]], writes=["tmpr%d" % ti])
                        if h == 0:
                            S.op("dve", lambda e: e.tensor_scalar(out=Isc[:, part * nk * 128 + c0:part * nk * 128 + c0 + cw], in0=tmpr[ti][:, 0:cw],
                                                                  scalar1=sgnq[:, qb, 0:1], scalar2=None, op0=ALU.mult),
                                 reads=["tmpr%d" % ti, "sgnq"], writes=["Isc"])
                        else:
                            S.op("dve", lambda e, h=h: e.scalar_tensor_tensor(out=Isc[:, part * nk * 128 + c0:part * nk * 128 + c0 + cw], in0=tmpr[ti][:, 0:cw],
                                                                              scalar=sgnq[:, qb, h:h + 1], in1=Isc[:, part * nk * 128 + c0:part * nk * 128 + c0 + cw],
                                                                              op0=ALU.mult, op1=ALU.add),
                                 reads=["tmpr%d" % ti, "sgnq", "Isc"], writes=["Isc"])
            S.op("dve", lambda e: e.tensor_tensor(out=Isc[:, i * 128:(i + 1) * 128], in0=Isc[:, i * 128:(i + 1) * 128],
                                                  in1=diagb[:], op=ALU.add), reads=["Isc", "diagb"], writes=["Isc"])
            S.op("dve", lambda e: e.tensor_tensor(out=Isc[:, nk * 128 + i * 128:nk * 128 + (i + 1) * 128],
                                                  in0=Isc[:, nk * 128 + i * 128:nk * 128 + (i + 1) * 128],
                                                  in1=rbias[:], op=ALU.add), reads=["Isc", "rbias"], writes=["Isc"])
            Iv = Isc[:, 0:2 * nk * 128]
            Wv = work[:, 0:2 * nk * 128]
            if i == 0:
                S.op("dve", lambda e: e.memset(thr[:], -1.0e29), writes=["thr"])
            else:
                for it in range(32):
                    src = Iv if it == 0 else Wv
                    S.op("dve", lambda e: e.max(out=m8[:], in_=src), reads=["Isc", "work"], writes=["m8"])
                    if it < 31:
                        S.op("dve", lambda e: e.match_replace(out=Wv, in_to_replace=m8[:], in_values=src, imm_value=NEG),
                             reads=["Isc", "work", "m8"], writes=["work"])
                S.op("dve", lambda e: e.tensor_scalar(out=thr[:], in0=m8[:, 7:8], scalar1=-1.0e29, scalar2=None, op0=ALU.max),
                     reads=["m8"], writes=["thr"])
            S.op("dve", lambda e: e.tensor_scalar(out=msk[:, 0:2 * nk * 128], in0=Iv, scalar1=thr[:], scalar2=None, op0=ALU.is_ge),
                 reads=["Isc", "thr"], writes=["msk"])
            for part in range(2):
                for kb in range(nk):
                    pi = cnt["ps"] % 4
                    cnt["ps"] += 1
                    pb = ps[pi][:].bitcast(BF)
                    S.op("pe", lambda e: e.transpose(out=pb[:, 0:128], in_=msk[:, (part * nk + kb) * 128:(part * nk + kb + 1) * 128], identity=identb[:]),
                         reads=["msk", "identb"], writes=[psk[pi]])
                    S.op("act", lambda e: e.copy(out=mskT[:, part, kb, qb * 128:(qb + 1) * 128], in_=pb[:, 0:128]),
                         reads=[psk[pi]], writes=["mskT"])
        for h in range(8):
            for part in range(2):
                S.dma("sp", R(kT[part][:, 0:NKB * 128]), R(AKT[h, :, part * 2048:part * 2048 + NKB * 128]), writes=[kTk[part]])
                S.dma("sp", R(vt[part][:, 0:NKB, :]),
                      R(AV[part * 2048:part * 2048 + NKB * 128, h * 128:(h + 1) * 128]).rearrange("(kb p) d -> p kb d", p=128),
                      writes=[vk[part]])
            attn_core(j, h, kT, kTk, vt, vk, aqT[:, h, :], "aqT", SCALE_A, OT, "OT",
                      lambda part, kb: mskT[:, part, kb:kb + 2, :].rearrange("p a b -> p (a b)"))
        out_norm(j, OT, "OT", 0)
    pop()
    if stage <= 3:
        return nc, S, es

    push()
    KRT2 = T("KRT2", [64, SEQ])
    for tg in tglist:
        S.dma("sp", R(KRT2[:, tg * 512:(tg + 1) * 512]), R(KRT_d[:, tg * 512:(tg + 1) * 512]), writes=["KRT2"])
    ong = T("ong", [128, 16])
    S.dma("sp", ong[:], ong_fm, writes=["ong"])
    mlam = T("mlam", [128, 4, 256])
    S.dma("sp", mlam[:], mlam_d.rearrange("p (a b) -> p a b", a=4), writes=["mskT"])
    qbn = T("qbn", [128, 8, 256])
    qbr = T("qbr", [64, 8, 256])
    kT = [T("kT%d" % i, [128, 2048]) for i in range(2)]
    vt = [T("vt%d" % i, [128, 16, 128]) for i in range(2)]
    ptl = [T("pt%d" % i, [128, 512]) for i in range(2)]
    ptm = [T("ptm%d" % i, [128, 512]) for i in range(2)]
    OT = T("OT", [128, 8, 256])
    sq = T("sq", [128, 8, 256])
    sq2 = T("sq2", [128, 8, 256])
    rden = T("rden", [128, 256])
    for j in range(npairs):
        q0 = j * 256
        NKB = 2 * j + 2
        S.dma("sp", R(qbn[:]), R(QBN[:, :, q0:q0 + 256]).rearrange("h p q -> p h q"), writes=["qbn"])
        S.dma("sp", R(qbr[:]), R(QBR[:, :, q0:q0 + 256]).rearrange("h p q -> p h q"), writes=["qbr"])
        for h in range(8):
            for part in range(2):
                S.dma("sp", R(kT[part][:, 0:NKB * 128]), R(KBT[h, :, part * 2048:part * 2048 + NKB * 128]), writes=[kTk[part]])
                S.dma("sp", R(vt[part][:, 0:NKB, :]),
                      R(VB[part * 2048:part * 2048 + NKB * 128, h * 128:(h + 1) * 128]).rearrange("(kb p) d -> p kb d", p=128),
                      writes=[vk[part]])
            attn_core(j, h, kT, kTk, vt, vk, qbn[:, h, :], "qbn", SCALE_B, OT, "OT",
                      lambda part, kb, NKB=NKB: (mlam[:, part * 2:part * 2 + 2, :].rearrange("p a b -> p (a b)")
                                                 if kb == NKB - 2 else None),
                      extra_qk=(KRT2, "KRT2", qbr[:, h, :], "qbr"))
        out_norm(j, OT, "OT", 8)
    pop()
    if stage <= 4:
        return nc, S, es

    push()
    wbuf = [T("wbuf%d" % i, [128, 16, 256]) for i in range(3)]
    bc = {}
    for nm, src in [("g1", ada_d[:, 2 * D:3 * D]), ("sh2", ada_d[:, 3 * D:4 * D]), ("sc2", ada_d[:, 4 * D:5 * D]),
                    ("lnmg", lnmg), ("lnmb", lnmb)]:
        bc[nm] = T("bc_" + nm, [128, D])
        S.dma("sp", bc[nm][:], src.partition_broadcast(128), writes=["bc_" + nm])
    S.op("dve", lambda e: e.tensor_scalar(out=bc["sc2"][:], in0=bc["sc2"][:], scalar1=1.0, scalar2=None, op0=ALU.add),
         reads=["bc_sc2"], writes=["bc_sc2"])
    mT = T("mT", [128, 16, 256])
    ymix = [T("ymix%d" % i, [128, D]) for i in range(2)]
    xt = [T("xt%d" % i, [128, D]) for i in range(2)]
    h2b = T("h2b", [128, 16, 128])
    wr = T("wr", [128, 16, NEXP])
    S.dma("sp", R(wr[:]), w_router.rearrange("(kc p) e -> p kc e", p=128), writes=["wr"])
    brt = T("brt", [128, NEXP])
    S.dma("sp", brt[:], b_router.partition_broadcast(128), writes=["brt"])
    lg = T("lg", [128, NEXP]); ex = T("ex", [128, NEXP]); mk = T("mk", [128, NEXP])
    m8 = T("m8", [128, 8]); nmx = T("nmx", [128, 1]); rs = T("rs", [128, 1])
    gts = T("gts", [128, 128])
    st6 = T("st6", [128, 4, 6]); mv = T("mv", [128, 2]); rstd = T("rstd", [128, 1])
    w_out_v = w_out.rearrange("(kc p) f -> p kc f", p=128)
    for j in range(npairs):
        q0 = j * 256
        S.dma("sp", R(mT[:]), R(MRG[:, :, q0:q0 + 256]), writes=["mT"])
        for tb in range(2):
            S.dma("sp", xt[tb][:], xl[q0 + tb * 128:q0 + (tb + 1) * 128, :], writes=["xt%d" % tb])
        for cg in range(8):
            wt, wkey = load_w(w_out_v, cg * 256, 256, 16)
            for tb in range(2):
                pt_, pk = proj_block(mT, "mT", 16, wt, wkey, 256, tb)
                S.op("dve", lambda e: e.tensor_tensor(out=ymix[tb][:, cg * 256:(cg + 1) * 256], in0=pt_[:, 0:256],
                                                      in1=bc["g1"][:, cg * 256:(cg + 1) * 256], op=ALU.mult),
                     reads=[pk, "bc_g1"], writes=["ymix%d" % tb])
        for tb in range(2):
            blk = 2 * j + tb
            xb, xk = xt[tb], "xt%d" % tb
            ym, yk = ymix[tb], "ymix%d" % tb
            S.op("dve", lambda e: e.scalar_tensor_tensor(out=ym[:], in0=xb[:], scalar=ALPHA, in1=ym[:], op0=ALU.mult, op1=ALU.add),
                 reads=[xk, yk], writes=[yk])
            ln_stats2 = lambda src, key: _ln_stats(src, D, key)
            _ln_stats(ym, D, yk)
            S.op("dve", lambda e: e.tensor_scalar(out=ym[:], in0=ym[:], scalar1=mv[:, 0:1], scalar2=rstd[:],
                                                  op0=ALU.subtract, op1=ALU.mult), reads=[yk, "mv", "rstd"], writes=[yk])
            S.op("pool", lambda e: e.tensor_tensor(out=ym[:], in0=ym[:], in1=bc["lnmg"][:], op=ALU.mult),
                 reads=[yk, "bc_lnmg"], writes=[yk])
            S.op("pool", lambda e: e.tensor_tensor(out=ym[:], in0=ym[:], in1=bc["lnmb"][:], op=ALU.add),
                 reads=[yk, "bc_lnmb"], writes=[yk])
            S.dma("pool", X1[blk * 128:(blk + 1) * 128, :], ym[:], reads=[yk], writes=["X1"])
            _ln_stats(ym, D, yk)
            S.op("dve", lambda e: e.tensor_scalar(out=xb[:], in0=ym[:], scalar1=mv[:, 0:1], scalar2=rstd[:],
                                                  op0=ALU.subtract, op1=ALU.mult), reads=[yk, "mv", "rstd"], writes=[xk])
            S.op("pool", lambda e: e.tensor_tensor(out=xb[:], in0=xb[:], in1=bc["sc2"][:], op=ALU.mult),
                 reads=[xk, "bc_sc2"], writes=[xk])
            S.op("pool", lambda e: e.tensor_tensor(out=xb[:], in0=xb[:], in1=bc["sh2"][:], op=ALU.add),
                 reads=[xk, "bc_sh2"], writes=[xk])
            for kc in range(16):
                transpose_to(h2b[:, kc, :], "h2b", xb[:, kc * 128:(kc + 1) * 128], xk, 128)
            S.dma("pool", H2T[:, :, blk * 128:(blk + 1) * 128], h2b[:], reads=["h2b"], writes=["H2T"])
            pi = cnt["ps"] % 4
            cnt["ps"] += 1
            for kc in range(16):
                S.op("pe", lambda e, kc=kc: e.matmul(ps[pi][:, 0:NEXP], lhsT=R(h2b[:, kc, :]), rhs=R(wr[:, kc, :]),
                                                     start=(kc == 0), stop=(kc == 15)),
                     reads=["h2b", "wr"], writes=[psk[pi]], nosync_self=True)
            S.op("dve", lambda e: e.tensor_tensor(out=lg[:], in0=ps[pi][:, 0:NEXP], in1=brt[:], op=ALU.add),
                 reads=[psk[pi], "brt"], writes=["lg"])
            S.op("dve", lambda e: e.max(out=m8[:], in_=lg[:]), reads=["lg"], writes=["m8"])
            S.op("dve", lambda e: e.tensor_scalar(out=nmx[:], in0=m8[:, 0:1], scalar1=-1.0, scalar2=None, op0=ALU.mult),
                 reads=["m8"], writes=["nmx"])
            S.op("act", lambda e: e.activation(out=ex[:], in_=lg[:], func=AF.Exp, bias=nmx[:], scale=1.0),
                 reads=["lg", "nmx"], writes=["ex"])
            S.op("dve", lambda e: e.tensor_scalar(out=mk[:], in0=lg[:], scalar1=m8[:, 3:4], scalar2=None, op0=ALU.is_ge),
                 reads=["lg", "m8"], writes=["mk"])
            S.op("dve", lambda e: e.tensor_tensor(out=ex[:], in0=ex[:], in1=mk[:], op=ALU.mult), reads=["ex", "mk"], writes=["ex"])
            S.op("dve", lambda e: e.reduce_sum(out=rs[:], in_=ex[:], axis=AX.X), reads=["ex"], writes=["rs"])
            S.op("dve", lambda e: e.reciprocal(out=rs[:], in_=rs[:]), reads=["rs"], writes=["rs"])
            S.op("dve", lambda e: e.tensor_scalar(out=ex[:], in0=ex[:], scalar1=rs[:], scalar2=None, op0=ALU.mult),
                 reads=["ex", "rs"], writes=["ex"])
            transpose_to(gts[0:NEXP, :], "gts", ex[:], "ex", NEXP)
            S.dma("pool", GT_d[:, blk * 128:(blk + 1) * 128], gts[0:NEXP, :], reads=["gts"], writes=["GT_d"])
    pop()
    if stage <= 5:
        return nc, S, es

    FFT = dram_scr("FFT", [128, 16, 2048])
    push()
    wbuf = [T("wbig%d" % i, [128, 16, 512]) for i in range(2)]
    wk = ["wbig0", "wbig1"]
    TS = 512
    h2T = T("h2T", [128, 16, TS])
    accT = T("accT", [128, 16, TS])
    actT = T("actT", [128, 16, TS])
    gbc = [T("gbc%d" % i, [128, TS]) for i in range(2)]
    gt = T("gt", [NEXP, TS])
    bdn = T("bdn", [NEXP, D])
    S.dma("sp", R(bdn[:]), b_dn, writes=["bdn"])
    bgu = T("bgu", [128, NEXP, 16, 2])
    S.dma("sp", bgu[:], bgu_fm.rearrange("p (e j t) -> p e j t", e=NEXP, j=16), writes=["bgu"])
    tg_ = [T("tg%d" % i, [128, TS]) for i in range(2)]
    tsg = [T("tsg%d" % i, [128, TS]) for i in range(2)]
    tu = [T("tu%d" % i, [128, TS]) for i in range(2)]
    tsb = [T("tsb%d" % i, [128, TS]) for i in range(2)]
    nsp = int(os.environ.get("KDBG_NSP", 2048 // TS))
    nexp = int(os.environ.get("KDBG_NEXP", NEXP))
    for sp in range(nsp):
        t0 = sp * TS
        S.dma("sp", R(h2T[:]), R(H2T[:, :, t0:t0 + TS]), writes=["h2T"])
        S.dma("sp", R(gt[:]), R(GT_d[:, t0:t0 + TS]), writes=["gt"])
        for dc in range(16):
            pi = cnt["ps"] % 4
            cnt["ps"] += 1
            S.op("pe", lambda e: e.matmul(ps[pi][:, 0:TS], lhsT=R(bdn[:, dc * 128:(dc + 1) * 128]), rhs=R(gt[:]), start=True, stop=True),
                 reads=["bdn", "gt"], writes=[psk[pi]], nosync_self=True)
            S.op("act", lambda e: e.copy(out=accT[:, dc, :], in_=ps[pi][:, 0:TS]), reads=[psk[pi]], writes=["accT"])
        for ex_ in range(nexp):
            gb, gbk = gbc[ex_ % 2], "gbc%d" % (ex_ % 2)
            S.dma("sp", gb[:], GT_d[ex_:ex_ + 1, t0:t0 + TS].partition_broadcast(128), writes=[gbk])
            wgu_v = w_gu[ex_].rearrange("(kc p) f -> p kc f", p=128)
            wdn_v = w_dn[ex_].rearrange("(kc p) f -> p kc f", p=128)
            for jf2 in range(8):
                wt, wkey = load_w(wgu_v, jf2 * 512, 512, 16)
                for sub in range(2):
                    jf = jf2 * 2 + sub
                    c0 = sub * 256
                    pg = cnt["ps"] % 4; cnt["ps"] += 1
                    pu = cnt["ps"] % 4; cnt["ps"] += 1
                    for kc in range(16):
                        S.op("pe", lambda e, kc=kc: e.matmul(ps[pg][:, 0:TS], lhsT=R(wt[:, kc, c0:c0 + 256:2]), rhs=R(h2T[:, kc, :]),
                                                             start=(kc == 0), stop=(kc == 15)),
                             reads=[wkey, "h2T"], writes=[psk[pg]], nosync_self=True)
                    for kc in range(16):
                        S.op("pe", lambda e, kc=kc: e.matmul(ps[pu][:, 0:TS], lhsT=R(wt[:, kc, c0 + 1:c0 + 256:2]), rhs=R(h2T[:, kc, :]),
                                                             start=(kc == 0), stop=(kc == 15)),
                             reads=[wkey, "h2T"], writes=[psk[pu]], nosync_self=True)
                    a = jf % 2
                    S.op("dve", lambda e: e.tensor_scalar(out=tg_[a][:], in0=ps[pg][:, 0:TS], scalar1=bgu[:, ex_, jf, 0:1], scalar2=7.0,
                                                          op0=ALU.add, op1=ALU.min), reads=[psk[pg], "bgu"], writes=["tg%d" % a])
                    S.op("act", lambda e: e.activation(out=tsg[a][:], in_=tg_[a][:], func=AF.Sigmoid, scale=1.702),
                         reads=["tg%d" % a], writes=["tsg%d" % a])
                    S.op("act", lambda e: e.activation(out=tu[a][:], in_=ps[pu][:, 0:TS], func=AF.Identity,
                                                       bias=bgu[:, ex_, jf, 1:2], scale=1.0),
                         reads=[psk[pu], "bgu"], writes=["tu%d" % a])
                    S.op("pool", lambda e: e.tensor_tensor(out=tsb[a][:], in0=tsg[a][:], in1=gb[:], op=ALU.mult),
                         reads=["tsg%d" % a, gbk], writes=["tsb%d" % a])
                    S.op("dve", lambda e: e.tensor_scalar(out=tu[a][:], in0=tu[a][:], scalar1=7.0, scalar2=-7.0,
                                                          op0=ALU.min, op1=ALU.max), reads=["tu%d" % a], writes=["tu%d" % a])
                    S.op("dve", lambda e: e.tensor_tensor(out=tg_[a][:], in0=tg_[a][:], in1=tsb[a][:], op=ALU.mult),
                         reads=["tg%d" % a, "tsb%d" % a], writes=["tg%d" % a])
                    S.op("dve", lambda e: e.scalar_tensor_tensor(out=R(actT[:, jf, :]), in0=tu[a][:], scalar=1.0, in1=tg_[a][:],
                                                                 op0=ALU.add, op1=ALU.mult),
                         reads=["tu%d" % a, "tg%d" % a], writes=["actT"])
            for dg in range(4):
                wt, wkey = load_w(wdn_v, dg * 512, 512, 16)
                for dd in range(4):
                    dc = dg * 4 + dd
                    pi = cnt["ps"] % 4
                    cnt["ps"] += 1
                    for fc in range(16):
                        S.op("pe", lambda e, fc=fc: e.matmul(ps[pi][:, 0:TS], lhsT=R(wt[:, fc, dd * 128:(dd + 1) * 128]),
                                                             rhs=R(actT[:, fc, :]), start=(fc == 0), stop=(fc == 15)),
                             reads=[wkey, "actT"], writes=[psk[pi]], nosync_self=True)
                    S.op("dve", lambda e: e.tensor_tensor(out=accT[:, dc, :], in0=ps[pi][:, 0:TS], in1=accT[:, dc, :], op=ALU.add),
                         reads=[psk[pi], "accT"], writes=["accT"])
        S.dma("pool", FFT[:, :, t0:t0 + TS], accT[:], reads=["accT"], writes=["FFT"])
    pop()
    if stage <= 6:
        return nc, S, es

    push()
    g2b = T("g2b", [128, D]); lgb_t = T("lgb", [128, D]); lbb_t = T("lbb", [128, D])
    S.dma("sp", g2b[:], ada_d[:, 5 * D:6 * D].partition_broadcast(128), writes=["g2b"])
    S.dma("sp", lgb_t[:], lnfg.partition_broadcast(128), writes=["lgb"])
    S.dma("sp", lbb_t[:], lnfb.partition_broadcast(128), writes=["lbb"])
    st6 = T("st6", [128, 4, 6]); mv = T("mv", [128, 2]); rstd = T("rstd", [128, 1])
    fb = [T("fb%d" % i, [128, 16, 128]) for i in range(2)]
    yfl = [T("yf%d" % i, [128, D]) for i in range(2)]
    x1l = [T("x1t%d" % i, [128, D]) for i in range(2)]
    nblk = nsp * (TS // 128)
    for blk in range(nblk):
        a = blk % 2
        fbt, fbk = fb[a], "fb%d" % a
        yf, yk = yfl[a], "yf%d" % a
        x1t, x1k = x1l[a], "x1t%d" % a
        S.dma("sp", fbt[:], FFT[:, :, blk * 128:(blk + 1) * 128], writes=[fbk])
        S.dma("sp", x1t[:], X1[blk * 128:(blk + 1) * 128, :], writes=[x1k])
        for dc in range(16):
            i = cnt["ps"] % 4 + 4
            cnt["ps"] += 1
            S.op("pe", lambda e: e.transpose(out=ps[i][:, 0:128], in_=fbt[:, dc, :], identity=ident[:]),
                 reads=[fbk, "ident"], writes=[psk[i]])
            S.op("act", lambda e: e.copy(out=yf[:, dc * 128:(dc + 1) * 128], in_=ps[i][:, 0:128]), reads=[psk[i]], writes=[yk])
        S.op("pool", lambda e: e.tensor_tensor(out=yf[:], in0=yf[:], in1=g2b[:], op=ALU.mult), reads=[yk, "g2b"], writes=[yk])
        S.op("dve", lambda e: e.scalar_tensor_tensor(out=yf[:], in0=x1t[:], scalar=ALPHA, in1=yf[:], op0=ALU.mult, op1=ALU.add),
             reads=[yk, x1k], writes=[yk])
        _ln_stats(yf, D, yk)
        S.op("dve", lambda e: e.tensor_scalar(out=yf[:], in0=yf[:], scalar1=mv[:, 0:1], scalar2=rstd[:],
                                              op0=ALU.subtract, op1=ALU.mult), reads=[yk, "mv", "rstd"], writes=[yk])
        S.op("pool", lambda e: e.tensor_tensor(out=yf[:], in0=yf[:], in1=lgb_t[:], op=ALU.mult), reads=[yk, "lgb"], writes=[yk])
        S.op("pool", lambda e: e.tensor_tensor(out=yf[:], in0=yf[:], in1=lbb_t[:], op=ALU.add), reads=[yk, "lbb"], writes=[yk])
        S.dma("pool", out_d[blk * 128:(blk + 1) * 128, :], yf[:], reads=[yk], writes=["out"])
    pop()
    return nc, S, es


def finish(nc, S, out_written=True):
    S.barrier()


_INVF = np.concatenate([
    (THETA ** (-np.arange(16, dtype=np.float32) / np.float32(16))).astype(np.float32),
    (THETA ** (-np.arange(8, dtype=np.float32) / np.float32(8))).astype(np.float32),
    (THETA ** (-np.arange(32, dtype=np.float32) / np.float32(32))).astype(np.float32),
]).astype(np.float32)[None, :]


def make_in_maps(inp):
    f = lambda a: np.ascontiguousarray(np.asarray(a, dtype=np.float32))
    x = f(inp["x"]); c = f(inp["c"]); pos = np.asarray(inp["positions"]).astype(np.int32)
    shared = dict(
        w_ada=f(inp["w_ada"][0]), b_ada=f(inp["b_ada"][0])[None, :], w_in=f(inp["w_in"][0]),
        ikg=f(inp["idx_k_norm_g"][0])[None, :], ikb=f(inp["idx_k_norm_b"][0])[None, :],
        qng=f(inp["q_norm_g"][0])[None, :], w_q_up=f(inp["w_q_up"][0]),
        kvng=f(inp["kv_norm_g"][0])[None, :], w_kv_up=f(inp["w_kv_up"][0]),
        ong_fm=f(np.concatenate([np.asarray(inp["out_norm_a_g"][0]).reshape(8, 128),
                                 np.asarray(inp["out_norm_b_g"][0]).reshape(8, 128)], 0).T),
        w_out=f(inp["w_out"][0]), lnmg=f(inp["ln_mix_g"][0])[None, :], lnmb=f(inp["ln_mix_b"][0])[None, :],
        w_router=f(inp["w_router"][0]), b_router=f(inp["b_router"][0])[None, :],
        w_gu=f(inp["w_gate_up"][0]),
        bgu_fm=f(np.asarray(inp["b_gate_up"][0]).reshape(NEXP, 16, 128, 2).transpose(2, 0, 1, 3).reshape(128, NEXP * 32)),
        w_dn=f(inp["w_down"][0]), b_dn=f(inp["b_down"][0]),
        lnfg=f(inp["ln_ffn_g"][0])[None, :], lnfb=f(inp["ln_ffn_b"][0])[None, :],
        ident=np.eye(128, dtype=np.float32), ones=np.ones((128, 128), np.float32),
        invf=_INVF,
    )
    esel = np.zeros((NEXP, NEXP, 128), np.float32)
    for e in range(NEXP):
        esel[e, e, :] = 1.0
    shared["esel"] = esel.reshape(NEXP, NEXP * 128)
    qi = np.arange(128)[:, None] // 64
    ki = np.arange(128)[None, :] // 64
    diag_ok = (ki <= qi)
    shared["diagb"] = np.where(diag_ok, 0.0, NEG).astype(np.float32)
    maps = []
    for core in range(8):
        b, r = core // 2, core % 2
        own_blocks = [2 * i + r for i in range(NOWN)]
        oth_blocks = [2 * i + 1 - r for i in range(NOWN)]
        order = own_blocks + oth_blocks
        xb = x[b].reshape(NB, 128, D)[order].reshape(SEQ, D)
        pb = pos[b].reshape(NB, 128)[order]
        m = dict(shared)
        m["xl"] = np.ascontiguousarray(xb)
        m["c_fm"] = np.ascontiguousarray(c[b].reshape(16, 128).T)
        m["pos_i"] = np.ascontiguousarray(pb.T.astype(np.int32))
        m["rbias"] = np.full((128, 128), 0.0 if r == 1 else NEG, np.float32)
        dT = diag_ok.T.astype(np.float32)
        one = np.ones((128, 128), np.float32); zero = np.zeros((128, 128), np.float32)
        rr = one * float(r)
        mm = np.stack([np.concatenate([dT, one], 1), np.concatenate([zero, dT], 1),
                       np.concatenate([rr, one], 1), np.concatenate([zero, rr], 1)], 1)
        m["mlam"] = np.ascontiguousarray(mm.reshape(128, 1024).astype(np.float32))
        maps.append(m)
    return maps


def kernel(**inputs):
    nc, S, es = build()
    finish(nc, S)
    maps = make_in_maps(inputs)
    res = run_bass_kernel_spmd(nc, maps, core_ids=list(range(8)))
    out = np.zeros((4, SEQ, D), np.float32)
    for core in range(8):
        b, r = core // 2, core % 2
        o = np.asarray(res.results[core]["out"]).reshape(NOWN, 128, D)
        ov = out[b].reshape(NB, 128, D)
        for i in range(NOWN):
            ov[2 * i + r] = o[i]
    return out
```

```python
import os
from contextlib import ExitStack
import numpy as np
import concourse.bass as bass
import concourse.mybir as mybir
from concourse.bass_utils import run_bass_kernel_spmd

F32 = mybir.dt.float32
F32R = mybir.dt.float32r
BF16 = mybir.dt.bfloat16
I32 = mybir.dt.int32
AF = mybir.ActivationFunctionType
ALU = mybir.AluOpType
AX = mybir.AxisListType

D = 2048
SEQ = 4096
NB = 32
NOWN = 16
EPS = 1e-5
ALPHA = 2.0 ** 0.25
THETA = 500000.0
NEXP = 32
NEG = -1.0e30
IN_W = 5008
TWO_PI = 2.0 * np.pi
C1 = 6.28125
C2 = TWO_PI - C1


def R(ap):
    return ap.bitcast(F32R)


class Sched:
    def __init__(self, nc, es, ndma=28):
        self.nc = nc
        self.E = {}
        for name, eng in [("pe", nc.tensor), ("act", nc.scalar), ("dve", nc.vector),
                          ("pool", nc.gpsimd), ("sp", nc.sync)]:
            sem = es.enter_context(nc.semaphore("sem_" + name))
            self.E[name] = dict(eng=eng, sem=sem, cnt=0, waited={})
        self.dsem = [es.enter_context(nc.semaphore("dsem%d" % i)) for i in range(ndma)]
        self.dcnt = [0] * ndma
        self.dpool = {"sp": list(range(0, 16)), "act": list(range(16, ndma))}
        self.drr = {"sp": 0, "act": 0}
        self.lw = {}
        self.rd = {}
        self.ninst = 0

    def semh(self, sid):
        if isinstance(sid, tuple):
            return self.dsem[sid[1]]
        return self.E[sid]["sem"]

    def _wait(self, en, toks):
        E = self.E[en]
        need = {}
        for t in toks:
            if t is None:
                continue
            sid, v = t
            if E["waited"].get(sid, 0) < v:
                need[sid] = max(need.get(sid, 0), v)
        for sid, v in need.items():
            E["eng"].wait_ge(self.semh(sid), v)
            E["waited"][sid] = v
            self.ninst += 1

    def _deps(self, reads, writes):
        toks = []
        for k in reads:
            toks.append(self.lw.get(k))
            if isinstance(k, str) and k.startswith("ps"):
                toks += list(self.rd.get(k, {}).items())
        for k in writes:
            toks.append(self.lw.get(k))
            toks += list(self.rd.get(k, {}).items())
        return toks

    def _record(self, tok, reads, writes):
        for k in reads:
            d = self.rd.setdefault(k, {})
            d[tok[0]] = max(d.get(tok[0], 0), tok[1])
        for k in writes:
            self.lw[k] = tok
            self.rd[k] = {}

    def op(self, en, fn, reads=(), writes=(), nosync_self=False):
        toks = self._deps(reads, writes)
        if nosync_self:
            toks = [t for t in toks if t is not None and t[0] != en]
        self._wait(en, toks)
        E = self.E[en]
        E["cnt"] += 1
        ins = fn(E["eng"])
        ins.then_inc(E["sem"], 1)
        self.ninst += 1
        tok = (en, E["cnt"])
        E["waited"][en] = max(E["waited"].get(en, 0), 0)
        self._record(tok, reads, writes)
        return tok

    def dma(self, qn, out, in_, reads=(), writes=()):
        if qn == "pool":
            qn = "act"
        pl = self.dpool[qn]
        i = pl[self.drr[qn] % len(pl)]
        self.drr[qn] += 1
        toks = self._deps(reads, writes)
        if self.dcnt[i] > 0:
            toks.append((("d", i), 16 * self.dcnt[i]))
        self._wait(qn, toks)
        self.dcnt[i] += 1
        self.E[qn]["eng"].dma_start(out=out, in_=in_).then_inc(self.dsem[i], 16)
        self.ninst += 1
        tok = (("d", i), 16 * self.dcnt[i])
        self._record(tok, reads, writes)
        return tok

    def barrier(self):
        toks = [(n, e["cnt"]) for n, e in self.E.items() if e["cnt"] > 0]
        toks += [(("d", i), 16 * c) for i, c in enumerate(self.dcnt) if c > 0]
        for en in self.E:
            self._wait(en, toks)
        self.lw = {}
        self.rd = {}


def build(stage=99, debug=False):
    nc = bass.Bass("TRN2", target_bir_lowering=False)
    nc.dge_precook = False
    es = ExitStack()
    S = Sched(nc, es)
    dbg_kind = "ExternalOutput" if debug else "Internal"

    def dram_in(name, shape, dt=F32):
        return nc.dram_tensor(name, list(shape), dt, kind="ExternalInput").ap()

    def dram_scr(name, shape, dt=F32):
        return nc.dram_tensor(name, list(shape), dt, kind=dbg_kind).ap()

    xl = dram_in("xl", [SEQ, D])
    c_fm = dram_in("c_fm", [128, 16])
    pos_i = dram_in("pos_i", [128, NB], I32)
    invf = dram_in("invf", [1, 56])
    w_ada = dram_in("w_ada", [D, 6 * D], F32R)
    b_ada = dram_in("b_ada", [1, 6 * D])
    w_in = dram_in("w_in", [D, IN_W], F32R)
    ikg = dram_in("ikg", [1, 64])
    ikb = dram_in("ikb", [1, 64])
    qng = dram_in("qng", [1, 512])
    w_q_up = dram_in("w_q_up", [512, 1536], F32R)
    kvng = dram_in("kvng", [1, 256])
    w_kv_up = dram_in("w_kv_up", [256, 2048], F32R)
    ong_fm = dram_in("ong_fm", [128, 16])
    w_out = dram_in("w_out", [D, D], F32R)
    lnmg = dram_in("lnmg", [1, D])
    lnmb = dram_in("lnmb", [1, D])
    w_router = dram_in("w_router", [D, NEXP], F32R)
    b_router = dram_in("b_router", [1, NEXP])
    big = stage >= 6
    w_gu = dram_in("w_gu", [NEXP, D, 2 * D], F32R) if big else None
    bgu_fm = dram_in("bgu_fm", [128, NEXP * 32])
    w_dn = dram_in("w_dn", [NEXP, D, D], F32R) if big else None
    b_dn = dram_in("b_dn", [NEXP, D], F32R)
    lnfg = dram_in("lnfg", [1, D])
    lnfb = dram_in("lnfb", [1, D])
    ident_d = dram_in("ident", [128, 128])
    ones_d = dram_in("ones", [128, 128], F32R)
    esel_d = dram_in("esel", [NEXP, NEXP * 128], F32R)
    diagb_d = dram_in("diagb", [128, 128])
    rbias_d = dram_in("rbias", [128, 128])
    mlam_d = dram_in("mlam", [128, 4 * 256])
    out_d = nc.dram_tensor("out", [NOWN * 128, D], F32, kind="ExternalOutput").ap()

    ada_d = dram_scr("ada_d", [1, 6 * D])
    AKT = dram_scr("AKT", [8, 128, SEQ])
    AV = dram_scr("AV", [SEQ, 1024])
    KBT = dram_scr("KBT", [8, 128, SEQ])
    VB = dram_scr("VB", [SEQ, 1024])
    AQT = dram_scr("AQT", [8, 128, 2048])
    IQT = dram_scr("IQT", [8, 128, 2048])
    QBN = dram_scr("QBN", [8, 128, 2048])
    QBR = dram_scr("QBR", [8, 64, 2048])
    IKT_d = dram_scr("IKT_d", [128, SEQ])
    KRT_d = dram_scr("KRT_d", [64, SEQ])
    SGN_d = dram_scr("SGN_d", [2048, 16])
    X1 = dram_scr("X1", [2048, D])
    H2T = dram_scr("H2T", [128, 16, 2048])
    GT_d = dram_scr("GT_d", [NEXP, 2048])
    MRG = dram_scr("MRG", [128, 16, 2048])

    scopes = [es]

    used_names = {}

    def T(name, shape, dt=F32):
        n = used_names.get(name, 0)
        used_names[name] = n + 1
        nm = "sb_" + name + ("" if n == 0 else "_v%d" % n)
        return scopes[-1].enter_context(nc.sbuf_tensor(nm, list(shape), dt))

    def push():
        scopes.append(ExitStack())

    def pop():
        S.barrier()
        scopes.pop().close()

    def P(name, shape, dt=F32):
        return es.enter_context(nc.psum_tensor("pp_" + name, list(shape), dt))

    ident = T("ident", [128, 128])
    ones = T("ones", [128, 128])
    S.dma("sp", ident[:], ident_d, writes=["ident"])
    S.dma("sp", R(ones[:]), ones_d, writes=["ones"])
    ps = [P("ps%d" % i, [128, 512]) for i in range(8)]
    psk = ["ps%d" % i for i in range(8)]
    eps_t = T("eps_t", [128, 1])
    S.op("dve", lambda e: e.memset(eps_t[:], EPS), writes=["eps"])

    push()
    wbuf = [T("wbuf%d" % i, [128, 16, 256]) for i in range(3)]
    wk = ["wbuf%d" % i for i in range(3)]
    push()
    cfm = T("cfm", [128, 16])
    scr = T("scr", [128, 16, 128])
    S.dma("sp", cfm[:], c_fm, writes=["cfm"])
    S.op("act", lambda e: e.activation(out=cfm[:], in_=cfm[:], func=AF.Silu), reads=["cfm"], writes=["cfm"])
    S.op("dve", lambda e: e.tensor_copy(out=R(scr[:]), in_=cfm[:].unsqueeze(2).to_broadcast([128, 16, 128])),
         reads=["cfm"], writes=["scr"])
    bada = [T("bada%d" % i, [128, 256]) for i in range(2)]
    w_ada_v = w_ada.rearrange("(kc p) f -> p kc f", p=128)
    for g in range(48):
        wb, wkk = wbuf[g % 3], wk[g % 3]
        bt, bk = bada[g % 2], "bada%d" % (g % 2)
        S.dma("sp", R(wb[:]), w_ada_v[:, :, g * 256:(g + 1) * 256], writes=[wkk])
        S.dma("sp", bt[:], b_ada[:, g * 256:(g + 1) * 256].partition_broadcast(128), writes=[bk])
        pst, pk = ps[g % 2], psk[g % 2]
        for kc in range(16):
            S.op("pe", lambda e, kc=kc: e.matmul(pst[:, 0:256], lhsT=R(scr[:, kc, :]), rhs=R(wb[:, kc, :]),
                                                 start=(kc == 0), stop=(kc == 15)),
                 reads=["scr", wkk], writes=[pk], nosync_self=True)
        S.op("dve", lambda e: e.tensor_tensor(out=bt[:], in0=pst[:, 0:256], in1=bt[:], op=ALU.add),
             reads=[pk, bk], writes=[bk])
        S.dma("pool", ada_d[:, g * 256:(g + 1) * 256], bt[0:1, :], reads=[bk], writes=["ada_d"])
    pop()
    if stage <= 1:
        return nc, S, es

    SIN = T("SIN", [128, NB, 56])
    COS = T("COS", [128, NB, 56])
    push()
    posi = T("posi", [128, NB], I32)
    posf = T("posf", [128, NB])
    invb = T("invb", [128, 56])
    ang = T("ang", [128, NB, 56])
    tmpa = T("tmpa", [128, NB, 56])
    tmpi = T("tmpi", [128, NB, 56], I32)
    tmpb = T("tmpb", [128, NB, 56])
    S.dma("sp", posi[:], pos_i, writes=["posi"])
    S.dma("sp", invb[:], invf.partition_broadcast(128), writes=["invb"])
    S.op("dve", lambda e: e.tensor_copy(out=posf[:], in_=posi[:]), reads=["posi"], writes=["posf"])
    for blk in range(NB):
        S.op("dve", lambda e, blk=blk: e.tensor_scalar(out=ang[:, blk, :], in0=invb[:], scalar1=posf[:, blk:blk + 1],
                                                       scalar2=None, op0=ALU.mult),
             reads=["posf", "invb"], writes=["ang"])

    def sin_table(dst, dkey, shift):
        S.op("dve", lambda e: e.tensor_scalar(out=tmpa[:], in0=ang[:], scalar1=1.0 / TWO_PI,
                                              scalar2=shift / TWO_PI + 0.5, op0=ALU.mult, op1=ALU.add),
             reads=["ang"], writes=["tmpa"])
        S.op("dve", lambda e: e.tensor_copy(out=tmpi[:], in_=tmpa[:]), reads=["tmpa"], writes=["tmpi"])
        S.op("dve", lambda e: e.tensor_copy(out=tmpa[:], in_=tmpi[:]), reads=["tmpi"], writes=["tmpa"])
        S.op("dve", lambda e: e.scalar_tensor_tensor(out=tmpb[:], in0=tmpa[:], scalar=-C1, in1=ang[:],
                                                     op0=ALU.mult, op1=ALU.add),
             reads=["tmpa", "ang"], writes=["tmpb"])
        S.op("dve", lambda e: e.scalar_tensor_tensor(out=tmpb[:], in0=tmpa[:], scalar=-C2, in1=tmpb[:],
                                                     op0=ALU.mult, op1=ALU.add),
             reads=["tmpa", "tmpb"], writes=["tmpb"])
        if shift != 0.0:
            S.op("dve", lambda e: e.tensor_scalar(out=tmpb[:], in0=tmpb[:], scalar1=shift, scalar2=None, op0=ALU.add),
                 reads=["tmpb"], writes=["tmpb"])
        S.op("dve", lambda e: e.tensor_scalar(out=tmpa[:], in0=tmpb[:], scalar1=np.pi, scalar2=-TWO_PI,
                                              op0=ALU.is_gt, op1=ALU.mult), reads=["tmpb"], writes=["tmpa"])
        S.op("dve", lambda e: e.tensor_tensor(out=tmpb[:], in0=tmpb[:], in1=tmpa[:], op=ALU.add),
             reads=["tmpa", "tmpb"], writes=["tmpb"])
        S.op("dve", lambda e: e.tensor_scalar(out=tmpa[:], in0=tmpb[:], scalar1=-np.pi, scalar2=TWO_PI,
                                              op0=ALU.is_lt, op1=ALU.mult), reads=["tmpb"], writes=["tmpa"])
        S.op("dve", lambda e: e.tensor_tensor(out=tmpb[:], in0=tmpb[:], in1=tmpa[:], op=ALU.add),
             reads=["tmpa", "tmpb"], writes=["tmpb"])
        S.op("dve", lambda e: e.tensor_scalar(out=tmpb[:], in0=tmpb[:], scalar1=3.1415925, scalar2=-3.1415925,
                                              op0=ALU.min, op1=ALU.max), reads=["tmpb"], writes=["tmpb"])
        S.op("act", lambda e: e.activation(out=dst[:], in_=tmpb[:], func=AF.Sin), reads=["tmpb"], writes=[dkey])

    sin_table(SIN, "SIN", 0.0)
    sin_table(COS, "COS", np.pi / 2)
    pop()
    FA, FI, FM = (0, 16), (16, 8), (24, 32)

    G = 4
    sh1 = T("sh1", [128, D])
    sc1 = T("sc1", [128, D])
    S.dma("sp", sh1[:], ada_d[:, 0:D].partition_broadcast(128), writes=["sh1"])
    S.dma("sp", sc1[:], ada_d[:, D:2 * D].partition_broadcast(128), writes=["sc1"])
    S.op("dve", lambda e: e.tensor_scalar(out=sc1[:], in0=sc1[:], scalar1=1.0, scalar2=None, op0=ALU.add),
         reads=["sc1"], writes=["sc1"])
    ikg_b = T("ikg_b", [128, 64]); ikb_b = T("ikb_b", [128, 64])
    qng_b = T("qng_b", [128, 512]); kvng_b = T("kvng_b", [128, 256])
    S.dma("sp", ikg_b[:], ikg.partition_broadcast(128), writes=["ikg_b"])
    S.dma("sp", ikb_b[:], ikb.partition_broadcast(128), writes=["ikb_b"])
    S.dma("sp", qng_b[:], qng.partition_broadcast(128), writes=["qng_b"])
    S.dma("sp", kvng_b[:], kvng.partition_broadcast(128), writes=["kvng_b"])

    xt = [T("xt%d" % i, [128, D]) for i in range(2)]
    hT = T("hT", [128, 16, G * 128])
    stg = [T("stg%d" % i, [128, 512]) for i in range(2)]
    stT = [T("stT%d" % i, [128, 4, G * 128]) for i in range(2)]
    qdst = T("qdst", [128, G, 512])
    cqT = T("cqT", [128, 4, G * 128])
    ckvT = T("ckvT", [128, 2, G * 128])
    absw = T("absw", [128, G, 16])
    sgn = T("sgn", [128, G, 16])
    st6 = T("st6", [128, 4, 6]); mv = T("mv", [128, 2]); rstd = T("rstd", [128, 1])
    sm = [T("sm%d" % i, [128, 4, 32]) for i in range(4)]
    IKT = T("IKT", [128, SEQ])
    KRT = T("KRT", [64, SEQ])
    ikst = T("ikst", [128, 128])
    w_in_v = w_in.rearrange("(kc p) f -> p kc f", p=128)
    wq_v = w_q_up.rearrange("(kc p) f -> p kc f", p=128)
    wkv_v = w_kv_up.rearrange("(kc p) f -> p kc f", p=128)
    cnt = dict(w=0, ps=0, stg=0, stT=0, x=0)

    def ln_stats(src_ap, n, skey):
        return _ln_stats(src_ap, n, skey)

    def _ln_stats(src_ap, n, skey):
        nch = max(1, n // 512)
        w_ = n // nch
        for c in range(nch):
            S.op("dve", lambda e, c=c: e.bn_stats(out=st6[:, c, :], in_=src_ap[:, c * w_:(c + 1) * w_]),
                 reads=[skey], writes=["st6"])
        S.op("dve", lambda e: e.bn_aggr(out=mv[:], in_=st6[:, 0:nch, :]), reads=["st6"], writes=["mv"])
        S.op("act", lambda e: e.activation(out=rstd[:], in_=mv[:, 1:2], func=AF.Ln, bias=eps_t[:], scale=1.0),
             reads=["mv", "eps"], writes=["rstd"])
        S.op("act", lambda e: e.activation(out=rstd[:], in_=rstd[:], func=AF.Exp, scale=-0.5),
             reads=["rstd"], writes=["rstd"])

    def rms_rstd(src_ap, n, skey, junk_ap, jkey):
        S.op("act", lambda e: e.activation(out=junk_ap, in_=src_ap, func=AF.Square, accum_out=mv[:, 0:1]),
             reads=[skey], writes=[jkey, "mv"])
        S.op("act", lambda e: e.activation(out=rstd[:], in_=mv[:, 0:1], func=AF.Ln, bias=eps_t[:], scale=1.0 / n),
             reads=["mv", "eps"], writes=["rstd"])
        S.op("act", lambda e: e.activation(out=rstd[:], in_=rstd[:], func=AF.Exp, scale=-0.5),
             reads=["rstd"], writes=["rstd"])

    def rope(dst, dkey, src, skey, H, off, half, blk, fslot):
        f0 = fslot[0]
        cosb = COS[:, blk, f0:f0 + half].unsqueeze(1).to_broadcast([128, H, half])
        sinb = SIN[:, blk, f0:f0 + half].unsqueeze(1).to_broadcast([128, H, half])
        x1 = src[:, :, off:off + half]
        x2 = src[:, :, off + half:off + 2 * half]
        t = [sm[i][:, 0:H, 0:half] for i in range(4)]
        S.op("dve", lambda e: e.tensor_tensor(out=t[0], in0=x1, in1=cosb, op=ALU.mult), reads=[skey, "COS"], writes=["sm0"])
        S.op("dve", lambda e: e.tensor_tensor(out=t[1], in0=x2, in1=sinb, op=ALU.mult), reads=[skey, "SIN"], writes=["sm1"])
        S.op("dve", lambda e: e.tensor_tensor(out=t[2], in0=x2, in1=cosb, op=ALU.mult), reads=[skey, "COS"], writes=["sm2"])
        S.op("dve", lambda e: e.tensor_tensor(out=t[3], in0=x1, in1=sinb, op=ALU.mult), reads=[skey, "SIN"], writes=["sm3"])
        S.op("dve", lambda e: e.tensor_tensor(out=dst[:, :, off:off + half], in0=t[0], in1=t[1], op=ALU.subtract),
             reads=["sm0", "sm1"], writes=[dkey])
        S.op("dve", lambda e: e.tensor_tensor(out=dst[:, :, off + half:off + 2 * half], in0=t[2], in1=t[3], op=ALU.add),
             reads=["sm2", "sm3"], writes=[dkey])

    def transpose_to(dst_ap, dkey, src_ap, skey, ncol):
        i = cnt["ps"] % 4 + 4
        cnt["ps"] += 1
        S.op("pe", lambda e: e.transpose(out=ps[i][0:ncol, 0:128], in_=src_ap, identity=ident[:]),
             reads=[skey, "ident"], writes=[psk[i]])
        S.op("act", lambda e: e.copy(out=R(dst_ap), in_=ps[i][0:ncol, 0:128]), reads=[psk[i]], writes=[dkey])

    def proj_block(lhs_tile, lkey, nkc, wtile, wkey, ncols, bi):
        i = cnt["ps"] % 4
        cnt["ps"] += 1
        for kc in range(nkc):
            S.op("pe", lambda e, kc=kc: e.matmul(ps[i][:, 0:ncols], lhsT=R(lhs_tile[:, kc, bi * 128:(bi + 1) * 128]),
                                                 rhs=R(wtile[:, kc, 0:ncols]), start=(kc == 0), stop=(kc == nkc - 1)),
                 reads=[lkey, wkey], writes=[psk[i]], nosync_self=True)
        return ps[i], psk[i]

    def load_w(view, c0, ncols, nkc):
        i = cnt["w"] % len(wbuf)
        cnt["w"] += 1
        wt = wbuf[i]
        flat = wt[:].rearrange("p a b -> p (a b)")
        dst = flat[:, 0:nkc * ncols].rearrange("p (a b) -> p a b", a=nkc)
        S.dma("sp", R(dst), view[:, 0:nkc, c0:c0 + ncols], writes=[wk[i]])
        return dst, wk[i]


    ngroups = NB // G
    tglist = list(range(int(os.environ.get("KDBG_TG", ngroups))))
    if os.environ.get("KDBG_TGLIST"):
        tglist = [int(v) for v in os.environ["KDBG_TGLIST"].split(",")]
    for tg in tglist:
        own = tg < (NOWN // G)
        for bi in range(G):
            blk = tg * G + bi
            xi = cnt["x"] % 2
            cnt["x"] += 1
            xb, xk = xt[xi], "xt%d" % xi
            S.dma("sp", xb[:], xl[blk * 128:(blk + 1) * 128, :], writes=[xk])
            ln_stats(xb, D, xk)
            S.op("dve", lambda e: e.tensor_scalar(out=xb[:], in0=xb[:], scalar1=mv[:, 0:1], scalar2=rstd[:],
                                                  op0=ALU.subtract, op1=ALU.mult), reads=[xk, "mv", "rstd"], writes=[xk])
            S.op("pool", lambda e: e.tensor_tensor(out=xb[:], in0=xb[:], in1=sc1[:], op=ALU.mult),
                 reads=[xk, "sc1"], writes=[xk])
            S.op("pool", lambda e: e.tensor_tensor(out=xb[:], in0=xb[:], in1=sh1[:], op=ALU.add),
                 reads=[xk, "sh1"], writes=[xk])
            for kc in range(16):
                transpose_to(hT[:, kc, bi * 128:(bi + 1) * 128], "hT", xb[:, kc * 128:(kc + 1) * 128], xk, 128)

        def stage_out(kind, sub):
            pass

        def run_group(kind, c0, ncols, sub):
            if os.environ.get("KDBG_KINDS") and kind not in os.environ["KDBG_KINDS"].split(","):
                return
            if kind == "qup":
                wt, wkey = load_w(wq_v, c0, ncols, 4)
                lhs, lkey, nkc = cqT, "cqT", 4
            elif kind == "kvup":
                wt, wkey = load_w(wkv_v, c0, ncols, 2)
                lhs, lkey, nkc = ckvT, "ckvT", 2
            else:
                wt, wkey = load_w(w_in_v, c0, ncols, 16)
                lhs, lkey, nkc = hT, "hT", 16
            si = cnt["stT"] % 2
            cnt["stT"] += 1
            sT, sTk = stT[si], "stT%d" % si
            for bi in range(G):
                blk = tg * G + bi
                pt, pk = proj_block(lhs, lkey, nkc, wt, wkey, ncols, bi)
                gi = cnt["stg"] % 2
                cnt["stg"] += 1
                sg, sgk = stg[gi], "stg%d" % gi
                if kind in ("aq", "ak"):
                    S.op("act", lambda e: e.copy(out=sg[:, 0:256], in_=pt[:, 0:256]), reads=[pk], writes=[sgk])
                    v = sg[:, 0:256].rearrange("p (h d) -> p h d", h=2)
                    pv = pt[:, 0:256].rearrange("p (h d) -> p h d", h=2)
                    rope(v, sgk, v, sgk, 2, 0, 16, blk, FA)
                    for h in range(2):
                        transpose_to(sT[:, h, bi * 128:(bi + 1) * 128], sTk, sg[:, h * 128:(h + 1) * 128], sgk, 128)
                elif kind == "av":
                    S.op("act", lambda e: e.copy(out=sg[:, 0:256], in_=pt[:, 0:256]), reads=[pk], writes=[sgk])
                    S.dma("pool", AV[blk * 128:(blk + 1) * 128, sub * 256:(sub + 1) * 256], sg[:, 0:256], reads=[sgk], writes=["AV"])
                elif kind == "iq":
                    S.op("act", lambda e: e.copy(out=sg[:, 0:256], in_=pt[:, 0:256]), reads=[pk], writes=[sgk])
                    v = sg[:, 0:256].rearrange("p (h d) -> p h d", h=4)
                    pv = pt[:, 0:256].rearrange("p (h d) -> p h d", h=4)
                    rope(v, sgk, v, sgk, 4, 0, 8, blk, FI)
                    S.op("dve", lambda e: e.tensor_tensor(out=v, in0=v, in1=absw[:, bi, sub * 4:(sub + 1) * 4].unsqueeze(2).to_broadcast([128, 4, 64]),
                                                          op=ALU.mult), reads=[sgk, "absw"], writes=[sgk])
                    for h in range(2):
                        transpose_to(sT[:, h, bi * 128:(bi + 1) * 128], sTk, sg[:, h * 128:(h + 1) * 128], sgk, 128)
                elif kind == "misc":
                    S.op("act", lambda e: e.copy(out=sg[:, 0:80], in_=pt[:, 0:80]), reads=[pk], writes=[sgk])
                    ln_stats(sg[:, 0:64], 64, sgk)
                    S.op("dve", lambda e: e.tensor_scalar(out=sg[:, 0:64], in0=sg[:, 0:64], scalar1=mv[:, 0:1], scalar2=rstd[:],
                                                          op0=ALU.subtract, op1=ALU.mult), reads=[sgk, "mv", "rstd"], writes=[sgk])
                    S.op("dve", lambda e: e.tensor_tensor(out=sg[:, 0:64], in0=sg[:, 0:64], in1=ikg_b[:], op=ALU.mult),
                         reads=[sgk, "ikg_b"], writes=[sgk])
                    S.op("dve", lambda e: e.tensor_tensor(out=sg[:, 128:192], in0=sg[:, 0:64], in1=ikb_b[:], op=ALU.add),
                         reads=[sgk, "ikb_b"], writes=[sgk])
                    v = sg[:, 128:192].rearrange("p (h d) -> p h d", h=1)
                    rope(v, sgk, v, sgk, 1, 0, 8, blk, FI)
                    S.op("dve", lambda e: e.tensor_copy(out=sg[:, 192:256], in_=sg[:, 128:192]), reads=[sgk], writes=[sgk])
                    transpose_to(IKT[:, blk * 128:(blk + 1) * 128], "IKT", sg[:, 128:256], sgk, 128)
                    if own:
                        S.op("act", lambda e: e.activation(out=absw[:, bi, :], in_=sg[:, 64:80], func=AF.Abs),
                             reads=[sgk], writes=["absw"])
                        S.op("dve", lambda e: e.tensor_scalar(out=sgn[:, bi, :], in0=sg[:, 64:80], scalar1=0.0, scalar2=-0.5,
                                                              op0=ALU.is_ge, op1=ALU.add), reads=[sgk], writes=["sgn"])
                        S.dma("pool", SGN_d[blk * 128:(blk + 1) * 128, :], sgn[:, bi, :], reads=["sgn"], writes=["SGN_d"])
                elif kind == "qd":
                    S.op("act", lambda e: e.copy(out=qdst[:, bi, sub * 256:(sub + 1) * 256], in_=pt[:, 0:256]),
                         reads=[pk], writes=["qdst"])
                    if sub == 1:
                        rms_rstd(qdst[:, bi, :], 512, "qdst", sg[:, 0:512], sgk)
                        S.op("dve", lambda e: e.scalar_tensor_tensor(out=qdst[:, bi, :], in0=qdst[:, bi, :], scalar=rstd[:],
                                                                     in1=qng_b[:], op0=ALU.mult, op1=ALU.mult),
                             reads=["qdst", "rstd", "qng_b"], writes=["qdst"])
                        for c in range(4):
                            transpose_to(cqT[:, c, bi * 128:(bi + 1) * 128], "cqT", qdst[:, bi, c * 128:(c + 1) * 128], "qdst", 128)
                elif kind == "kvd":
                    S.op("act", lambda e: e.copy(out=sg[:, 0:256], in_=pt[:, 0:256]), reads=[pk], writes=[sgk])
                    rms_rstd(sg[:, 0:256], 256, sgk, sg[:, 256:512], sgk)
                    S.op("dve", lambda e: e.scalar_tensor_tensor(out=sg[:, 0:256], in0=sg[:, 0:256], scalar=rstd[:],
                                                                 in1=kvng_b[:], op0=ALU.mult, op1=ALU.mult),
                         reads=[sgk, "rstd", "kvng_b"], writes=[sgk])
                    for c in range(2):
                        transpose_to(ckvT[:, c, bi * 128:(bi + 1) * 128], "ckvT", sg[:, c * 128:(c + 1) * 128], sgk, 128)
                elif kind == "kr":
                    S.op("act", lambda e: e.copy(out=sg[:, 0:64], in_=pt[:, 0:64]), reads=[pk], writes=[sgk])
                    v = sg[:, 0:64].rearrange("p (h d) -> p h d", h=1)
                    rope(v, sgk, v, sgk, 1, 0, 32, blk, FM)
                    transpose_to(KRT[:, blk * 128:(blk + 1) * 128], "KRT", sg[:, 0:64], sgk, 64)
                elif kind == "qup":
                    S.op("act", lambda e: e.copy(out=sg[:, 0:384], in_=pt[:, 0:384]), reads=[pk], writes=[sgk])
                    v = sg[:, 0:384].rearrange("p (h d) -> p h d", h=2)
                    pv = pt[:, 0:384].rearrange("p (h d) -> p h d", h=2)
                    rope(v, sgk, v, sgk, 2, 128, 32, blk, FM)
                    for h in range(2):
                        transpose_to(sT[:, h, bi * 128:(bi + 1) * 128], sTk, sg[:, h * 192:h * 192 + 128], sgk, 128)
                        transpose_to(sT[0:64, 2 + h, bi * 128:(bi + 1) * 128], sTk, sg[:, h * 192 + 128:(h + 1) * 192], sgk, 64)
                elif kind == "kvup":
                    S.op("act", lambda e: e.copy(out=sg[:, 0:256], in_=pt[:, 0:256]), reads=[pk], writes=[sgk])
                    transpose_to(sT[:, 0, bi * 128:(bi + 1) * 128], sTk, sg[:, 0:128], sgk, 128)
                    S.dma("pool", VB[blk * 128:(blk + 1) * 128, sub * 128:(sub + 1) * 128], sg[:, 128:256], reads=[sgk], writes=["VB"])
            t0 = tg * G * 128
            tw = G * 128
            if kind == "ak":
                for h in range(2):
                    S.dma("pool", AKT[sub * 2 + h, :, t0:t0 + tw], sT[:, h, :], reads=[sTk], writes=["AKT"])
            elif kind == "aq":
                for h in range(2):
                    S.dma("pool", AQT[sub * 2 + h, :, t0:t0 + tw], sT[:, h, :], reads=[sTk], writes=["AQT"])
            elif kind == "iq":
                for h in range(2):
                    S.dma("pool", IQT[sub * 2 + h, :, t0:t0 + tw], sT[:, h, :], reads=[sTk], writes=["IQT"])
            elif kind == "qup":
                for h in range(2):
                    S.dma("pool", QBN[sub * 2 + h, :, t0:t0 + tw], sT[:, h, :], reads=[sTk], writes=["QBN"])
                    S.dma("pool", QBR[sub * 2 + h, :, t0:t0 + tw], sT[0:64, 2 + h, :], reads=[sTk], writes=["QBR"])
            elif kind == "kvup":
                S.dma("pool", KBT[sub, :, t0:t0 + tw], sT[:, 0, :], reads=[sTk], writes=["KBT"])

        run_group("misc", 4096, 80, 0)
        for s_ in range(4):
            run_group("ak", 1024 + s_ * 256, 256, s_)
        for s_ in range(4):
            run_group("av", 2048 + s_ * 256, 256, s_)
        run_group("kvd", 4688, 256, 0)
        run_group("kr", 4944, 64, 0)
        for s_ in range(8):
            run_group("kvup", s_ * 256, 256, s_)
        if own:
            for s_ in range(4):
                run_group("aq", s_ * 256, 256, s_)
            for s_ in range(4):
                run_group("iq", 3072 + s_ * 256, 256, s_)
            for s_ in range(2):
                run_group("qd", 4176 + s_ * 256, 256, s_)
            for s_ in range(4):
                run_group("qup", s_ * 384, 384, s_)
    for tg in tglist:
        S.dma("pool", IKT_d[:, tg * 512:(tg + 1) * 512], IKT[:, tg * 512:(tg + 1) * 512], reads=["IKT"], writes=["IKT_d"])
        S.dma("pool", KRT_d[:, tg * 512:(tg + 1) * 512], KRT[:, tg * 512:(tg + 1) * 512], reads=["KRT"], writes=["KRT_d"])
    pop()
    if stage <= 2:
        return nc, S, es

    SCALE_A = 128.0 ** -0.5
    SCALE_B = 192.0 ** -0.5
    BF = BF16

    def attn_core(j, h, kT, kTk, vt, vk, qT_ap, qkey, scale, OT, okey, mask_fn, extra_qk=None, defer=None):
        NKB = 2 * j + 2
        blocks = [(part, kb) for part in range(2) for kb in range(0, NKB, 2)]
        for idx, (part, kb) in enumerate(blocks):
            pi = cnt["ps"] % 4
            cnt["ps"] += 1
            for t in range(2):
                kcol = (kb + t) * 128
                S.op("pe", lambda e, t=t, kcol=kcol: e.matmul(ps[pi][:, t * 256:(t + 1) * 256], lhsT=R(kT[part][:, kcol:kcol + 128]),
                                                              rhs=R(qT_ap), start=True, stop=(extra_qk is None)),
                     reads=[kTk[part], qkey], writes=[psk[pi]], nosync_self=True)
                if extra_qk is not None:
                    kr_ap, krkey, qr_ap, qrkey = extra_qk
                    S.op("pe", lambda e, t=t, kcol=kcol: e.matmul(ps[pi][:, t * 256:(t + 1) * 256],
                                                                  lhsT=R(kr_ap[0:64, part * 2048 + kcol:part * 2048 + kcol + 128]),
                                                                  rhs=R(qr_ap), start=False, stop=True),
                         reads=[krkey, qrkey], writes=[psk[pi]], nosync_self=True)
            pti = cnt["pt"] % 2
            cnt["pt"] += 1
            pt, ptk = ptl[pti], "pt%d" % pti
            m_ap = mask_fn(part, kb)
            if m_ap is None:
                S.op("act", lambda e: e.activation(out=R(pt[:]), in_=ps[pi][:, 0:512], func=AF.Exp, scale=scale),
                     reads=[psk[pi]], writes=[ptk])
                src, srck = pt, ptk
            else:
                S.op("act", lambda e: e.activation(out=R(pt[:]), in_=ps[pi][:, 0:512], func=AF.Exp, scale=scale),
                     reads=[psk[pi]], writes=[ptk])
                pm, pmk = ptm[pti], "ptm%d" % pti
                eng = "pool" if (defer is not None or idx % 2 == 1) else "dve"
                S.op(eng, lambda e: e.tensor_tensor(out=R(pm[:]), in0=pt[:], in1=m_ap, op=ALU.mult),
                     reads=[ptk, "mskT"], writes=[pmk])
                src, srck = pm, pmk
            last = idx == len(blocks) - 1
            for t in range(2):
                S.op("pe", lambda e, t=t: e.matmul(ps[6][:, 0:256], lhsT=R(vt[part][:, kb + t, :]), rhs=R(src[:, t * 256:(t + 1) * 256]),
                                                   start=(idx == 0 and t == 0), stop=(last and t == 1)),
                     reads=[vk[part], srck], writes=[psk[6]], nosync_self=True)
                S.op("pe", lambda e, t=t: e.matmul(ps[7][:, 0:256], lhsT=R(ones[:]), rhs=R(src[:, t * 256:(t + 1) * 256]),
                                                   start=(idx == 0 and t == 0), stop=(last and t == 1)),
                     reads=["ones", srck], writes=[psk[7]], nosync_self=True)
        if defer is not None:
            DEN, dkey = defer
            S.op("act", lambda e: e.copy(out=OT[:, h, :], in_=ps[6][:, 0:256]), reads=[psk[6]], writes=[okey])
            S.op("act", lambda e: e.copy(out=DEN[:, h, :], in_=ps[7][:, 0:256]), reads=[psk[7]], writes=[dkey])
            return
        S.op("dve", lambda e: e.reciprocal(out=rden[:], in_=ps[7][:, 0:256]), reads=[psk[7]], writes=["rden"])
        S.op("dve", lambda e: e.tensor_tensor(out=OT[:, h, :], in0=ps[6][:, 0:256], in1=rden[:], op=ALU.mult),
             reads=[psk[6], "rden"], writes=[okey])

    def out_norm(j, OT, okey, goff):
        q0 = j * 256
        for h in range(8):
            S.op("pool", lambda e, h=h: e.tensor_tensor(out=R(sq[:, h, :]), in0=OT[:, h, :], in1=OT[:, h, :], op=ALU.mult),
                 reads=[okey], writes=["sq"])
        for h in range(8):
            S.op("pe", lambda e, h=h: e.matmul(ps[5][:, 0:256], lhsT=R(ones[:]), rhs=R(sq[:, h, :]), start=(h == 0), stop=(h == 7)),
                 reads=["ones", "sq"], writes=[psk[5]], nosync_self=True)
        S.op("act", lambda e: e.activation(out=rden[:], in_=ps[5][:, 0:256], func=AF.Ln, bias=eps_t[:], scale=1.0 / 1024.0),
             reads=[psk[5], "eps"], writes=["rden"])
        S.op("act", lambda e: e.activation(out=rden[:], in_=rden[:], func=AF.Exp, scale=-0.5), reads=["rden"], writes=["rden"])
        for h in range(8):
            S.op("dve", lambda e, h=h: e.scalar_tensor_tensor(out=OT[:, h, :], in0=OT[:, h, :], scalar=ong[:, goff + h:goff + h + 1],
                                                              in1=rden[:], op0=ALU.mult, op1=ALU.mult),
                 reads=[okey, "ong", "rden"], writes=[okey])
        S.dma("pool", MRG[:, goff:goff + 8, q0:q0 + 256], OT[:], reads=[okey], writes=["MRG"])

    cnt["pt"] = 0
    push()
    IKT2 = T("IKT2", [128, SEQ])
    for tg in tglist:
        S.dma("sp", IKT2[:, tg * 512:(tg + 1) * 512], IKT_d[:, tg * 512:(tg + 1) * 512], writes=["IKT2"])
    ong = T("ong", [128, 16])
    S.dma("sp", ong[:], ong_fm, writes=["ong"])
    identb = T("identb", [128, 128], BF)
    S.op("dve", lambda e: e.tensor_copy(out=identb[:], in_=ident[:]), reads=["ident"], writes=["identb"])
    diagb = T("diagb", [128, 128]); rbias = T("rbias", [128, 128])
    S.dma("sp", diagb[:], diagb_d, writes=["diagb"])
    S.dma("sp", rbias[:], rbias_d, writes=["rbias"])
    Isc = T("Isc", [128, 4096])
    work = T("work", [128, 4096])
    msk = T("msk", [128, 4096], BF)
    mskT = T("mskT", [128, 2, 16, 256], BF)
    iqT = T("iqT", [128, 8, 256])
    aqT = T("aqT", [128, 8, 256])
    sgnq = T("sgnq", [128, 2, 16])
    tmpr = [T("tmpr%d" % i, [128, 512]) for i in range(2)]
    m8 = T("m8", [128, 8]); thr = T("thr", [128, 1])
    kT = [T("kT%d" % i, [128, 2048]) for i in range(2)]
    vt = [T("vt%d" % i, [128, 16, 128]) for i in range(2)]
    kTk = ["kT0", "kT1"]; vk = ["vt0", "vt1"]
    ptl = [T("pt%d" % i, [128, 512]) for i in range(2)]
    ptm = [T("ptm%d" % i, [128, 512]) for i in range(2)]
    OT = T("OT", [128, 8, 256])
    sq = T("sq", [128, 8, 256])
    rden = T("rden", [128, 256])
    KRT2 = T("KRT2", [64, SEQ])
    for tg in tglist:
        S.dma("sp", R(KRT2[:, tg * 512:(tg + 1) * 512]), R(KRT_d[:, tg * 512:(tg + 1) * 512]), writes=["KRT2"])
    mlam = T("mlam", [128, 4, 256])
    S.dma("sp", mlam[:], mlam_d.rearrange("p (a b) -> p a b", a=4), writes=["mskT"])
    qbn = T("qbn", [128, 8, 256])
    qbr = T("qbr", [64, 8, 256])
    OTB = T("OTB", [128, 8, 256])
    DEN = T("DEN", [128, 8, 256])
    npairs = int(os.environ.get("KDBG_NPAIR", 8))
    for j in range(npairs):
        q0 = j * 256
        NKB = 2 * j + 2
        S.dma("sp", R(qbn[:]), R(QBN[:, :, q0:q0 + 256]).rearrange("h p q -> p h q"), writes=["qbn"])
        S.dma("sp", R(qbr[:]), R(QBR[:, :, q0:q0 + 256]).rearrange("h p q -> p h q"), writes=["qbr"])
        S.dma("sp", iqT[:], IQT[:, :, q0:q0 + 256].rearrange("h p q -> p h q"), writes=["iqT"])
        S.dma("sp", R(aqT[:]), R(AQT[:, :, q0:q0 + 256]).rearrange("h p q -> p h q"), writes=["aqT"])
        S.dma("sp", sgnq[:], SGN_d[q0:q0 + 256, :].rearrange("(b p) h -> p b h", p=128), writes=["sgnq"])
        for part in range(2):
            S.op("pool", lambda e, part=part: e.memset(mskT[:, part, NKB - 1, 0:128], 0.0), writes=["mskT"])
        for qb in range(2):
            i = 2 * j + qb
            nk = i + 1
            for part in range(2):
                for c0 in range(0, nk * 128, 512):
                    cw = min(512, nk * 128 - c0)
                    for h in range(16):
                        pi = cnt["ps"] % 4
                        cnt["ps"] += 1
                        p0 = (h % 2) * 64
                        S.op("pe", lambda e, h=h, p0=p0: e.matmul(ps[pi][:, 0:cw], lhsT=iqT[p0:p0 + 64, h // 2, qb * 128:(qb + 1) * 128],
                                                                  rhs=IKT2[p0:p0 + 64, part * 2048 + c0:part * 2048 + c0 + cw],
                                                                  start=True, stop=True),
                             reads=["iqT", "IKT2"], writes=[psk[pi]], nosync_self=True)
                        ti = cnt["pt"] % 2
                        cnt["pt"] += 1
                        S.op("act", lambda e: e.activation(out=tmpr[ti][:, 0:cw], in_=ps[pi][:, 0:cw], func=AF.Relu),
                             reads=[psk[pi]], writes=["tmpr%d" % ti])
                        if h == 0:
                            S.op("dve", lambda e: e.tensor_scalar(out=Isc[:, part * nk * 128 + c0:part * nk * 128 + c0 + cw], in0=tmpr[ti][:, 0:cw],
                                                                  scalar1=sgnq[:, qb, 0:1], scalar2=None, op0=ALU.mult),
                                 reads=["tmpr%d" % ti, "sgnq"], writes=["Isc"])
                        else:
                            S.op("dve", lambda e, h=h: e.scalar_tensor_tensor(out=Isc[:, part * nk * 128 + c0:part * nk * 128 + c0 + cw], in0=tmpr[ti][:, 0:cw],
                                                                              scalar=sgnq[:, qb, h:h + 1], in1=Isc[:, part * nk * 128 + c0:part * nk * 128 + c0 + cw],
                                                                              op0=ALU.mult, op1=ALU.add),
                                 reads=["tmpr%d" % ti, "sgnq", "Isc"], writes=["Isc"])
            S.op("dve", lambda e: e.tensor_tensor(out=Isc[:, i * 128:(i + 1) * 128], in0=Isc[:, i * 128:(i + 1) * 128],
                                                  in1=diagb[:], op=ALU.add), reads=["Isc", "diagb"], writes=["Isc"])
            S.op("dve", lambda e: e.tensor_tensor(out=Isc[:, nk * 128 + i * 128:nk * 128 + (i + 1) * 128],
                                                  in0=Isc[:, nk * 128 + i * 128:nk * 128 + (i + 1) * 128],
                                                  in1=rbias[:], op=ALU.add), reads=["Isc", "rbias"], writes=["Isc"])
            for h in range(4 * qb, 4 * qb + 4):
                for part in range(2):
                    S.dma("sp", R(kT[part][:, 0:NKB * 128]), R(KBT[h, :, part * 2048:part * 2048 + NKB * 128]), writes=[kTk[part]])
                    S.dma("sp", R(vt[part][:, 0:NKB, :]),
                          R(VB[part * 2048:part * 2048 + NKB * 128, h * 128:(h + 1) * 128]).rearrange("(kb p) d -> p kb d", p=128),
                          writes=[vk[part]])
                attn_core(j, h, kT, kTk, vt, vk, qbn[:, h, :], "qbn", SCALE_B, OTB, "OTB",
                          lambda part, kb, NKB=NKB: (mlam[:, part * 2:part * 2 + 2, :].rearrange("p a b -> p (a b)")
                                                     if kb == NKB - 2 else None),
                          extra_qk=(KRT2, "KRT2", qbr[:, h, :], "qbr"), defer=(DEN, "DEN"))
            Iv = Isc[:, 0:2 * nk * 128]
            Wv = work[:, 0:2 * nk * 128]
            if i == 0:
                S.op("dve", lambda e: e.memset(thr[:], -1.0e29), writes=["thr"])
            else:
                for it in range(32):
                    src = Iv if it == 0 else Wv
                    S.op("dve", lambda e: e.max(out=m8[:], in_=src), reads=["Isc", "work"], writes=["m8"])
                    if it < 31:
                        S.op("dve", lambda e: e.match_replace(out=Wv, in_to_replace=m8[:], in_values=src, imm_value=NEG),
                             reads=["Isc", "work", "m8"], writes=["work"])
                S.op("dve", lambda e: e.tensor_scalar(out=thr[:], in0=m8[:, 7:8], scalar1=-1.0e29, scalar2=None, op0=ALU.max),
                     reads=["m8"], writes=["thr"])
            S.op("dve", lambda e: e.tensor_scalar(out=msk[:, 0:2 * nk * 128], in0=Iv, scalar1=thr[:], scalar2=None, op0=ALU.is_ge),
                 reads=["Isc", "thr"], writes=["msk"])
            for part in range(2):
                for kb in range(nk):
                    pi = cnt["ps"] % 4
                    cnt["ps"] += 1
                    pb = ps[pi][:].bitcast(BF)
                    S.op("pe", lambda e: e.transpose(out=pb[:, 0:128], in_=msk[:, (part * nk + kb) * 128:(part * nk + kb + 1) * 128], identity=identb[:]),
                         reads=["msk", "identb"], writes=[psk[pi]])
                    S.op("act", lambda e: e.copy(out=mskT[:, part, kb, qb * 128:(qb + 1) * 128], in_=pb[:, 0:128]),
                         reads=[psk[pi]], writes=["mskT"])
        for h in range(8):
            for part in range(2):
                S.dma("sp", R(kT[part][:, 0:NKB * 128]), R(AKT[h, :, part * 2048:part * 2048 + NKB * 128]), writes=[kTk[part]])
                S.dma("sp", R(vt[part][:, 0:NKB, :]),
                      R(AV[part * 2048:part * 2048 + NKB * 128, h * 128:(h + 1) * 128]).rearrange("(kb p) d -> p kb d", p=128),
                      writes=[vk[part]])
            attn_core(j, h, kT, kTk, vt, vk, aqT[:, h, :], "aqT", SCALE_A, OT, "OT",
                      lambda part, kb: mskT[:, part, kb:kb + 2, :].rearrange("p a b -> p (a b)"))
        out_norm(j, OT, "OT", 0)
        S.op("dve", lambda e: e.reciprocal(out=DEN[:], in_=DEN[:]), reads=["DEN"], writes=["DEN"])
        S.op("pool", lambda e: e.tensor_tensor(out=OTB[:], in0=OTB[:], in1=DEN[:], op=ALU.mult), reads=["OTB", "DEN"], writes=["OTB"])
        out_norm(j, OTB, "OTB", 8)
    pop()
    if stage <= 4:
        return nc, S, es

    push()
    wbuf = [T("wbuf%d" % i, [128, 16, 256]) for i in range(3)]
    bc = {}
    for nm, src in [("g1", ada_d[:, 2 * D:3 * D]), ("sh2", ada_d[:, 3 * D:4 * D]), ("sc2", ada_d[:, 4 * D:5 * D]),
                    ("lnmg", lnmg), ("lnmb", lnmb)]:
        bc[nm] = T("bc_" + nm, [128, D])
        S.dma("sp", bc[nm][:], src.partition_broadcast(128), writes=["bc_" + nm])
    S.op("dve", lambda e: e.tensor_scalar(out=bc["sc2"][:], in0=bc["sc2"][:], scalar1=1.0, scalar2=None, op0=ALU.add),
         reads=["bc_sc2"], writes=["bc_sc2"])
    mT = T("mT", [128, 16, 256])
    ymix = [T("ymix%d" % i, [128, D]) for i in range(2)]
    xt = [T("xt%d" % i, [128, D]) for i in range(2)]
    h2b = T("h2b", [128, 16, 128])
    wr = T("wr", [128, 16, NEXP])
    S.dma("sp", R(wr[:]), w_router.rearrange("(kc p) e -> p kc e", p=128), writes=["wr"])
    brt = T("brt", [128, NEXP])
    S.dma("sp", brt[:], b_router.partition_broadcast(128), writes=["brt"])
    lg = T("lg", [128, NEXP]); ex = T("ex", [128, NEXP]); mk = T("mk", [128, NEXP])
    m8 = T("m8", [128, 8]); nmx = T("nmx", [128, 1]); rs = T("rs", [128, 1])
    gts = T("gts", [128, 128])
    st6 = T("st6", [128, 4, 6]); mv = T("mv", [128, 2]); rstd = T("rstd", [128, 1])
    w_out_v = w_out.rearrange("(kc p) f -> p kc f", p=128)
    for j in range(npairs):
        q0 = j * 256
        S.dma("sp", R(mT[:]), R(MRG[:, :, q0:q0 + 256]), writes=["mT"])
        for tb in range(2):
            S.dma("sp", xt[tb][:], xl[q0 + tb * 128:q0 + (tb + 1) * 128, :], writes=["xt%d" % tb])
        for cg in range(8):
            wt, wkey = load_w(w_out_v, cg * 256, 256, 16)
            for tb in range(2):
                pt_, pk = proj_block(mT, "mT", 16, wt, wkey, 256, tb)
                S.op("dve", lambda e: e.tensor_tensor(out=ymix[tb][:, cg * 256:(cg + 1) * 256], in0=pt_[:, 0:256],
                                                      in1=bc["g1"][:, cg * 256:(cg + 1) * 256], op=ALU.mult),
                     reads=[pk, "bc_g1"], writes=["ymix%d" % tb])
        for tb in range(2):
            blk = 2 * j + tb
            xb, xk = xt[tb], "xt%d" % tb
            ym, yk = ymix[tb], "ymix%d" % tb
            S.op("dve", lambda e: e.scalar_tensor_tensor(out=ym[:], in0=xb[:], scalar=ALPHA, in1=ym[:], op0=ALU.mult, op1=ALU.add),
                 reads=[xk, yk], writes=[yk])
            ln_stats2 = lambda src, key: _ln_stats(src, D, key)
            _ln_stats(ym, D, yk)
            S.op("dve", lambda e: e.tensor_scalar(out=ym[:], in0=ym[:], scalar1=mv[:, 0:1], scalar2=rstd[:],
                                                  op0=ALU.subtract, op1=ALU.mult), reads=[yk, "mv", "rstd"], writes=[yk])
            S.op("pool", lambda e: e.tensor_tensor(out=ym[:], in0=ym[:], in1=bc["lnmg"][:], op=ALU.mult),
                 reads=[yk, "bc_lnmg"], writes=[yk])
            S.op("pool", lambda e: e.tensor_tensor(out=ym[:], in0=ym[:], in1=bc["lnmb"][:], op=ALU.add),
                 reads=[yk, "bc_lnmb"], writes=[yk])
            S.dma("pool", X1[blk * 128:(blk + 1) * 128, :], ym[:], reads=[yk], writes=["X1"])
            _ln_stats(ym, D, yk)
            S.op("dve", lambda e: e.tensor_scalar(out=xb[:], in0=ym[:], scalar1=mv[:, 0:1], scalar2=rstd[:],
                                                  op0=ALU.subtract, op1=ALU.mult), reads=[yk, "mv", "rstd"], writes=[xk])
            S.op("pool", lambda e: e.tensor_tensor(out=xb[:], in0=xb[:], in1=bc["sc2"][:], op=ALU.mult),
                 reads=[xk, "bc_sc2"], writes=[xk])
            S.op("pool", lambda e: e.tensor_tensor(out=xb[:], in0=xb[:], in1=bc["sh2"][:], op=ALU.add),
                 reads=[xk, "bc_sh2"], writes=[xk])
            for kc in range(16):
                transpose_to(h2b[:, kc, :], "h2b", xb[:, kc * 128:(kc + 1) * 128], xk, 128)
            S.dma("pool", H2T[:, :, blk * 128:(blk + 1) * 128], h2b[:], reads=["h2b"], writes=["H2T"])
            pi = cnt["ps"] % 4
            cnt["ps"] += 1
            for kc in range(16):
                S.op("pe", lambda e, kc=kc: e.matmul(ps[pi][:, 0:NEXP], lhsT=R(h2b[:, kc, :]), rhs=R(wr[:, kc, :]),
                                                     start=(kc == 0), stop=(kc == 15)),
                     reads=["h2b", "wr"], writes=[psk[pi]], nosync_self=True)
            S.op("dve", lambda e: e.tensor_tensor(out=lg[:], in0=ps[pi][:, 0:NEXP], in1=brt[:], op=ALU.add),
                 reads=[psk[pi], "brt"], writes=["lg"])
            S.op("dve", lambda e: e.max(out=m8[:], in_=lg[:]), reads=["lg"], writes=["m8"])
            S.op("dve", lambda e: e.tensor_scalar(out=nmx[:], in0=m8[:, 0:1], scalar1=-1.0, scalar2=None, op0=ALU.mult),
                 reads=["m8"], writes=["nmx"])
            S.op("act", lambda e: e.activation(out=ex[:], in_=lg[:], func=AF.Exp, bias=nmx[:], scale=1.0),
                 reads=["lg", "nmx"], writes=["ex"])
            S.op("dve", lambda e: e.tensor_scalar(out=mk[:], in0=lg[:], scalar1=m8[:, 3:4], scalar2=None, op0=ALU.is_ge),
                 reads=["lg", "m8"], writes=["mk"])
            S.op("dve", lambda e: e.tensor_tensor(out=ex[:], in0=ex[:], in1=mk[:], op=ALU.mult), reads=["ex", "mk"], writes=["ex"])
            S.op("dve", lambda e: e.reduce_sum(out=rs[:], in_=ex[:], axis=AX.X), reads=["ex"], writes=["rs"])
            S.op("dve", lambda e: e.reciprocal(out=rs[:], in_=rs[:]), reads=["rs"], writes=["rs"])
            S.op("dve", lambda e: e.tensor_scalar(out=ex[:], in0=ex[:], scalar1=rs[:], scalar2=None, op0=ALU.mult),
                 reads=["ex", "rs"], writes=["ex"])
            transpose_to(gts[0:NEXP, :], "gts", ex[:], "ex", NEXP)
            S.dma("pool", GT_d[:, blk * 128:(blk + 1) * 128], gts[0:NEXP, :], reads=["gts"], writes=["GT_d"])
    pop()
    if stage <= 5:
        return nc, S, es

    FFT = dram_scr("FFT", [128, 16, 2048])
    push()
    wbuf = [T("wbig%d" % i, [128, 16, 512]) for i in range(2)]
    wk = ["wbig0", "wbig1"]
    TS = 512
    h2T = T("h2T", [128, 16, TS])
    accT = T("accT", [128, 16, TS])
    actT = T("actT", [128, 16, TS])
    gbc = [T("gbc%d" % i, [128, TS]) for i in range(2)]
    gt = T("gt", [NEXP, TS])
    bdn = T("bdn", [NEXP, D])
    S.dma("sp", R(bdn[:]), b_dn, writes=["bdn"])
    bgu = T("bgu", [128, NEXP, 16, 2])
    S.dma("sp", bgu[:], bgu_fm.rearrange("p (e j t) -> p e j t", e=NEXP, j=16), writes=["bgu"])
    tg_ = [T("tg%d" % i, [128, TS]) for i in range(2)]
    tsg = [T("tsg%d" % i, [128, TS]) for i in range(2)]
    tu = [T("tu%d" % i, [128, TS]) for i in range(2)]
    tsb = [T("tsb%d" % i, [128, TS]) for i in range(2)]
    nsp = int(os.environ.get("KDBG_NSP", 2048 // TS))
    nexp = int(os.environ.get("KDBG_NEXP", NEXP))
    for sp in range(nsp):
        t0 = sp * TS
        S.dma("sp", R(h2T[:]), R(H2T[:, :, t0:t0 + TS]), writes=["h2T"])
        S.dma("sp", R(gt[:]), R(GT_d[:, t0:t0 + TS]), writes=["gt"])
        for dc in range(16):
            pi = cnt["ps"] % 4
            cnt["ps"] += 1
            S.op("pe", lambda e: e.matmul(ps[pi][:, 0:TS], lhsT=R(bdn[:, dc * 128:(dc + 1) * 128]), rhs=R(gt[:]), start=True, stop=True),
                 reads=["bdn", "gt"], writes=[psk[pi]], nosync_self=True)
            S.op("act", lambda e: e.copy(out=accT[:, dc, :], in_=ps[pi][:, 0:TS]), reads=[psk[pi]], writes=["accT"])
        for ex_ in range(nexp):
            gb, gbk = gbc[ex_ % 2], "gbc%d" % (ex_ % 2)
            S.dma("sp", gb[:], GT_d[ex_:ex_ + 1, t0:t0 + TS].partition_broadcast(128), writes=[gbk])
            wgu_v = w_gu[ex_].rearrange("(kc p) f -> p kc f", p=128)
            wdn_v = w_dn[ex_].rearrange("(kc p) f -> p kc f", p=128)
            for jf2 in range(8):
                wt, wkey = load_w(wgu_v, jf2 * 512, 512, 16)
                for sub in range(2):
                    jf = jf2 * 2 + sub
                    c0 = sub * 256
                    pg = cnt["ps"] % 4; cnt["ps"] += 1
                    pu = cnt["ps"] % 4; cnt["ps"] += 1
                    for kc in range(16):
                        S.op("pe", lambda e, kc=kc: e.matmul(ps[pg][:, 0:TS], lhsT=R(wt[:, kc, c0:c0 + 256:2]), rhs=R(h2T[:, kc, :]),
                                                             start=(kc == 0), stop=(kc == 15)),
                             reads=[wkey, "h2T"], writes=[psk[pg]], nosync_self=True)
                    for kc in range(16):
                        S.op("pe", lambda e, kc=kc: e.matmul(ps[pu][:, 0:TS], lhsT=R(wt[:, kc, c0 + 1:c0 + 256:2]), rhs=R(h2T[:, kc, :]),
                                                             start=(kc == 0), stop=(kc == 15)),
                             reads=[wkey, "h2T"], writes=[psk[pu]], nosync_self=True)
                    a = jf % 2
                    S.op("dve", lambda e: e.tensor_scalar(out=tg_[a][:], in0=ps[pg][:, 0:TS], scalar1=bgu[:, ex_, jf, 0:1], scalar2=7.0,
                                                          op0=ALU.add, op1=ALU.min), reads=[psk[pg], "bgu"], writes=["tg%d" % a])
                    S.op("act", lambda e: e.activation(out=tsg[a][:], in_=tg_[a][:], func=AF.Sigmoid, scale=1.702),
                         reads=["tg%d" % a], writes=["tsg%d" % a])
                    S.op("act", lambda e: e.activation(out=tu[a][:], in_=ps[pu][:, 0:TS], func=AF.Identity,
                                                       bias=bgu[:, ex_, jf, 1:2], scale=1.0),
                         reads=[psk[pu], "bgu"], writes=["tu%d" % a])
                    S.op("pool", lambda e: e.tensor_tensor(out=tsb[a][:], in0=tsg[a][:], in1=gb[:], op=ALU.mult),
                         reads=["tsg%d" % a, gbk], writes=["tsb%d" % a])
                    S.op("dve", lambda e: e.tensor_scalar(out=tu[a][:], in0=tu[a][:], scalar1=7.0, scalar2=-7.0,
                                                          op0=ALU.min, op1=ALU.max), reads=["tu%d" % a], writes=["tu%d" % a])
                    S.op("dve", lambda e: e.tensor_tensor(out=tg_[a][:], in0=tg_[a][:], in1=tsb[a][:], op=ALU.mult),
                         reads=["tg%d" % a, "tsb%d" % a], writes=["tg%d" % a])
                    S.op("dve", lambda e: e.scalar_tensor_tensor(out=R(actT[:, jf, :]), in0=tu[a][:], scalar=1.0, in1=tg_[a][:],
                                                                 op0=ALU.add, op1=ALU.mult),
                         reads=["tu%d" % a, "tg%d" % a], writes=["actT"])
            for dg in range(4):
                wt, wkey = load_w(wdn_v, dg * 512, 512, 16)
                for dd in range(4):
                    dc = dg * 4 + dd
                    pi = cnt["ps"] % 4
                    cnt["ps"] += 1
                    for fc in range(16):
                        S.op("pe", lambda e, fc=fc: e.matmul(ps[pi][:, 0:TS], lhsT=R(wt[:, fc, dd * 128:(dd + 1) * 128]),
                                                             rhs=R(actT[:, fc, :]), start=(fc == 0), stop=(fc == 15)),
                             reads=[wkey, "actT"], writes=[psk[pi]], nosync_self=True)
                    S.op("dve", lambda e: e.tensor_tensor(out=accT[:, dc, :], in0=ps[pi][:, 0:TS], in1=accT[:, dc, :], op=ALU.add),
                         reads=[psk[pi], "accT"], writes=["accT"])
        S.dma("pool", FFT[:, :, t0:t0 + TS], accT[:], reads=["accT"], writes=["FFT"])
    pop()
    if stage <= 6:
        return nc, S, es

    push()
    g2b = T("g2b", [128, D]); lgb_t = T("lgb", [128, D]); lbb_t = T("lbb", [128, D])
    S.dma("sp", g2b[:], ada_d[:, 5 * D:6 * D].partition_broadcast(128), writes=["g2b"])
    S.dma("sp", lgb_t[:], lnfg.partition_broadcast(128), writes=["lgb"])
    S.dma("sp", lbb_t[:], lnfb.partition_broadcast(128), writes=["lbb"])
    st6 = T("st6", [128, 4, 6]); mv = T("mv", [128, 2]); rstd = T("rstd", [128, 1])
    fb = [T("fb%d" % i, [128, 16, 128]) for i in range(2)]
    yfl = [T("yf%d" % i, [128, D]) for i in range(2)]
    x1l = [T("x1t%d" % i, [128, D]) for i in range(2)]
    nblk = nsp * (TS // 128)
    for blk in range(nblk):
        a = blk % 2
        fbt, fbk = fb[a], "fb%d" % a
        yf, yk = yfl[a], "yf%d" % a
        x1t, x1k = x1l[a], "x1t%d" % a
        S.dma("sp", fbt[:], FFT[:, :, blk * 128:(blk + 1) * 128], writes=[fbk])
        S.dma("sp", x1t[:], X1[blk * 128:(blk + 1) * 128, :], writes=[x1k])
        for dc in range(16):
            i = cnt["ps"] % 4 + 4
            cnt["ps"] += 1
            S.op("pe", lambda e: e.transpose(out=ps[i][:, 0:128], in_=fbt[:, dc, :], identity=ident[:]),
                 reads=[fbk, "ident"], writes=[psk[i]])
            S.op("act", lambda e: e.copy(out=yf[:, dc * 128:(dc + 1) * 128], in_=ps[i][:, 0:128]), reads=[psk[i]], writes=[yk])
        S.op("pool", lambda e: e.tensor_tensor(out=yf[:], in0=yf[:], in1=g2b[:], op=ALU.mult), reads=[yk, "g2b"], writes=[yk])
        S.op("dve", lambda e: e.scalar_tensor_tensor(out=yf[:], in0=x1t[:], scalar=ALPHA, in1=yf[:], op0=ALU.mult, op1=ALU.add),
             reads=[yk, x1k], writes=[yk])
        _ln_stats(yf, D, yk)
        S.op("dve", lambda e: e.tensor_scalar(out=yf[:], in0=yf[:], scalar1=mv[:, 0:1], scalar2=rstd[:],
                                              op0=ALU.subtract, op1=ALU.mult), reads=[yk, "mv", "rstd"], writes=[yk])
        S.op("pool", lambda e: e.tensor_tensor(out=yf[:], in0=yf[:], in1=lgb_t[:], op=ALU.mult), reads=[yk, "lgb"], writes=[yk])
        S.op("pool", lambda e: e.tensor_tensor(out=yf[:], in0=yf[:], in1=lbb_t[:], op=ALU.add), reads=[yk, "lbb"], writes=[yk])
        S.dma("pool", out_d[blk * 128:(blk + 1) * 128, :], yf[:], reads=[yk], writes=["out"])
    pop()
    return nc, S, es


def finish(nc, S, out_written=True):
    S.barrier()


_INVF = np.concatenate([
    (THETA ** (-np.arange(16, dtype=np.float32) / np.float32(16))).astype(np.float32),
    (THETA ** (-np.arange(8, dtype=np.float32) / np.float32(8))).astype(np.float32),
    (THETA ** (-np.arange(32, dtype=np.float32) / np.float32(32))).astype(np.float32),
]).astype(np.float32)[None, :]


def make_in_maps(inp):
    f = lambda a: np.ascontiguousarray(np.asarray(a, dtype=np.float32))
    x = f(inp["x"]); c = f(inp["c"]); pos = np.asarray(inp["positions"]).astype(np.int32)
    shared = dict(
        w_ada=f(inp["w_ada"][0]), b_ada=f(inp["b_ada"][0])[None, :], w_in=f(inp["w_in"][0]),
        ikg=f(inp["idx_k_norm_g"][0])[None, :], ikb=f(inp["idx_k_norm_b"][0])[None, :],
        qng=f(inp["q_norm_g"][0])[None, :], w_q_up=f(inp["w_q_up"][0]),
        kvng=f(inp["kv_norm_g"][0])[None, :], w_kv_up=f(inp["w_kv_up"][0]),
        ong_fm=f(np.concatenate([np.asarray(inp["out_norm_a_g"][0]).reshape(8, 128),
                                 np.asarray(inp["out_norm_b_g"][0]).reshape(8, 128)], 0).T),
        w_out=f(inp["w_out"][0]), lnmg=f(inp["ln_mix_g"][0])[None, :], lnmb=f(inp["ln_mix_b"][0])[None, :],
        w_router=f(inp["w_router"][0]), b_router=f(inp["b_router"][0])[None, :],
        w_gu=f(inp["w_gate_up"][0]),
        bgu_fm=f(np.asarray(inp["b_gate_up"][0]).reshape(NEXP, 16, 128, 2).transpose(2, 0, 1, 3).reshape(128, NEXP * 32)),
        w_dn=f(inp["w_down"][0]), b_dn=f(inp["b_down"][0]),
        lnfg=f(inp["ln_ffn_g"][0])[None, :], lnfb=f(inp["ln_ffn_b"][0])[None, :],
        ident=np.eye(128, dtype=np.float32), ones=np.ones((128, 128), np.float32),
        invf=_INVF,
    )
    esel = np.zeros((NEXP, NEXP, 128), np.float32)
    for e in range(NEXP):
        esel[e, e, :] = 1.0
    shared["esel"] = esel.reshape(NEXP, NEXP * 128)
    qi = np.arange(128)[:, None] // 64
    ki = np.arange(128)[None, :] // 64
    diag_ok = (ki <= qi)
    shared["diagb"] = np.where(diag_ok, 0.0, NEG).astype(np.float32)
    maps = []
    for core in range(8):
        b, r = core // 2, core % 2
        own_blocks = [2 * i + r for i in range(NOWN)]
        oth_blocks = [2 * i + 1 - r for i in range(NOWN)]
        order = own_blocks + oth_blocks
        xb = x[b].reshape(NB, 128, D)[order].reshape(SEQ, D)
        pb = pos[b].reshape(NB, 128)[order]
        m = dict(shared)
        m["xl"] = np.ascontiguousarray(xb)
        m["c_fm"] = np.ascontiguousarray(c[b].reshape(16, 128).T)
        m["pos_i"] = np.ascontiguousarray(pb.T.astype(np.int32))
        m["rbias"] = np.full((128, 128), 0.0 if r == 1 else NEG, np.float32)
        dT = diag_ok.T.astype(np.float32)
        one = np.ones((128, 128), np.float32); zero = np.zeros((128, 128), np.float32)
        rr = one * float(r)
        mm = np.stack([np.concatenate([dT, one], 1), np.concatenate([zero, dT], 1),
                       np.concatenate([rr, one], 1), np.concatenate([zero, rr], 1)], 1)
        m["mlam"] = np.ascontiguousarray(mm.reshape(128, 1024).astype(np.float32))
        maps.append(m)
    return maps


def kernel(**inputs):
    nc, S, es = build()
    finish(nc, S)
    maps = make_in_maps(inputs)
    res = run_bass_kernel_spmd(nc, maps, core_ids=list(range(8)))
    out = np.zeros((4, SEQ, D), np.float32)
    for core in range(8):
        b, r = core // 2, core % 2
        o = np.asarray(res.results[core]["out"]).reshape(NOWN, 128, D)
        ov = out[b].reshape(NB, 128, D)
        for i in range(NOWN):
            ov[2 * i + r] = o[i]
    return out
```

```python
import os
from contextlib import ExitStack
import numpy as np
import concourse.bass as bass
import concourse.mybir as mybir
from concourse.bass_utils import run_bass_kernel_spmd

F32 = mybir.dt.float32
F32R = mybir.dt.float32r
BF16 = mybir.dt.bfloat16
I32 = mybir.dt.int32
AF = mybir.ActivationFunctionType
ALU = mybir.AluOpType
AX = mybir.AxisListType

D = 2048
SEQ = 4096
NB = 32
NOWN = 16
EPS = 1e-5
ALPHA = 2.0 ** 0.25
THETA = 500000.0
NEXP = 32
NEG = -1.0e30
IN_W = 5008
TWO_PI = 2.0 * np.pi
C1 = 6.28125
C2 = TWO_PI - C1


def R(ap):
    return ap.bitcast(F32R)


class Sched:
    def __init__(self, nc, es, ndma=28):
        self.nc = nc
        self.E = {}
        for name, eng in [("pe", nc.tensor), ("act", nc.scalar), ("dve", nc.vector),
                          ("pool", nc.gpsimd), ("sp", nc.sync)]:
            sem = es.enter_context(nc.semaphore("sem_" + name))
            self.E[name] = dict(eng=eng, sem=sem, cnt=0, waited={})
        self.dsem = [es.enter_context(nc.semaphore("dsem%d" % i)) for i in range(ndma)]
        self.dcnt = [0] * ndma
        self.dpool = {"sp": list(range(0, 16)), "act": list(range(16, ndma))}
        self.drr = {"sp": 0, "act": 0}
        self.lw = {}
        self.rd = {}
        self.ninst = 0

    def semh(self, sid):
        if isinstance(sid, tuple):
            return self.dsem[sid[1]]
        return self.E[sid]["sem"]

    def _wait(self, en, toks):
        E = self.E[en]
        need = {}
        for t in toks:
            if t is None:
                continue
            sid, v = t
            if E["waited"].get(sid, 0) < v:
                need[sid] = max(need.get(sid, 0), v)
        for sid, v in need.items():
            E["eng"].wait_ge(self.semh(sid), v)
            E["waited"][sid] = v
            self.ninst += 1

    def _deps(self, reads, writes):
        toks = []
        for k in reads:
            toks.append(self.lw.get(k))
            if isinstance(k, str) and k.startswith("ps"):
                toks += list(self.rd.get(k, {}).items())
        for k in writes:
            toks.append(self.lw.get(k))
            toks += list(self.rd.get(k, {}).items())
        return toks

    def _record(self, tok, reads, writes):
        for k in reads:
            d = self.rd.setdefault(k, {})
            d[tok[0]] = max(d.get(tok[0], 0), tok[1])
        for k in writes:
            self.lw[k] = tok
            self.rd[k] = {}

    def op(self, en, fn, reads=(), writes=(), nosync_self=False):
        toks = self._deps(reads, writes)
        if nosync_self:
            toks = [t for t in toks if t is not None and t[0] != en]
        self._wait(en, toks)
        E = self.E[en]
        E["cnt"] += 1
        ins = fn(E["eng"])
        ins.then_inc(E["sem"], 1)
        self.ninst += 1
        tok = (en, E["cnt"])
        E["waited"][en] = max(E["waited"].get(en, 0), 0)
        self._record(tok, reads, writes)
        return tok

    def dma(self, qn, out, in_, reads=(), writes=()):
        if qn == "pool":
            qn = "act"
        pl = self.dpool[qn]
        i = pl[self.drr[qn] % len(pl)]
        self.drr[qn] += 1
        toks = self._deps(reads, writes)
        if self.dcnt[i] > 0:
            toks.append((("d", i), 16 * self.dcnt[i]))
        self._wait(qn, toks)
        self.dcnt[i] += 1
        self.E[qn]["eng"].dma_start(out=out, in_=in_).then_inc(self.dsem[i], 16)
        self.ninst += 1
        tok = (("d", i), 16 * self.dcnt[i])
        self._record(tok, reads, writes)
        return tok

    def barrier(self):
        toks = [(n, e["cnt"]) for n, e in self.E.items() if e["cnt"] > 0]
        toks += [(("d", i), 16 * c) for i, c in enumerate(self.dcnt) if c > 0]
        for en in self.E:
            self._wait(en, toks)
        self.lw = {}
        self.rd = {}


def build(stage=99, debug=False):
    nc = bass.Bass("TRN2", target_bir_lowering=False)
    nc.dge_precook = False
    es = ExitStack()
    S = Sched(nc, es)
    dbg_kind = "ExternalOutput" if debug else "Internal"

    def dram_in(name, shape, dt=F32):
        return nc.dram_tensor(name, list(shape), dt, kind="ExternalInput").ap()

    def dram_scr(name, shape, dt=F32):
        return nc.dram_tensor(name, list(shape), dt, kind=dbg_kind).ap()

    xl = dram_in("xl", [SEQ, D])
    c_fm = dram_in("c_fm", [128, 16])
    pos_i = dram_in("pos_i", [128, NB], I32)
    invf = dram_in("invf", [1, 56])
    w_ada = dram_in("w_ada", [D, 6 * D], F32R)
    b_ada = dram_in("b_ada", [1, 6 * D])
    w_in = dram_in("w_in", [D, IN_W], F32R)
    ikg = dram_in("ikg", [1, 64])
    ikb = dram_in("ikb", [1, 64])
    qng = dram_in("qng", [1, 512])
    w_q_up = dram_in("w_q_up", [512, 1536], F32R)
    kvng = dram_in("kvng", [1, 256])
    w_kv_up = dram_in("w_kv_up", [256, 2048], F32R)
    ong_fm = dram_in("ong_fm", [128, 16])
    w_out = dram_in("w_out", [D, D], F32R)
    lnmg = dram_in("lnmg", [1, D])
    lnmb = dram_in("lnmb", [1, D])
    w_router = dram_in("w_router", [D, NEXP], F32R)
    b_router = dram_in("b_router", [1, NEXP])
    big = stage >= 6
    w_gu = dram_in("w_gu", [NEXP, D, 2 * D], F32R) if big else None
    bgu_fm = dram_in("bgu_fm", [128, NEXP * 32])
    w_dn = dram_in("w_dn", [NEXP, D, D], F32R) if big else None
    b_dn = dram_in("b_dn", [NEXP, D], F32R)
    lnfg = dram_in("lnfg", [1, D])
    lnfb = dram_in("lnfb", [1, D])
    ident_d = dram_in("ident", [128, 128])
    ones_d = dram_in("ones", [128, 128], F32R)
    esel_d = dram_in("esel", [NEXP, NEXP * 128], F32R)
    diagb_d = dram_in("diagb", [128, 128])
    rbias_d = dram_in("rbias", [128, 128])
    mlam_d = dram_in("mlam", [128, 4 * 256])
    out_d = nc.dram_tensor("out", [NOWN * 128, D], F32, kind="ExternalOutput").ap()

    ada_d = dram_scr("ada_d", [1, 6 * D])
    AKT = dram_scr("AKT", [8, 128, SEQ])
    AV = dram_scr("AV", [SEQ, 1024])
    KBT = dram_scr("KBT", [8, 128, SEQ])
    VB = dram_scr("VB", [SEQ, 1024])
    AQT = dram_scr("AQT", [8, 128, 2048])
    IQT = dram_scr("IQT", [8, 128, 2048])
    QBN = dram_scr("QBN", [8, 128, 2048])
    QBR = dram_scr("QBR", [8, 64, 2048])
    IKT_d = dram_scr("IKT_d", [128, SEQ])
    KRT_d = dram_scr("KRT_d", [64, SEQ])
    SGN_d = dram_scr("SGN_d", [2048, 16])
    X1 = dram_scr("X1", [2048, D])
    H2T = dram_scr("H2T", [128, 16, 2048])
    GT_d = dram_scr("GT_d", [NEXP, 2048])
    MRG = dram_scr("MRG", [128, 16, 2048])

    scopes = [es]

    used_names = {}

    def T(name, shape, dt=F32):
        n = used_names.get(name, 0)
        used_names[name] = n + 1
        nm = "sb_" + name + ("" if n == 0 else "_v%d" % n)
        return scopes[-1].enter_context(nc.sbuf_tensor(nm, list(shape), dt))

    def push():
        scopes.append(ExitStack())

    def pop():
        S.barrier()
        scopes.pop().close()

    def P(name, shape, dt=F32):
        return es.enter_context(nc.psum_tensor("pp_" + name, list(shape), dt))

    ident = T("ident", [128, 128])
    ones = T("ones", [128, 128])
    S.dma("sp", ident[:], ident_d, writes=["ident"])
    S.dma("sp", R(ones[:]), ones_d, writes=["ones"])
    ps = [P("ps%d" % i, [128, 512]) for i in range(8)]
    psk = ["ps%d" % i for i in range(8)]
    eps_t = T("eps_t", [128, 1])
    S.op("dve", lambda e: e.memset(eps_t[:], EPS), writes=["eps"])

    push()
    wbuf = [T("wbuf%d" % i, [128, 16, 256]) for i in range(3)]
    wk = ["wbuf%d" % i for i in range(3)]
    push()
    cfm = T("cfm", [128, 16])
    scr = T("scr", [128, 16, 128])
    S.dma("sp", cfm[:], c_fm, writes=["cfm"])
    S.op("act", lambda e: e.activation(out=cfm[:], in_=cfm[:], func=AF.Silu), reads=["cfm"], writes=["cfm"])
    S.op("dve", lambda e: e.tensor_copy(out=R(scr[:]), in_=cfm[:].unsqueeze(2).to_broadcast([128, 16, 128])),
         reads=["cfm"], writes=["scr"])
    bada = [T("bada%d" % i, [128, 256]) for i in range(2)]
    w_ada_v = w_ada.rearrange("(kc p) f -> p kc f", p=128)
    for g in range(48):
        wb, wkk = wbuf[g % 3], wk[g % 3]
        bt, bk = bada[g % 2], "bada%d" % (g % 2)
        S.dma("sp", R(wb[:]), w_ada_v[:, :, g * 256:(g + 1) * 256], writes=[wkk])
        S.dma("sp", bt[:], b_ada[:, g * 256:(g + 1) * 256].partition_broadcast(128), writes=[bk])
        pst, pk = ps[g % 2], psk[g % 2]
        for kc in range(16):
            S.op("pe", lambda e, kc=kc: e.matmul(pst[:, 0:256], lhsT=R(scr[:, kc, :]), rhs=R(wb[:, kc, :]),
                                                 start=(kc == 0), stop=(kc == 15)),
                 reads=["scr", wkk], writes=[pk], nosync_self=True)
        S.op("dve", lambda e: e.tensor_tensor(out=bt[:], in0=pst[:, 0:256], in1=bt[:], op=ALU.add),
             reads=[pk, bk], writes=[bk])
        S.dma("pool", ada_d[:, g * 256:(g + 1) * 256], bt[0:1, :], reads=[bk], writes=["ada_d"])
    pop()
    if stage <= 1:
        return nc, S, es

    SIN = T("SIN", [128, NB, 56])
    COS = T("COS", [128, NB, 56])
    push()
    posi = T("posi", [128, NB], I32)
    posf = T("posf", [128, NB])
    invb = T("invb", [128, 56])
    ang = T("ang", [128, NB, 56])
    tmpa = T("tmpa", [128, NB, 56])
    tmpi = T("tmpi", [128, NB, 56], I32)
    tmpb = T("tmpb", [128, NB, 56])
    S.dma("sp", posi[:], pos_i, writes=["posi"])
    S.dma("sp", invb[:], invf.partition_broadcast(128), writes=["invb"])
    S.op("dve", lambda e: e.tensor_copy(out=posf[:], in_=posi[:]), reads=["posi"], writes=["posf"])
    for blk in range(NB):
        S.op("dve", lambda e, blk=blk: e.tensor_scalar(out=ang[:, blk, :], in0=invb[:], scalar1=posf[:, blk:blk + 1],
                                                       scalar2=None, op0=ALU.mult),
             reads=["posf", "invb"], writes=["ang"])

    def sin_table(dst, dkey, shift):
        S.op("dve", lambda e: e.tensor_scalar(out=tmpa[:], in0=ang[:], scalar1=1.0 / TWO_PI,
                                              scalar2=shift / TWO_PI + 0.5, op0=ALU.mult, op1=ALU.add),
             reads=["ang"], writes=["tmpa"])
        S.op("dve", lambda e: e.tensor_copy(out=tmpi[:], in_=tmpa[:]), reads=["tmpa"], writes=["tmpi"])
        S.op("dve", lambda e: e.tensor_copy(out=tmpa[:], in_=tmpi[:]), reads=["tmpi"], writes=["tmpa"])
        S.op("dve", lambda e: e.scalar_tensor_tensor(out=tmpb[:], in0=tmpa[:], scalar=-C1, in1=ang[:],
                                                     op0=ALU.mult, op1=ALU.add),
             reads=["tmpa", "ang"], writes=["tmpb"])
        S.op("dve", lambda e: e.scalar_tensor_tensor(out=tmpb[:], in0=tmpa[:], scalar=-C2, in1=tmpb[:],
                                                     op0=ALU.mult, op1=ALU.add),
             reads=["tmpa", "tmpb"], writes=["tmpb"])
        if shift != 0.0:
            S.op("dve", lambda e: e.tensor_scalar(out=tmpb[:], in0=tmpb[:], scalar1=shift, scalar2=None, op0=ALU.add),
                 reads=["tmpb"], writes=["tmpb"])
        S.op("dve", lambda e: e.tensor_scalar(out=tmpa[:], in0=tmpb[:], scalar1=np.pi, scalar2=-TWO_PI,
                                              op0=ALU.is_gt, op1=ALU.mult), reads=["tmpb"], writes=["tmpa"])
        S.op("dve", lambda e: e.tensor_tensor(out=tmpb[:], in0=tmpb[:], in1=tmpa[:], op=ALU.add),
             reads=["tmpa", "tmpb"], writes=["tmpb"])
        S.op("dve", lambda e: e.tensor_scalar(out=tmpa[:], in0=tmpb[:], scalar1=-np.pi, scalar2=TWO_PI,
                                              op0=ALU.is_lt, op1=ALU.mult), reads=["tmpb"], writes=["tmpa"])
        S.op("dve", lambda e: e.tensor_tensor(out=tmpb[:], in0=tmpb[:], in1=tmpa[:], op=ALU.add),
             reads=["tmpa", "tmpb"], writes=["tmpb"])
        S.op("dve", lambda e: e.tensor_scalar(out=tmpb[:], in0=tmpb[:], scalar1=3.1415925, scalar2=-3.1415925,
                                              op0=ALU.min, op1=ALU.max), reads=["tmpb"], writes=["tmpb"])
        S.op("act", lambda e: e.activation(out=dst[:], in_=tmpb[:], func=AF.Sin), reads=["tmpb"], writes=[dkey])

    sin_table(SIN, "SIN", 0.0)
    sin_table(COS, "COS", np.pi / 2)
    pop()
    FA, FI, FM = (0, 16), (16, 8), (24, 32)

    G = 4
    sh1 = T("sh1", [128, D])
    sc1 = T("sc1", [128, D])
    S.dma("sp", sh1[:], ada_d[:, 0:D].partition_broadcast(128), writes=["sh1"])
    S.dma("sp", sc1[:], ada_d[:, D:2 * D].partition_broadcast(128), writes=["sc1"])
    S.op("dve", lambda e: e.tensor_scalar(out=sc1[:], in0=sc1[:], scalar1=1.0, scalar2=None, op0=ALU.add),
         reads=["sc1"], writes=["sc1"])
    ikg_b = T("ikg_b", [128, 64]); ikb_b = T("ikb_b", [128, 64])
    qng_b = T("qng_b", [128, 512]); kvng_b = T("kvng_b", [128, 256])
    S.dma("sp", ikg_b[:], ikg.partition_broadcast(128), writes=["ikg_b"])
    S.dma("sp", ikb_b[:], ikb.partition_broadcast(128), writes=["ikb_b"])
    S.dma("sp", qng_b[:], qng.partition_broadcast(128), writes=["qng_b"])
    S.dma("sp", kvng_b[:], kvng.partition_broadcast(128), writes=["kvng_b"])

    xt = [T("xt%d" % i, [128, D]) for i in range(2)]
    hT = T("hT", [128, 16, G * 128])
    stg = [T("stg%d" % i, [128, 512]) for i in range(2)]
    stT = [T("stT%d" % i, [128, 4, G * 128]) for i in range(2)]
    qdst = T("qdst", [128, G, 512])
    cqT = T("cqT", [128, 4, G * 128])
    ckvT = T("ckvT", [128, 2, G * 128])
    absw = T("absw", [128, G, 16])
    sgn = T("sgn", [128, G, 16])
    st6 = T("st6", [128, 4, 6]); mv = T("mv", [128, 2]); rstd = T("rstd", [128, 1])
    sm = [T("sm%d" % i, [128, 4, 32]) for i in range(4)]
    IKT = T("IKT", [128, SEQ])
    KRT = T("KRT", [64, SEQ])
    ikst = T("ikst", [128, 128])
    w_in_v = w_in.rearrange("(kc p) f -> p kc f", p=128)
    wq_v = w_q_up.rearrange("(kc p) f -> p kc f", p=128)
    wkv_v = w_kv_up.rearrange("(kc p) f -> p kc f", p=128)
    cnt = dict(w=0, ps=0, stg=0, stT=0, x=0)

    def ln_stats(src_ap, n, skey):
        return _ln_stats(src_ap, n, skey)

    def _ln_stats(src_ap, n, skey):
        nch = max(1, n // 512)
        w_ = n // nch
        for c in range(nch):
            S.op("dve", lambda e, c=c: e.bn_stats(out=st6[:, c, :], in_=src_ap[:, c * w_:(c + 1) * w_]),
                 reads=[skey], writes=["st6"])
        S.op("dve", lambda e: e.bn_aggr(out=mv[:], in_=st6[:, 0:nch, :]), reads=["st6"], writes=["mv"])
        S.op("act", lambda e: e.activation(out=rstd[:], in_=mv[:, 1:2], func=AF.Ln, bias=eps_t[:], scale=1.0),
             reads=["mv", "eps"], writes=["rstd"])
        S.op("act", lambda e: e.activation(out=rstd[:], in_=rstd[:], func=AF.Exp, scale=-0.5),
             reads=["rstd"], writes=["rstd"])

    def rms_rstd(src_ap, n, skey, junk_ap, jkey):
        S.op("act", lambda e: e.activation(out=junk_ap, in_=src_ap, func=AF.Square, accum_out=mv[:, 0:1]),
             reads=[skey], writes=[jkey, "mv"])
        S.op("act", lambda e: e.activation(out=rstd[:], in_=mv[:, 0:1], func=AF.Ln, bias=eps_t[:], scale=1.0 / n),
             reads=["mv", "eps"], writes=["rstd"])
        S.op("act", lambda e: e.activation(out=rstd[:], in_=rstd[:], func=AF.Exp, scale=-0.5),
             reads=["rstd"], writes=["rstd"])

    def rope(dst, dkey, src, skey, H, off, half, blk, fslot):
        f0 = fslot[0]
        cosb = COS[:, blk, f0:f0 + half].unsqueeze(1).to_broadcast([128, H, half])
        sinb = SIN[:, blk, f0:f0 + half].unsqueeze(1).to_broadcast([128, H, half])
        x1 = src[:, :, off:off + half]
        x2 = src[:, :, off + half:off + 2 * half]
        t = [sm[i][:, 0:H, 0:half] for i in range(4)]
        S.op("dve", lambda e: e.tensor_tensor(out=t[0], in0=x1, in1=cosb, op=ALU.mult), reads=[skey, "COS"], writes=["sm0"])
        S.op("dve", lambda e: e.tensor_tensor(out=t[1], in0=x2, in1=sinb, op=ALU.mult), reads=[skey, "SIN"], writes=["sm1"])
        S.op("dve", lambda e: e.tensor_tensor(out=t[2], in0=x2, in1=cosb, op=ALU.mult), reads=[skey, "COS"], writes=["sm2"])
        S.op("dve", lambda e: e.tensor_tensor(out=t[3], in0=x1, in1=sinb, op=ALU.mult), reads=[skey, "SIN"], writes=["sm3"])
        S.op("dve", lambda e: e.tensor_tensor(out=dst[:, :, off:off + half], in0=t[0], in1=t[1], op=ALU.subtract),
             reads=["sm0", "sm1"], writes=[dkey])
        S.op("dve", lambda e: e.tensor_tensor(out=dst[:, :, off + half:off + 2 * half], in0=t[2], in1=t[3], op=ALU.add),
             reads=["sm2", "sm3"], writes=[dkey])

    deferred_T = []
    defer_on = [False]

    def flush_T():
        while deferred_T:
            a = deferred_T.pop(0)
            _transpose_to(*a)

    def transpose_to(dst_ap, dkey, src_ap, skey, ncol):
        if defer_on[0]:
            deferred_T.append((dst_ap, dkey, src_ap, skey, ncol))
        else:
            _transpose_to(dst_ap, dkey, src_ap, skey, ncol)

    def _transpose_to(dst_ap, dkey, src_ap, skey, ncol):
        i = cnt["ps"] % 4 + 4
        cnt["ps"] += 1
        S.op("pe", lambda e: e.transpose(out=ps[i][0:ncol, 0:128], in_=src_ap, identity=ident[:]),
             reads=[skey, "ident"], writes=[psk[i]])
        S.op("act", lambda e: e.copy(out=R(dst_ap), in_=ps[i][0:ncol, 0:128]), reads=[psk[i]], writes=[dkey])

    def proj_block(lhs_tile, lkey, nkc, wtile, wkey, ncols, bi):
        i = cnt["ps"] % 4
        cnt["ps"] += 1
        for kc in range(nkc):
            S.op("pe", lambda e, kc=kc: e.matmul(ps[i][:, 0:ncols], lhsT=R(lhs_tile[:, kc, bi * 128:(bi + 1) * 128]),
                                                 rhs=R(wtile[:, kc, 0:ncols]), start=(kc == 0), stop=(kc == nkc - 1)),
                 reads=[lkey, wkey], writes=[psk[i]], nosync_self=True)
        return ps[i], psk[i]

    def load_w(view, c0, ncols, nkc):
        i = cnt["w"] % len(wbuf)
        cnt["w"] += 1
        wt = wbuf[i]
        flat = wt[:].rearrange("p a b -> p (a b)")
        dst = flat[:, 0:nkc * ncols].rearrange("p (a b) -> p a b", a=nkc)
        S.dma("sp", R(dst), view[:, 0:nkc, c0:c0 + ncols], writes=[wk[i]])
        return dst, wk[i]


    ngroups = NB // G
    tglist = list(range(int(os.environ.get("KDBG_TG", ngroups))))
    if os.environ.get("KDBG_TGLIST"):
        tglist = [int(v) for v in os.environ["KDBG_TGLIST"].split(",")]
    for tg in tglist:
        own = tg < (NOWN // G)
        for bi in range(G):
            blk = tg * G + bi
            xi = cnt["x"] % 2
            cnt["x"] += 1
            xb, xk = xt[xi], "xt%d" % xi
            S.dma("sp", xb[:], xl[blk * 128:(blk + 1) * 128, :], writes=[xk])
            ln_stats(xb, D, xk)
            S.op("dve", lambda e: e.tensor_scalar(out=xb[:], in0=xb[:], scalar1=mv[:, 0:1], scalar2=rstd[:],
                                                  op0=ALU.subtract, op1=ALU.mult), reads=[xk, "mv", "rstd"], writes=[xk])
            S.op("pool", lambda e: e.tensor_tensor(out=xb[:], in0=xb[:], in1=sc1[:], op=ALU.mult),
                 reads=[xk, "sc1"], writes=[xk])
            S.op("pool", lambda e: e.tensor_tensor(out=xb[:], in0=xb[:], in1=sh1[:], op=ALU.add),
                 reads=[xk, "sh1"], writes=[xk])
            for kc in range(16):
                transpose_to(hT[:, kc, bi * 128:(bi + 1) * 128], "hT", xb[:, kc * 128:(kc + 1) * 128], xk, 128)

        def stage_out(kind, sub):
            pass

        def run_group(kind, c0, ncols, sub):
            if os.environ.get("KDBG_KINDS") and kind not in os.environ["KDBG_KINDS"].split(","):
                return
            if kind == "qup":
                wt, wkey = load_w(wq_v, c0, ncols, 4)
                lhs, lkey, nkc = cqT, "cqT", 4
            elif kind == "kvup":
                wt, wkey = load_w(wkv_v, c0, ncols, 2)
                lhs, lkey, nkc = ckvT, "ckvT", 2
            else:
                wt, wkey = load_w(w_in_v, c0, ncols, 16)
                lhs, lkey, nkc = hT, "hT", 16
            si = cnt["stT"] % 2
            cnt["stT"] += 1
            sT, sTk = stT[si], "stT%d" % si
            defer_on[0] = True
            for bi in range(G):
                blk = tg * G + bi
                pt, pk = proj_block(lhs, lkey, nkc, wt, wkey, ncols, bi)
                flush_T()
                gi = cnt["stg"] % 2
                cnt["stg"] += 1
                sg, sgk = stg[gi], "stg%d" % gi
                if kind in ("aq", "ak"):
                    S.op("act", lambda e: e.copy(out=sg[:, 0:256], in_=pt[:, 0:256]), reads=[pk], writes=[sgk])
                    v = sg[:, 0:256].rearrange("p (h d) -> p h d", h=2)
                    pv = pt[:, 0:256].rearrange("p (h d) -> p h d", h=2)
                    rope(v, sgk, v, sgk, 2, 0, 16, blk, FA)
                    for h in range(2):
                        transpose_to(sT[:, h, bi * 128:(bi + 1) * 128], sTk, sg[:, h * 128:(h + 1) * 128], sgk, 128)
                elif kind == "av":
                    S.op("act", lambda e: e.copy(out=sg[:, 0:256], in_=pt[:, 0:256]), reads=[pk], writes=[sgk])
                    S.dma("pool", AV[blk * 128:(blk + 1) * 128, sub * 256:(sub + 1) * 256], sg[:, 0:256], reads=[sgk], writes=["AV"])
                elif kind == "iq":
                    S.op("act", lambda e: e.copy(out=sg[:, 0:256], in_=pt[:, 0:256]), reads=[pk], writes=[sgk])
                    v = sg[:, 0:256].rearrange("p (h d) -> p h d", h=4)
                    pv = pt[:, 0:256].rearrange("p (h d) -> p h d", h=4)
                    rope(v, sgk, v, sgk, 4, 0, 8, blk, FI)
                    S.op("dve", lambda e: e.tensor_tensor(out=v, in0=v, in1=absw[:, bi, sub * 4:(sub + 1) * 4].unsqueeze(2).to_broadcast([128, 4, 64]),
                                                          op=ALU.mult), reads=[sgk, "absw"], writes=[sgk])
                    for h in range(2):
                        transpose_to(sT[:, h, bi * 128:(bi + 1) * 128], sTk, sg[:, h * 128:(h + 1) * 128], sgk, 128)
                elif kind == "misc":
                    S.op("act", lambda e: e.copy(out=sg[:, 0:80], in_=pt[:, 0:80]), reads=[pk], writes=[sgk])
                    ln_stats(sg[:, 0:64], 64, sgk)
                    S.op("dve", lambda e: e.tensor_scalar(out=sg[:, 0:64], in0=sg[:, 0:64], scalar1=mv[:, 0:1], scalar2=rstd[:],
                                                          op0=ALU.subtract, op1=ALU.mult), reads=[sgk, "mv", "rstd"], writes=[sgk])
                    S.op("dve", lambda e: e.tensor_tensor(out=sg[:, 0:64], in0=sg[:, 0:64], in1=ikg_b[:], op=ALU.mult),
                         reads=[sgk, "ikg_b"], writes=[sgk])
                    S.op("dve", lambda e: e.tensor_tensor(out=sg[:, 128:192], in0=sg[:, 0:64], in1=ikb_b[:], op=ALU.add),
                         reads=[sgk, "ikb_b"], writes=[sgk])
                    v = sg[:, 128:192].rearrange("p (h d) -> p h d", h=1)
                    rope(v, sgk, v, sgk, 1, 0, 8, blk, FI)
                    S.op("dve", lambda e: e.tensor_copy(out=sg[:, 192:256], in_=sg[:, 128:192]), reads=[sgk], writes=[sgk])
                    transpose_to(IKT[:, blk * 128:(blk + 1) * 128], "IKT", sg[:, 128:256], sgk, 128)
                    if own:
                        S.op("act", lambda e: e.activation(out=absw[:, bi, :], in_=sg[:, 64:80], func=AF.Abs),
                             reads=[sgk], writes=["absw"])
                        S.op("dve", lambda e: e.tensor_scalar(out=sgn[:, bi, :], in0=sg[:, 64:80], scalar1=0.0, scalar2=-0.5,
                                                              op0=ALU.is_ge, op1=ALU.add), reads=[sgk], writes=["sgn"])
                        S.dma("pool", SGN_d[blk * 128:(blk + 1) * 128, :], sgn[:, bi, :], reads=["sgn"], writes=["SGN_d"])
                elif kind == "qd":
                    S.op("act", lambda e: e.copy(out=qdst[:, bi, sub * 256:(sub + 1) * 256], in_=pt[:, 0:256]),
                         reads=[pk], writes=["qdst"])
                    if sub == 1:
                        rms_rstd(qdst[:, bi, :], 512, "qdst", sg[:, 0:512], sgk)
                        S.op("dve", lambda e: e.scalar_tensor_tensor(out=qdst[:, bi, :], in0=qdst[:, bi, :], scalar=rstd[:],
                                                                     in1=qng_b[:], op0=ALU.mult, op1=ALU.mult),
                             reads=["qdst", "rstd", "qng_b"], writes=["qdst"])
                        for c in range(4):
                            transpose_to(cqT[:, c, bi * 128:(bi + 1) * 128], "cqT", qdst[:, bi, c * 128:(c + 1) * 128], "qdst", 128)
                elif kind == "kvd":
                    S.op("act", lambda e: e.copy(out=sg[:, 0:256], in_=pt[:, 0:256]), reads=[pk], writes=[sgk])
                    rms_rstd(sg[:, 0:256], 256, sgk, sg[:, 256:512], sgk)
                    S.op("dve", lambda e: e.scalar_tensor_tensor(out=sg[:, 0:256], in0=sg[:, 0:256], scalar=rstd[:],
                                                                 in1=kvng_b[:], op0=ALU.mult, op1=ALU.mult),
                         reads=[sgk, "rstd", "kvng_b"], writes=[sgk])
                    for c in range(2):
                        transpose_to(ckvT[:, c, bi * 128:(bi + 1) * 128], "ckvT", sg[:, c * 128:(c + 1) * 128], sgk, 128)
                elif kind == "kr":
                    S.op("act", lambda e: e.copy(out=sg[:, 0:64], in_=pt[:, 0:64]), reads=[pk], writes=[sgk])
                    v = sg[:, 0:64].rearrange("p (h d) -> p h d", h=1)
                    rope(v, sgk, v, sgk, 1, 0, 32, blk, FM)
                    transpose_to(KRT[:, blk * 128:(blk + 1) * 128], "KRT", sg[:, 0:64], sgk, 64)
                elif kind == "qup":
                    S.op("act", lambda e: e.copy(out=sg[:, 0:384], in_=pt[:, 0:384]), reads=[pk], writes=[sgk])
                    v = sg[:, 0:384].rearrange("p (h d) -> p h d", h=2)
                    pv = pt[:, 0:384].rearrange("p (h d) -> p h d", h=2)
                    rope(v, sgk, v, sgk, 2, 128, 32, blk, FM)
                    for h in range(2):
                        transpose_to(sT[:, h, bi * 128:(bi + 1) * 128], sTk, sg[:, h * 192:h * 192 + 128], sgk, 128)
                        transpose_to(sT[0:64, 2 + h, bi * 128:(bi + 1) * 128], sTk, sg[:, h * 192 + 128:(h + 1) * 192], sgk, 64)
                elif kind == "kvup":
                    S.op("act", lambda e: e.copy(out=sg[:, 0:256], in_=pt[:, 0:256]), reads=[pk], writes=[sgk])
                    transpose_to(sT[:, 0, bi * 128:(bi + 1) * 128], sTk, sg[:, 0:128], sgk, 128)
                    S.dma("pool", VB[blk * 128:(blk + 1) * 128, sub * 128:(sub + 1) * 128], sg[:, 128:256], reads=[sgk], writes=["VB"])
            defer_on[0] = False
            flush_T()
            t0 = tg * G * 128
            tw = G * 128
            if kind == "ak":
                for h in range(2):
                    S.dma("pool", AKT[sub * 2 + h, :, t0:t0 + tw], sT[:, h, :], reads=[sTk], writes=["AKT"])
            elif kind == "aq":
                for h in range(2):
                    S.dma("pool", AQT[sub * 2 + h, :, t0:t0 + tw], sT[:, h, :], reads=[sTk], writes=["AQT"])
            elif kind == "iq":
                for h in range(2):
                    S.dma("pool", IQT[sub * 2 + h, :, t0:t0 + tw], sT[:, h, :], reads=[sTk], writes=["IQT"])
            elif kind == "qup":
                for h in range(2):
                    S.dma("pool", QBN[sub * 2 + h, :, t0:t0 + tw], sT[:, h, :], reads=[sTk], writes=["QBN"])
                    S.dma("pool", QBR[sub * 2 + h, :, t0:t0 + tw], sT[0:64, 2 + h, :], reads=[sTk], writes=["QBR"])
            elif kind == "kvup":
                S.dma("pool", KBT[sub, :, t0:t0 + tw], sT[:, 0, :], reads=[sTk], writes=["KBT"])

        run_group("misc", 4096, 80, 0)
        for s_ in range(4):
            run_group("ak", 1024 + s_ * 256, 256, s_)
        for s_ in range(4):
            run_group("av", 2048 + s_ * 256, 256, s_)
        run_group("kvd", 4688, 256, 0)
        run_group("kr", 4944, 64, 0)
        for s_ in range(8):
            run_group("kvup", s_ * 256, 256, s_)
        if own:
            for s_ in range(4):
                run_group("aq", s_ * 256, 256, s_)
            for s_ in range(4):
                run_group("iq", 3072 + s_ * 256, 256, s_)
            for s_ in range(2):
                run_group("qd", 4176 + s_ * 256, 256, s_)
            for s_ in range(4):
                run_group("qup", s_ * 384, 384, s_)
    for tg in tglist:
        S.dma("pool", IKT_d[:, tg * 512:(tg + 1) * 512], IKT[:, tg * 512:(tg + 1) * 512], reads=["IKT"], writes=["IKT_d"])
        S.dma("pool", KRT_d[:, tg * 512:(tg + 1) * 512], KRT[:, tg * 512:(tg + 1) * 512], reads=["KRT"], writes=["KRT_d"])
    pop()
    if stage <= 2:
        return nc, S, es

    SCALE_A = 128.0 ** -0.5
    SCALE_B = 192.0 ** -0.5
    BF = BF16

    def attn_core(j, h, kT, kTk, vt, vk, qT_ap, qkey, scale, OT, okey, mask_fn, extra_qk=None, defer=None):
        NKB = 2 * j + 2
        blocks = [(part, kb) for part in range(2) for kb in range(0, NKB, 2)]
        for idx, (part, kb) in enumerate(blocks):
            pi = cnt["ps"] % 4
            cnt["ps"] += 1
            for t in range(2):
                kcol = (kb + t) * 128
                S.op("pe", lambda e, t=t, kcol=kcol: e.matmul(ps[pi][:, t * 256:(t + 1) * 256], lhsT=R(kT[part][:, kcol:kcol + 128]),
                                                              rhs=R(qT_ap), start=True, stop=(extra_qk is None)),
                     reads=[kTk[part], qkey], writes=[psk[pi]], nosync_self=True)
                if extra_qk is not None:
                    kr_ap, krkey, qr_ap, qrkey = extra_qk
                    S.op("pe", lambda e, t=t, kcol=kcol: e.matmul(ps[pi][:, t * 256:(t + 1) * 256],
                                                                  lhsT=R(kr_ap[0:64, part * 2048 + kcol:part * 2048 + kcol + 128]),
                                                                  rhs=R(qr_ap), start=False, stop=True),
                         reads=[krkey, qrkey], writes=[psk[pi]], nosync_self=True)
            pti = cnt["pt"] % 2
            cnt["pt"] += 1
            pt, ptk = ptl[pti], "pt%d" % pti
            m_ap = mask_fn(part, kb)
            if m_ap is None:
                S.op("act", lambda e: e.activation(out=R(pt[:]), in_=ps[pi][:, 0:512], func=AF.Exp, scale=scale),
                     reads=[psk[pi]], writes=[ptk])
                src, srck = pt, ptk
            else:
                S.op("act", lambda e: e.activation(out=R(pt[:]), in_=ps[pi][:, 0:512], func=AF.Exp, scale=scale),
                     reads=[psk[pi]], writes=[ptk])
                pm, pmk = ptm[pti], "ptm%d" % pti
                eng = "pool" if (defer is not None or idx % 2 == 1) else "dve"
                S.op(eng, lambda e: e.tensor_tensor(out=R(pm[:]), in0=pt[:], in1=m_ap, op=ALU.mult),
                     reads=[ptk, "mskT"], writes=[pmk])
                src, srck = pm, pmk
            last = idx == len(blocks) - 1
            for t in range(2):
                S.op("pe", lambda e, t=t: e.matmul(ps[6][:, 0:256], lhsT=R(vt[part][:, kb + t, :]), rhs=R(src[:, t * 256:(t + 1) * 256]),
                                                   start=(idx == 0 and t == 0), stop=(last and t == 1)),
                     reads=[vk[part], srck], writes=[psk[6]], nosync_self=True)
                S.op("pe", lambda e, t=t: e.matmul(ps[7][:, 0:256], lhsT=R(ones[:]), rhs=R(src[:, t * 256:(t + 1) * 256]),
                                                   start=(idx == 0 and t == 0), stop=(last and t == 1)),
                     reads=["ones", srck], writes=[psk[7]], nosync_self=True)
        if defer is not None:
            DEN, dkey = defer
            S.op("act", lambda e: e.copy(out=OT[:, h, :], in_=ps[6][:, 0:256]), reads=[psk[6]], writes=[okey])
            S.op("act", lambda e: e.copy(out=DEN[:, h, :], in_=ps[7][:, 0:256]), reads=[psk[7]], writes=[dkey])
            return
        S.op("dve", lambda e: e.reciprocal(out=rden[:], in_=ps[7][:, 0:256]), reads=[psk[7]], writes=["rden"])
        S.op("dve", lambda e: e.tensor_tensor(out=OT[:, h, :], in0=ps[6][:, 0:256], in1=rden[:], op=ALU.mult),
             reads=[psk[6], "rden"], writes=[okey])

    def out_norm(j, OT, okey, goff):
        q0 = j * 256
        for h in range(8):
            S.op("pool", lambda e, h=h: e.tensor_tensor(out=R(sq[:, h, :]), in0=OT[:, h, :], in1=OT[:, h, :], op=ALU.mult),
                 reads=[okey], writes=["sq"])
        for h in range(8):
            S.op("pe", lambda e, h=h: e.matmul(ps[5][:, 0:256], lhsT=R(ones[:]), rhs=R(sq[:, h, :]), start=(h == 0), stop=(h == 7)),
                 reads=["ones", "sq"], writes=[psk[5]], nosync_self=True)
        S.op("act", lambda e: e.activation(out=rden[:], in_=ps[5][:, 0:256], func=AF.Ln, bias=eps_t[:], scale=1.0 / 1024.0),
             reads=[psk[5], "eps"], writes=["rden"])
        S.op("act", lambda e: e.activation(out=rden[:], in_=rden[:], func=AF.Exp, scale=-0.5), reads=["rden"], writes=["rden"])
        for h in range(8):
            S.op("dve", lambda e, h=h: e.scalar_tensor_tensor(out=OT[:, h, :], in0=OT[:, h, :], scalar=ong[:, goff + h:goff + h + 1],
                                                              in1=rden[:], op0=ALU.mult, op1=ALU.mult),
                 reads=[okey, "ong", "rden"], writes=[okey])
        S.dma("pool", MRG[:, goff:goff + 8, q0:q0 + 256], OT[:], reads=[okey], writes=["MRG"])

    cnt["pt"] = 0
    push()
    IKT2 = T("IKT2", [128, SEQ])
    for tg in tglist:
        S.dma("sp", IKT2[:, tg * 512:(tg + 1) * 512], IKT_d[:, tg * 512:(tg + 1) * 512], writes=["IKT2"])
    ong = T("ong", [128, 16])
    S.dma("sp", ong[:], ong_fm, writes=["ong"])
    identb = T("identb", [128, 128], BF)
    S.op("dve", lambda e: e.tensor_copy(out=identb[:], in_=ident[:]), reads=["ident"], writes=["identb"])
    diagb = T("diagb", [128, 128]); rbias = T("rbias", [128, 128])
    S.dma("sp", diagb[:], diagb_d, writes=["diagb"])
    S.dma("sp", rbias[:], rbias_d, writes=["rbias"])
    Isc = T("Isc", [128, 4096])
    work = T("work", [128, 4096])
    msk = T("msk", [128, 4096], BF)
    mskT = T("mskT", [128, 2, 16, 256], BF)
    iqT = T("iqT", [128, 8, 256])
    aqT = T("aqT", [128, 8, 256])
    sgnq = T("sgnq", [128, 2, 16])
    tmpr = [T("tmpr%d" % i, [128, 512]) for i in range(2)]
    m8 = T("m8", [128, 8]); thr = T("thr", [128, 1])
    kT = [T("kT%d" % i, [128, 2048]) for i in range(2)]
    vt = [T("vt%d" % i, [128, 16, 128]) for i in range(2)]
    kTk = ["kT0", "kT1"]; vk = ["vt0", "vt1"]
    ptl = [T("pt%d" % i, [128, 512]) for i in range(2)]
    ptm = [T("ptm%d" % i, [128, 512]) for i in range(2)]
    OT = T("OT", [128, 8, 256])
    sq = T("sq", [128, 8, 256])
    rden = T("rden", [128, 256])
    KRT2 = T("KRT2", [64, SEQ])
    for tg in tglist:
        S.dma("sp", R(KRT2[:, tg * 512:(tg + 1) * 512]), R(KRT_d[:, tg * 512:(tg + 1) * 512]), writes=["KRT2"])
    mlam = T("mlam", [128, 4, 256])
    S.dma("sp", mlam[:], mlam_d.rearrange("p (a b) -> p a b", a=4), writes=["mskT"])
    qbn = T("qbn", [128, 8, 256])
    qbr = T("qbr", [64, 8, 256])
    OTB = T("OTB", [128, 8, 256])
    DEN = T("DEN", [128, 8, 256])
    npairs = int(os.environ.get("KDBG_NPAIR", 8))
    for j in range(npairs):
        q0 = j * 256
        NKB = 2 * j + 2
        S.dma("sp", R(qbn[:]), R(QBN[:, :, q0:q0 + 256]).rearrange("h p q -> p h q"), writes=["qbn"])
        S.dma("sp", R(qbr[:]), R(QBR[:, :, q0:q0 + 256]).rearrange("h p q -> p h q"), writes=["qbr"])
        S.dma("sp", iqT[:], IQT[:, :, q0:q0 + 256].rearrange("h p q -> p h q"), writes=["iqT"])
        S.dma("sp", R(aqT[:]), R(AQT[:, :, q0:q0 + 256]).rearrange("h p q -> p h q"), writes=["aqT"])
        S.dma("sp", sgnq[:], SGN_d[q0:q0 + 256, :].rearrange("(b p) h -> p b h", p=128), writes=["sgnq"])
        for part in range(2):
            S.op("pool", lambda e, part=part: e.memset(mskT[:, part, NKB - 1, 0:128], 0.0), writes=["mskT"])
        for qb in range(2):
            i = 2 * j + qb
            nk = i + 1
            for part in range(2):
                for c0 in range(0, nk * 128, 512):
                    cw = min(512, nk * 128 - c0)
                    for h in range(16):
                        pi = cnt["ps"] % 4
                        cnt["ps"] += 1
                        p0 = (h % 2) * 64
                        S.op("pe", lambda e, h=h, p0=p0: e.matmul(ps[pi][:, 0:cw], lhsT=iqT[p0:p0 + 64, h // 2, qb * 128:(qb + 1) * 128],
                                                                  rhs=IKT2[p0:p0 + 64, part * 2048 + c0:part * 2048 + c0 + cw],
                                                                  start=True, stop=True),
                             reads=["iqT", "IKT2"], writes=[psk[pi]], nosync_self=True)
                        ti = cnt["pt"] % 2
                        cnt["pt"] += 1
                        S.op("act", lambda e: e.activation(out=tmpr[ti][:, 0:cw], in_=ps[pi][:, 0:cw], func=AF.Relu),
                             reads=[psk[pi]], writes=["tmpr%d" % ti])
                        if h == 0:
                            S.op("dve", lambda e: e.tensor_scalar(out=Isc[:, part * nk * 128 + c0:part * nk * 128 + c0 + cw], in0=tmpr[ti][:, 0:cw],
                                                                  scalar1=sgnq[:, qb, 0:1], scalar2=None, op0=ALU.mult),
                                 reads=["tmpr%d" % ti, "sgnq"], writes=["Isc"])
                        else:
                            S.op("dve", lambda e, h=h: e.scalar_tensor_tensor(out=Isc[:, part * nk * 128 + c0:part * nk * 128 + c0 + cw], in0=tmpr[ti][:, 0:cw],
                                                                              scalar=sgnq[:, qb, h:h + 1], in1=Isc[:, part * nk * 128 + c0:part * nk * 128 + c0 + cw],
                                                                              op0=ALU.mult, op1=ALU.add),
                                 reads=["tmpr%d" % ti, "sgnq", "Isc"], writes=["Isc"])
            S.op("dve", lambda e: e.tensor_tensor(out=Isc[:, i * 128:(i + 1) * 128], in0=Isc[:, i * 128:(i + 1) * 128],
                                                  in1=diagb[:], op=ALU.add), reads=["Isc", "diagb"], writes=["Isc"])
            S.op("dve", lambda e: e.tensor_tensor(out=Isc[:, nk * 128 + i * 128:nk * 128 + (i + 1) * 128],
                                                  in0=Isc[:, nk * 128 + i * 128:nk * 128 + (i + 1) * 128],
                                                  in1=rbias[:], op=ALU.add), reads=["Isc", "rbias"], writes=["Isc"])
            for h in range(4 * qb, 4 * qb + 4):
                for part in range(2):
                    S.dma("sp", R(kT[part][:, 0:NKB * 128]), R(KBT[h, :, part * 2048:part * 2048 + NKB * 128]), writes=[kTk[part]])
                    S.dma("sp", R(vt[part][:, 0:NKB, :]),
                          R(VB[part * 2048:part * 2048 + NKB * 128, h * 128:(h + 1) * 128]).rearrange("(kb p) d -> p kb d", p=128),
                          writes=[vk[part]])
                attn_core(j, h, kT, kTk, vt, vk, qbn[:, h, :], "qbn", SCALE_B, OTB, "OTB",
                          lambda part, kb, NKB=NKB: (mlam[:, part * 2:part * 2 + 2, :].rearrange("p a b -> p (a b)")
                                                     if kb == NKB - 2 else None),
                          extra_qk=(KRT2, "KRT2", qbr[:, h, :], "qbr"), defer=(DEN, "DEN"))
            Iv = Isc[:, 0:2 * nk * 128]
            Wv = work[:, 0:2 * nk * 128]
            if i == 0:
                S.op("dve", lambda e: e.memset(thr[:], -1.0e29), writes=["thr"])
            else:
                for it in range(32):
                    src = Iv if it == 0 else Wv
                    S.op("dve", lambda e: e.max(out=m8[:], in_=src), reads=["Isc", "work"], writes=["m8"])
                    if it < 31:
                        S.op("dve", lambda e: e.match_replace(out=Wv, in_to_replace=m8[:], in_values=src, imm_value=NEG),
                             reads=["Isc", "work", "m8"], writes=["work"])
                S.op("dve", lambda e: e.tensor_scalar(out=thr[:], in0=m8[:, 7:8], scalar1=-1.0e29, scalar2=None, op0=ALU.max),
                     reads=["m8"], writes=["thr"])
            S.op("dve", lambda e: e.tensor_scalar(out=msk[:, 0:2 * nk * 128], in0=Iv, scalar1=thr[:], scalar2=None, op0=ALU.is_ge),
                 reads=["Isc", "thr"], writes=["msk"])
            for part in range(2):
                for kb in range(nk):
                    pi = cnt["ps"] % 4
                    cnt["ps"] += 1
                    pb = ps[pi][:].bitcast(BF)
                    S.op("pe", lambda e: e.transpose(out=pb[:, 0:128], in_=msk[:, (part * nk + kb) * 128:(part * nk + kb + 1) * 128], identity=identb[:]),
                         reads=["msk", "identb"], writes=[psk[pi]])
                    S.op("act", lambda e: e.copy(out=mskT[:, part, kb, qb * 128:(qb + 1) * 128], in_=pb[:, 0:128]),
                         reads=[psk[pi]], writes=["mskT"])
        for h in range(8):
            for part in range(2):
                S.dma("sp", R(kT[part][:, 0:NKB * 128]), R(AKT[h, :, part * 2048:part * 2048 + NKB * 128]), writes=[kTk[part]])
                S.dma("sp", R(vt[part][:, 0:NKB, :]),
                      R(AV[part * 2048:part * 2048 + NKB * 128, h * 128:(h + 1) * 128]).rearrange("(kb p) d -> p kb d", p=128),
                      writes=[vk[part]])
            attn_core(j, h, kT, kTk, vt, vk, aqT[:, h, :], "aqT", SCALE_A, OT, "OT",
                      lambda part, kb: mskT[:, part, kb:kb + 2, :].rearrange("p a b -> p (a b)"))
        out_norm(j, OT, "OT", 0)
        S.op("dve", lambda e: e.reciprocal(out=DEN[:], in_=DEN[:]), reads=["DEN"], writes=["DEN"])
        S.op("pool", lambda e: e.tensor_tensor(out=OTB[:], in0=OTB[:], in1=DEN[:], op=ALU.mult), reads=["OTB", "DEN"], writes=["OTB"])
        out_norm(j, OTB, "OTB", 8)
    pop()
    if stage <= 4:
        return nc, S, es

    push()
    wbuf = [T("wbuf%d" % i, [128, 16, 256]) for i in range(3)]
    bc = {}
    for nm, src in [("g1", ada_d[:, 2 * D:3 * D]), ("sh2", ada_d[:, 3 * D:4 * D]), ("sc2", ada_d[:, 4 * D:5 * D]),
                    ("lnmg", lnmg), ("lnmb", lnmb)]:
        bc[nm] = T("bc_" + nm, [128, D])
        S.dma("sp", bc[nm][:], src.partition_broadcast(128), writes=["bc_" + nm])
    S.op("dve", lambda e: e.tensor_scalar(out=bc["sc2"][:], in0=bc["sc2"][:], scalar1=1.0, scalar2=None, op0=ALU.add),
         reads=["bc_sc2"], writes=["bc_sc2"])
    mT = T("mT", [128, 16, 256])
    ymix = [T("ymix%d" % i, [128, D]) for i in range(2)]
    xt = [T("xt%d" % i, [128, D]) for i in range(2)]
    h2b = T("h2b", [128, 16, 128])
    wr = T("wr", [128, 16, NEXP])
    S.dma("sp", R(wr[:]), w_router.rearrange("(kc p) e -> p kc e", p=128), writes=["wr"])
    brt = T("brt", [128, NEXP])
    S.dma("sp", brt[:], b_router.partition_broadcast(128), writes=["brt"])
    lg = T("lg", [128, NEXP]); ex = T("ex", [128, NEXP]); mk = T("mk", [128, NEXP])
    m8 = T("m8", [128, 8]); nmx = T("nmx", [128, 1]); rs = T("rs", [128, 1])
    gts = T("gts", [128, 128])
    st6 = T("st6", [128, 4, 6]); mv = T("mv", [128, 2]); rstd = T("rstd", [128, 1])
    w_out_v = w_out.rearrange("(kc p) f -> p kc f", p=128)
    for j in range(npairs):
        q0 = j * 256
        S.dma("sp", R(mT[:]), R(MRG[:, :, q0:q0 + 256]), writes=["mT"])
        for tb in range(2):
            S.dma("sp", xt[tb][:], xl[q0 + tb * 128:q0 + (tb + 1) * 128, :], writes=["xt%d" % tb])
        for cg in range(8):
            wt, wkey = load_w(w_out_v, cg * 256, 256, 16)
            for tb in range(2):
                pt_, pk = proj_block(mT, "mT", 16, wt, wkey, 256, tb)
                S.op("dve", lambda e: e.tensor_tensor(out=ymix[tb][:, cg * 256:(cg + 1) * 256], in0=pt_[:, 0:256],
                                                      in1=bc["g1"][:, cg * 256:(cg + 1) * 256], op=ALU.mult),
                     reads=[pk, "bc_g1"], writes=["ymix%d" % tb])
        for tb in range(2):
            blk = 2 * j + tb
            xb, xk = xt[tb], "xt%d" % tb
            ym, yk = ymix[tb], "ymix%d" % tb
            S.op("dve", lambda e: e.scalar_tensor_tensor(out=ym[:], in0=xb[:], scalar=ALPHA, in1=ym[:], op0=ALU.mult, op1=ALU.add),
                 reads=[xk, yk], writes=[yk])
            ln_stats2 = lambda src, key: _ln_stats(src, D, key)
            _ln_stats(ym, D, yk)
            S.op("dve", lambda e: e.tensor_scalar(out=ym[:], in0=ym[:], scalar1=mv[:, 0:1], scalar2=rstd[:],
                                                  op0=ALU.subtract, op1=ALU.mult), reads=[yk, "mv", "rstd"], writes=[yk])
            S.op("pool", lambda e: e.tensor_tensor(out=ym[:], in0=ym[:], in1=bc["lnmg"][:], op=ALU.mult),
                 reads=[yk, "bc_lnmg"], writes=[yk])
            S.op("pool", lambda e: e.tensor_tensor(out=ym[:], in0=ym[:], in1=bc["lnmb"][:], op=ALU.add),
                 reads=[yk, "bc_lnmb"], writes=[yk])
            S.dma("pool", X1[blk * 128:(blk + 1) * 128, :], ym[:], reads=[yk], writes=["X1"])
            _ln_stats(ym, D, yk)
            S.op("dve", lambda e: e.tensor_scalar(out=xb[:], in0=ym[:], scalar1=mv[:, 0:1], scalar2=rstd[:],
                                                  op0=ALU.subtract, op1=ALU.mult), reads=[yk, "mv", "rstd"], writes=[xk])
            S.op("pool", lambda e: e.tensor_tensor(out=xb[:], in0=xb[:], in1=bc["sc2"][:], op=ALU.mult),
                 reads=[xk, "bc_sc2"], writes=[xk])
            S.op("pool", lambda e: e.tensor_tensor(out=xb[:], in0=xb[:], in1=bc["sh2"][:], op=ALU.add),
                 reads=[xk, "bc_sh2"], writes=[xk])
            for kc in range(16):
                transpose_to(h2b[:, kc, :], "h2b", xb[:, kc * 128:(kc + 1) * 128], xk, 128)
            S.dma("pool", H2T[:, :, blk * 128:(blk + 1) * 128], h2b[:], reads=["h2b"], writes=["H2T"])
            pi = cnt["ps"] % 4
            cnt["ps"] += 1
            for kc in range(16):
                S.op("pe", lambda e, kc=kc: e.matmul(ps[pi][:, 0:NEXP], lhsT=R(h2b[:, kc, :]), rhs=R(wr[:, kc, :]),
                                                     start=(kc == 0), stop=(kc == 15)),
                     reads=["h2b", "wr"], writes=[psk[pi]], nosync_self=True)
            S.op("dve", lambda e: e.tensor_tensor(out=lg[:], in0=ps[pi][:, 0:NEXP], in1=brt[:], op=ALU.add),
                 reads=[psk[pi], "brt"], writes=["lg"])
            S.op("dve", lambda e: e.max(out=m8[:], in_=lg[:]), reads=["lg"], writes=["m8"])
            S.op("dve", lambda e: e.tensor_scalar(out=nmx[:], in0=m8[:, 0:1], scalar1=-1.0, scalar2=None, op0=ALU.mult),
                 reads=["m8"], writes=["nmx"])
            S.op("act", lambda e: e.activation(out=ex[:], in_=lg[:], func=AF.Exp, bias=nmx[:], scale=1.0),
                 reads=["lg", "nmx"], writes=["ex"])
            S.op("dve", lambda e: e.tensor_scalar(out=mk[:], in0=lg[:], scalar1=m8[:, 3:4], scalar2=None, op0=ALU.is_ge),
                 reads=["lg", "m8"], writes=["mk"])
            S.op("dve", lambda e: e.tensor_tensor(out=ex[:], in0=ex[:], in1=mk[:], op=ALU.mult), reads=["ex", "mk"], writes=["ex"])
            S.op("dve", lambda e: e.reduce_sum(out=rs[:], in_=ex[:], axis=AX.X), reads=["ex"], writes=["rs"])
            S.op("dve", lambda e: e.reciprocal(out=rs[:], in_=rs[:]), reads=["rs"], writes=["rs"])
            S.op("dve", lambda e: e.tensor_scalar(out=ex[:], in0=ex[:], scalar1=rs[:], scalar2=None, op0=ALU.mult),
                 reads=["ex", "rs"], writes=["ex"])
            transpose_to(gts[0:NEXP, :], "gts", ex[:], "ex", NEXP)
            S.dma("pool", GT_d[:, blk * 128:(blk + 1) * 128], gts[0:NEXP, :], reads=["gts"], writes=["GT_d"])
    pop()
    if stage <= 5:
        return nc, S, es

    FFT = dram_scr("FFT", [128, 16, 2048])
    push()
    wbuf = [T("wbig%d" % i, [128, 16, 512]) for i in range(2)]
    wk = ["wbig0", "wbig1"]
    TS = 512
    h2T = T("h2T", [128, 16, TS])
    accT = T("accT", [128, 16, TS])
    actT = T("actT", [128, 16, TS])
    gbc = [T("gbc%d" % i, [128, TS]) for i in range(2)]
    gt = T("gt", [NEXP, TS])
    bdn = T("bdn", [NEXP, D])
    S.dma("sp", R(bdn[:]), b_dn, writes=["bdn"])
    bgu = T("bgu", [128, NEXP, 16, 2])
    S.dma("sp", bgu[:], bgu_fm.rearrange("p (e j t) -> p e j t", e=NEXP, j=16), writes=["bgu"])
    tg_ = [T("tg%d" % i, [128, TS]) for i in range(2)]
    tsg = [T("tsg%d" % i, [128, TS]) for i in range(2)]
    tu = [T("tu%d" % i, [128, TS]) for i in range(2)]
    tsb = [T("tsb%d" % i, [128, TS]) for i in range(2)]
    nsp = int(os.environ.get("KDBG_NSP", 2048 // TS))
    nexp = int(os.environ.get("KDBG_NEXP", NEXP))
    for sp in range(nsp):
        t0 = sp * TS
        S.dma("sp", R(h2T[:]), R(H2T[:, :, t0:t0 + TS]), writes=["h2T"])
        S.dma("sp", R(gt[:]), R(GT_d[:, t0:t0 + TS]), writes=["gt"])
        for dc in range(16):
            pi = cnt["ps"] % 4
            cnt["ps"] += 1
            S.op("pe", lambda e: e.matmul(ps[pi][:, 0:TS], lhsT=R(bdn[:, dc * 128:(dc + 1) * 128]), rhs=R(gt[:]), start=True, stop=True),
                 reads=["bdn", "gt"], writes=[psk[pi]], nosync_self=True)
            S.op("act", lambda e: e.copy(out=accT[:, dc, :], in_=ps[pi][:, 0:TS]), reads=[psk[pi]], writes=["accT"])
        for ex_ in range(nexp):
            gb, gbk = gbc[ex_ % 2], "gbc%d" % (ex_ % 2)
            S.dma("sp", gb[:], GT_d[ex_:ex_ + 1, t0:t0 + TS].partition_broadcast(128), writes=[gbk])
            wgu_v = w_gu[ex_].rearrange("(kc p) f -> p kc f", p=128)
            wdn_v = w_dn[ex_].rearrange("(kc p) f -> p kc f", p=128)
            for jf2 in range(8):
                wt, wkey = load_w(wgu_v, jf2 * 512, 512, 16)
                for sub in range(2):
                    jf = jf2 * 2 + sub
                    c0 = sub * 256
                    pg = cnt["ps"] % 4; cnt["ps"] += 1
                    pu = cnt["ps"] % 4; cnt["ps"] += 1
                    for kc in range(16):
                        S.op("pe", lambda e, kc=kc: e.matmul(ps[pg][:, 0:TS], lhsT=R(wt[:, kc, c0:c0 + 256:2]), rhs=R(h2T[:, kc, :]),
                                                             start=(kc == 0), stop=(kc == 15)),
                             reads=[wkey, "h2T"], writes=[psk[pg]], nosync_self=True)
                    for kc in range(16):
                        S.op("pe", lambda e, kc=kc: e.matmul(ps[pu][:, 0:TS], lhsT=R(wt[:, kc, c0 + 1:c0 + 256:2]), rhs=R(h2T[:, kc, :]),
                                                             start=(kc == 0), stop=(kc == 15)),
                             reads=[wkey, "h2T"], writes=[psk[pu]], nosync_self=True)
                    a = jf % 2
                    S.op("dve", lambda e: e.tensor_scalar(out=tg_[a][:], in0=ps[pg][:, 0:TS], scalar1=bgu[:, ex_, jf, 0:1], scalar2=7.0,
                                                          op0=ALU.add, op1=ALU.min), reads=[psk[pg], "bgu"], writes=["tg%d" % a])
                    S.op("act", lambda e: e.activation(out=tsg[a][:], in_=tg_[a][:], func=AF.Sigmoid, scale=1.702),
                         reads=["tg%d" % a], writes=["tsg%d" % a])
                    S.op("act", lambda e: e.activation(out=tu[a][:], in_=ps[pu][:, 0:TS], func=AF.Identity,
                                                       bias=bgu[:, ex_, jf, 1:2], scale=1.0),
                         reads=[psk[pu], "bgu"], writes=["tu%d" % a])
                    S.op("pool", lambda e: e.tensor_tensor(out=tsb[a][:], in0=tsg[a][:], in1=gb[:], op=ALU.mult),
                         reads=["tsg%d" % a, gbk], writes=["tsb%d" % a])
                    S.op("dve", lambda e: e.tensor_scalar(out=tu[a][:], in0=tu[a][:], scalar1=7.0, scalar2=-7.0,
                                                          op0=ALU.min, op1=ALU.max), reads=["tu%d" % a], writes=["tu%d" % a])
                    S.op("dve", lambda e: e.tensor_tensor(out=tg_[a][:], in0=tg_[a][:], in1=tsb[a][:], op=ALU.mult),
                         reads=["tg%d" % a, "tsb%d" % a], writes=["tg%d" % a])
                    S.op("dve", lambda e: e.scalar_tensor_tensor(out=R(actT[:, jf, :]), in0=tu[a][:], scalar=1.0, in1=tg_[a][:],
                                                                 op0=ALU.add, op1=ALU.mult),
                         reads=["tu%d" % a, "tg%d" % a], writes=["actT"])
            for dg in range(4):
                wt, wkey = load_w(wdn_v, dg * 512, 512, 16)
                for dd in range(4):
                    dc = dg * 4 + dd
                    pi = cnt["ps"] % 4
                    cnt["ps"] += 1
                    for fc in range(16):
                        S.op("pe", lambda e, fc=fc: e.matmul(ps[pi][:, 0:TS], lhsT=R(wt[:, fc, dd * 128:(dd + 1) * 128]),
                                                             rhs=R(actT[:, fc, :]), start=(fc == 0), stop=(fc == 15)),
                             reads=[wkey, "actT"], writes=[psk[pi]], nosync_self=True)
                    S.op("dve", lambda e: e.tensor_tensor(out=accT[:, dc, :], in0=ps[pi][:, 0:TS], in1=accT[:, dc, :], op=ALU.add),
                         reads=[psk[pi], "accT"], writes=["accT"])
        S.dma("pool", FFT[:, :, t0:t0 + TS], accT[:], reads=["accT"], writes=["FFT"])
    pop()
    if stage <= 6:
        return nc, S, es

    push()
    g2b = T("g2b", [128, D]); lgb_t = T("lgb", [128, D]); lbb_t = T("lbb", [128, D])
    S.dma("sp", g2b[:], ada_d[:, 5 * D:6 * D].partition_broadcast(128), writes=["g2b"])
    S.dma("sp", lgb_t[:], lnfg.partition_broadcast(128), writes=["lgb"])
    S.dma("sp", lbb_t[:], lnfb.partition_broadcast(128), writes=["lbb"])
    st6 = T("st6", [128, 4, 6]); mv = T("mv", [128, 2]); rstd = T("rstd", [128, 1])
    fb = [T("fb%d" % i, [128, 16, 128]) for i in range(2)]
    yfl = [T("yf%d" % i, [128, D]) for i in range(2)]
    x1l = [T("x1t%d" % i, [128, D]) for i in range(2)]
    nblk = nsp * (TS // 128)
    for blk in range(nblk):
        a = blk % 2
        fbt, fbk = fb[a], "fb%d" % a
        yf, yk = yfl[a], "yf%d" % a
        x1t, x1k = x1l[a], "x1t%d" % a
        S.dma("sp", fbt[:], FFT[:, :, blk * 128:(blk + 1) * 128], writes=[fbk])
        S.dma("sp", x1t[:], X1[blk * 128:(blk + 1) * 128, :], writes=[x1k])
        for dc in range(16):
            i = cnt["ps"] % 4 + 4
            cnt["ps"] += 1
            S.op("pe", lambda e: e.transpose(out=ps[i][:, 0:128], in_=fbt[:, dc, :], identity=ident[:]),
                 reads=[fbk, "ident"], writes=[psk[i]])
            S.op("act", lambda e: e.copy(out=yf[:, dc * 128:(dc + 1) * 128], in_=ps[i][:, 0:128]), reads=[psk[i]], writes=[yk])
        S.op("pool", lambda e: e.tensor_tensor(out=yf[:], in0=yf[:], in1=g2b[:], op=ALU.mult), reads=[yk, "g2b"], writes=[yk])
        S.op("dve", lambda e: e.scalar_tensor_tensor(out=yf[:], in0=x1t[:], scalar=ALPHA, in1=yf[:], op0=ALU.mult, op1=ALU.add),
             reads=[yk, x1k], writes=[yk])
        _ln_stats(yf, D, yk)
        S.op("dve", lambda e: e.tensor_scalar(out=yf[:], in0=yf[:], scalar1=mv[:, 0:1], scalar2=rstd[:],
                                              op0=ALU.subtract, op1=ALU.mult), reads=[yk, "mv", "rstd"], writes=[yk])
        S.op("pool", lambda e: e.tensor_tensor(out=yf[:], in0=yf[:], in1=lgb_t[:], op=ALU.mult), reads=[yk, "lgb"], writes=[yk])
        S.op("pool", lambda e: e.tensor_tensor(out=yf[:], in0=yf[:], in1=lbb_t[:], op=ALU.add), reads=[yk, "lbb"], writes=[yk])
        S.dma("pool", out_d[blk * 128:(blk + 1) * 128, :], yf[:], reads=[yk], writes=["out"])
    pop()
    return nc, S, es


def finish(nc, S, out_written=True):
    S.barrier()


_INVF = np.concatenate([
    (THETA ** (-np.arange(16, dtype=np.float32) / np.float32(16))).astype(np.float32),
    (THETA ** (-np.arange(8, dtype=np.float32) / np.float32(8))).astype(np.float32),
    (THETA ** (-np.arange(32, dtype=np.float32) / np.float32(32))).astype(np.float32),
]).astype(np.float32)[None, :]


def make_in_maps(inp):
    f = lambda a: np.ascontiguousarray(np.asarray(a, dtype=np.float32))
    x = f(inp["x"]); c = f(inp["c"]); pos = np.asarray(inp["positions"]).astype(np.int32)
    shared = dict(
        w_ada=f(inp["w_ada"][0]), b_ada=f(inp["b_ada"][0])[None, :], w_in=f(inp["w_in"][0]),
        ikg=f(inp["idx_k_norm_g"][0])[None, :], ikb=f(inp["idx_k_norm_b"][0])[None, :],
        qng=f(inp["q_norm_g"][0])[None, :], w_q_up=f(inp["w_q_up"][0]),
        kvng=f(inp["kv_norm_g"][0])[None, :], w_kv_up=f(inp["w_kv_up"][0]),
        ong_fm=f(np.concatenate([np.asarray(inp["out_norm_a_g"][0]).reshape(8, 128),
                                 np.asarray(inp["out_norm_b_g"][0]).reshape(8, 128)], 0).T),
        w_out=f(inp["w_out"][0]), lnmg=f(inp["ln_mix_g"][0])[None, :], lnmb=f(inp["ln_mix_b"][0])[None, :],
        w_router=f(inp["w_router"][0]), b_router=f(inp["b_router"][0])[None, :],
        w_gu=f(inp["w_gate_up"][0]),
        bgu_fm=f(np.asarray(inp["b_gate_up"][0]).reshape(NEXP, 16, 128, 2).transpose(2, 0, 1, 3).reshape(128, NEXP * 32)),
        w_dn=f(inp["w_down"][0]), b_dn=f(inp["b_down"][0]),
        lnfg=f(inp["ln_ffn_g"][0])[None, :], lnfb=f(inp["ln_ffn_b"][0])[None, :],
        ident=np.eye(128, dtype=np.float32), ones=np.ones((128, 128), np.float32),
        invf=_INVF,
    )
    esel = np.zeros((NEXP, NEXP, 128), np.float32)
    for e in range(NEXP):
        esel[e, e, :] = 1.0
    shared["esel"] = esel.reshape(NEXP, NEXP * 128)
    qi = np.arange(128)[:, None] // 64
    ki = np.arange(128)[None, :] // 64
    diag_ok = (ki <= qi)
    shared["diagb"] = np.where(diag_ok, 0.0, NEG).astype(np.float32)
    maps = []
    for core in range(8):
        b, r = core // 2, core % 2
        own_blocks = [2 * i + r for i in range(NOWN)]
        oth_blocks = [2 * i + 1 - r for i in range(NOWN)]
        order = own_blocks + oth_blocks
        xb = x[b].reshape(NB, 128, D)[order].reshape(SEQ, D)
        pb = pos[b].reshape(NB, 128)[order]
        m = dict(shared)
        m["xl"] = np.ascontiguousarray(xb)
        m["c_fm"] = np.ascontiguousarray(c[b].reshape(16, 128).T)
        m["pos_i"] = np.ascontiguousarray(pb.T.astype(np.int32))
        m["rbias"] = np.full((128, 128), 0.0 if r == 1 else NEG, np.float32)
        dT = diag_ok.T.astype(np.float32)
        one = np.ones((128, 128), np.float32); zero = np.zeros((128, 128), np.float32)
        rr = one * float(r)
        mm = np.stack([np.concatenate([dT, one], 1), np.concatenate([zero, dT], 1),
                       np.concatenate([rr, one], 1), np.concatenate([zero, rr], 1)], 1)
        m["mlam"] = np.ascontiguousarray(mm.reshape(128, 1024).astype(np.float32))
        maps.append(m)
    return maps


def kernel(**inputs):
    nc, S, es = build()
    finish(nc, S)
    maps = make_in_maps(inputs)
    res = run_bass_kernel_spmd(nc, maps, core_ids=list(range(8)))
    out = np.zeros((4, SEQ, D), np.float32)
    for core in range(8):
        b, r = core // 2, core % 2
        o = np.asarray(res.results[core]["out"]).reshape(NOWN, 128, D)
        ov = out[b].reshape(NB, 128, D)
        for i in range(NOWN):
            ov[2 * i + r] = o[i]
    return out
```
